# Optimizing a Trainium2 kernel written in Bass

```python
import math
import jax, jax.numpy as jnp
from jax import lax
import numpy as np

D_MODEL = 1024
BATCH = 8
SEQ = 4096
DEPTH = 2

GRID_W = 64
CTX_LEN = 256
HEAD_DIM = 64
N_BRANCH = 4
BRANCH_W = D_MODEL // 4
POOL_WINDOWS = (2, 4, 8, 16)
POOL_GROUPS = 4
POOL_GW = BRANCH_W // POOL_GROUPS
DIFF_HEADS = 4
DIFF_DQ = HEAD_DIM // 2
DIFF_DV = HEAD_DIM
NAT_HEADS = 4
NAT_WIN_R = 8
NAT_WIN_C = 16
GQA_HEADS = 4
GQA_KV_HEADS = 2
ROPE_THETA = 10000.0
QBLK = 128
N_GROUPS = 4
EXPERTS_PER_GROUP = 8
N_EXPERTS = N_GROUPS * EXPERTS_PER_GROUP
TOP_K_IN_GROUP = 2
D_EXPERT = 512
RMS_EPS = 1e-6
NEG_INF = -1e30
SPLIT_WIDTHS = (BRANCH_W,
                DIFF_HEADS * HEAD_DIM, DIFF_HEADS * HEAD_DIM, DIFF_HEADS * DIFF_DV,
                NAT_HEADS * HEAD_DIM, NAT_HEADS * HEAD_DIM, NAT_HEADS * HEAD_DIM,
                GQA_HEADS * HEAD_DIM, GQA_KV_HEADS * HEAD_DIM, GQA_KV_HEADS * HEAD_DIM,
                N_BRANCH * D_MODEL)
IN_COLS = sum(SPLIT_WIDTHS)
CTX_KV_IDX = (2, 3, 5, 6, 8, 9)

kernel_name = 'hybrid_pool_diff_nat_gqa_hmoe_dit'


def _rmsnorm(x, g):
    xf = x.astype(jnp.float32)
    y = xf * lax.rsqrt(jnp.mean(xf * xf, axis=-1, keepdims=True) + RMS_EPS)
    return y.astype(x.dtype) * g


def _modulate(x, g, shift, scale):
    return _rmsnorm(x, g) * (1.0 + scale[:, None]) + shift[:, None]


def _split_cols(a):
    outs, o = [], 0
    for w in SPLIT_WIDTHS:
        outs.append(a[..., o:o + w])
        o += w
    return outs


def _rope_1d(x, pos):
    half = x.shape[-1] // 2
    inv = ROPE_THETA ** (-jnp.arange(half, dtype=jnp.float32) / half)
    ang = pos.astype(jnp.float32)[:, None] * inv[None, :]
    cos = jnp.cos(ang).astype(x.dtype)
    sin = jnp.sin(ang).astype(x.dtype)
    x1, x2 = x[..., :half], x[..., half:]
    return jnp.concatenate([x1 * cos - x2 * sin, x1 * sin + x2 * cos], axis=-1)


def _axial_rope(x):
    n, d = x.shape[-2], x.shape[-1]
    t = jnp.arange(n, dtype=jnp.int32)
    h = d // 2
    return jnp.concatenate([_rope_1d(x[..., :h], t // GRID_W), _rope_1d(x[..., h:], t % GRID_W)], axis=-1)


def _heads(a, n_heads):
    b, n, _ = a.shape
    return a.reshape(b, n, n_heads, -1).transpose(0, 2, 1, 3)


def _unheads(o):
    b, h, n, d = o.shape
    return o.transpose(0, 2, 1, 3).reshape(b, n, h * d)


def _sweep_queries(fn, q):
    n = q.shape[-2]
    nb = n // QBLK
    qb = jnp.moveaxis(q.reshape(q.shape[:-2] + (nb, QBLK, q.shape[-1])), -3, 0)
    out = jnp.moveaxis(lax.map(fn, qb), 0, -3)
    return out.reshape(out.shape[:-3] + (n, out.shape[-1]))


def _dense_core(q, k, v, scale):
    s = jnp.einsum('bhqd,bhkd->bhqk', q, k).astype(jnp.float32) * scale
    p = jax.nn.softmax(s, axis=-1)
    return jnp.einsum('bhqk,bhkd->bhqd', p.astype(v.dtype), v)


def _pool_mixer(u, pool_w, pool_scale):
    b, n, ch = u.shape
    cs = jnp.concatenate([jnp.zeros((b, 1, ch), jnp.float32), jnp.cumsum(u.astype(jnp.float32), axis=1)], axis=1)
    win = jnp.repeat(jnp.array(POOL_WINDOWS, jnp.int32), POOL_GW)
    t = jnp.arange(n, dtype=jnp.int32)[:, None]
    lo = jnp.clip(t - win // 2, 0, n)
    hi = jnp.clip(t + win - win // 2, 0, n)
    chan = jnp.arange(ch)[None, :]
    mean = (cs[:, hi, chan] - cs[:, lo, chan]) / (hi - lo).astype(jnp.float32)
    pooled = (mean - u.astype(jnp.float32)).astype(u.dtype)
    y = jnp.einsum('bngc,gcd->bngd', pooled.reshape(b, n, POOL_GROUPS, POOL_GW), pool_w)
    return y.reshape(b, n, ch) * pool_scale


def _diff_core(qb, k, v, lam):
    s = jnp.einsum('bhcqd,bhckd->bhcqk', qb, k).astype(jnp.float32) * (DIFF_DQ ** -0.5)
    p = jax.nn.softmax(s, axis=-1)
    pd = p[:, :, 0] - lam * p[:, :, 1]
    return jnp.einsum('bhqk,bhkd->bhqd', pd.astype(v.dtype), v)


def _diff_attention(q_l, k_l, v_l, q_c, k_c, v_c, lam_p, norm_g, layer, need_ctx):
    lam_init = 0.8 - 0.6 * math.exp(-0.3 * layer)
    lf = lam_p.astype(jnp.float32)
    lam = jnp.exp(jnp.sum(lf[0] * lf[1])) - jnp.exp(jnp.sum(lf[2] * lf[3])) + lam_init

    def qk_heads(a):
        b, n, _ = a.shape
        return a.reshape(b, n, DIFF_HEADS, 2, DIFF_DQ).transpose(0, 2, 3, 1, 4)

    def finish(o, dtype):
        return _unheads(_rmsnorm(o, norm_g) * (1.0 - lam_init)).astype(dtype)

    k_ctx = qk_heads(k_c)
    v_ctx = _heads(v_c, DIFF_HEADS)
    k_all = jnp.concatenate([_axial_rope(qk_heads(k_l)), k_ctx], axis=3)
    v_all = jnp.concatenate([_heads(v_l, DIFF_HEADS), v_ctx], axis=2)
    o_l = _sweep_queries(lambda qb: _diff_core(qb, k_all, v_all, lam), _axial_rope(qk_heads(q_l)))
    y_l = finish(o_l, q_l.dtype)
    y_c = finish(_diff_core(qk_heads(q_c), k_ctx, v_ctx, lam), q_c.dtype) if need_ctx else None
    return y_l, y_c


def _neighbourhood_attention(q_l, k_l, v_l, q_c, k_c, v_c, rpb, need_ctx):
    b, n, _ = q_l.shape
    rows = n // GRID_W
    kr = min(NAT_WIN_R, rows)
    kc = min(NAT_WIN_C, GRID_W)
    scale = HEAD_DIM ** -0.5

    def grid(a):
        return a.reshape(b, rows, GRID_W, NAT_HEADS, HEAD_DIM).transpose(0, 3, 1, 2, 4)

    qg, kg, vg = grid(q_l), grid(k_l), grid(v_l)
    k_ctx, v_ctx = _heads(k_c, NAT_HEADS), _heads(v_c, NAT_HEADS)
    r = jnp.arange(rows, dtype=jnp.int32)
    row_idx = jnp.clip(r - kr // 2, 0, rows - kr)[:, None] + jnp.arange(kr, dtype=jnp.int32)[None, :]
    dr = row_idx - r[:, None] + (NAT_WIN_R - 1)
    j = jnp.arange(GRID_W, dtype=jnp.int32)
    col_start = jnp.clip(j - kc // 2, 0, GRID_W - kc)
    valid = (j[None, :] >= col_start[:, None]) & (j[None, :] < col_start[:, None] + kc)
    dc = jnp.clip(j[None, :] - j[:, None], -(NAT_WIN_C - 1), NAT_WIN_C - 1) + (NAT_WIN_C - 1)

    def row_block(args):
        q_r, ridx, dr_r = args
        kb = kg[:, :, ridx]
        vb = vg[:, :, ridx]
        bias = rpb[:, dr_r[None, :, None], dc[:, None, :]].astype(jnp.float32)
        s_band = jnp.einsum('bhqd,bhnwd->bhqnw', q_r, kb).astype(jnp.float32) * scale + bias[None]
        s_band = jnp.where(valid[:, None, :], s_band, NEG_INF).reshape(b, NAT_HEADS, GRID_W, kr * GRID_W)
        s_ctx = jnp.einsum('bhqd,bhkd->bhqk', q_r, k_ctx).astype(jnp.float32) * scale
        p = jax.nn.softmax(jnp.concatenate([s_band, s_ctx], axis=-1), axis=-1)
        p_band = p[..., :kr * GRID_W].reshape(b, NAT_HEADS, GRID_W, kr, GRID_W).astype(vb.dtype)
        p_ctx = p[..., kr * GRID_W:].astype(v_ctx.dtype)
        return (jnp.einsum('bhqnw,bhnwd->bhqd', p_band, vb)
                + jnp.einsum('bhqk,bhkd->bhqd', p_ctx, v_ctx))

    o = lax.map(row_block, (jnp.moveaxis(qg, 2, 0), row_idx, dr))
    y_l = o.transpose(1, 0, 3, 2, 4).reshape(b, n, NAT_HEADS * HEAD_DIM)
    y_c = _unheads(_dense_core(_heads(q_c, NAT_HEADS), k_ctx, v_ctx, scale)) if need_ctx else None
    return y_l, y_c


def _gqa_core(qb, k, v):
    s = jnp.einsum('bhgqd,bhkd->bhgqk', qb, k).astype(jnp.float32) * (HEAD_DIM ** -0.5)
    p = jax.nn.softmax(s, axis=-1)
    return jnp.einsum('bhgqk,bhkd->bhgqd', p.astype(v.dtype), v)


def _gqa_attention(q_l, k_l, v_l, q_c, k_c, v_c, q_norm, k_norm, need_ctx):
    grp = GQA_HEADS // GQA_KV_HEADS

    def q_heads(a):
        b, n, _ = a.shape
        return _rmsnorm(a.reshape(b, n, GQA_KV_HEADS, grp, HEAD_DIM), q_norm).transpose(0, 2, 3, 1, 4)

    def k_heads(a):
        b, n, _ = a.shape
        return _rmsnorm(a.reshape(b, n, GQA_KV_HEADS, HEAD_DIM), k_norm).transpose(0, 2, 1, 3)

    def merge(o):
        b, h, g, n, d = o.shape
        return o.transpose(0, 3, 1, 2, 4).reshape(b, n, h * g * d)

    k_ctx, v_ctx = k_heads(k_c), _heads(v_c, GQA_KV_HEADS)
    k_all = jnp.concatenate([_axial_rope(k_heads(k_l)), k_ctx], axis=2)
    v_all = jnp.concatenate([_heads(v_l, GQA_KV_HEADS), v_ctx], axis=2)
    y_l = merge(_sweep_queries(lambda qb: _gqa_core(qb, k_all, v_all), _axial_rope(q_heads(q_l))))
    y_c = merge(_gqa_core(q_heads(q_c), k_ctx, v_ctx)) if need_ctx else None
    return y_l, y_c


def _merge_branches(branches, gate_logits, w_branch, w_out):
    b, n, _ = gate_logits.shape
    gates = jax.nn.sigmoid(gate_logits.astype(jnp.float32)).astype(gate_logits.dtype).reshape(b, n, N_BRANCH, D_MODEL)
    merged = gates[:, :, 0] * (branches[0] @ w_branch[0])
    for i in range(1, N_BRANCH):
        merged = merged + gates[:, :, i] * (branches[i] @ w_branch[i])
    return merged @ w_out


def _mixer(h_lat, h_ctx, w_in, pool_w, pool_scale, diff_lam, diff_g, rpb, q_norm, k_norm, w_branch, w_out, layer, need_ctx):
    pl = _split_cols(h_lat @ w_in)
    if need_ctx:
        pc = _split_cols(h_ctx @ w_in)
    else:
        pc = [h_ctx @ blk if i in CTX_KV_IDX else None for i, blk in enumerate(_split_cols(w_in))]
    d_l, d_c = _diff_attention(pl[1], pl[2], pl[3], pc[1], pc[2], pc[3], diff_lam, diff_g, layer, need_ctx)
    n_l, n_c = _neighbourhood_attention(pl[4], pl[5], pl[6], pc[4], pc[5], pc[6], rpb, need_ctx)
    g_l, g_c = _gqa_attention(pl[7], pl[8], pl[9], pc[7], pc[8], pc[9], q_norm, k_norm, need_ctx)
    out_l = _merge_branches((_pool_mixer(pl[0], pool_w, pool_scale), d_l, n_l, g_l), pl[10], w_branch, w_out)
    out_c = None
    if need_ctx:
        out_c = _merge_branches((_pool_mixer(pc[0], pool_w, pool_scale), d_c, n_c, g_c), pc[10], w_branch, w_out)
    return out_l, out_c


def _moe(h, w_rg, b_rg, w_re, b_re, w_gate, w_up, w_down):
    b, n, d = h.shape
    t = h.reshape(b * n, d)
    nt = t.shape[0]
    tok = jnp.arange(nt)
    g_logits = (t @ w_rg).astype(jnp.float32) + b_rg.astype(jnp.float32)
    g_prob = jax.nn.softmax(g_logits, axis=-1)
    grp = jnp.argmax(g_logits, axis=-1)
    w_grp = g_prob[tok, grp][:, None]
    e_logits = ((t @ w_re).astype(jnp.float32) + b_re.astype(jnp.float32)).reshape(nt, N_GROUPS, EXPERTS_PER_GROUP)
    top_v, top_i = lax.top_k(e_logits[tok, grp], TOP_K_IN_GROUP)
    weights = jax.nn.softmax(top_v, axis=-1) * w_grp
    eid = grp[:, None] * EXPERTS_PER_GROUP + top_i
    comb = jnp.sum(jax.nn.one_hot(eid, N_EXPERTS, dtype=jnp.float32) * weights[..., None], axis=1).astype(h.dtype)
    y = jnp.zeros_like(t)
    for e in range(N_EXPERTS):
        a = jax.nn.silu(t @ w_gate[e]) * (t @ w_up[e])
        y = y + comb[:, e:e + 1] * (a @ w_down[e])
    return y.reshape(b, n, d)


def setup_inputs(seed: int = 0) -> dict:
    key = jax.random.key(seed)
    ks = iter(jax.random.split(key, 32))
    L, D = DEPTH, D_MODEL

    def nrm(shape, s):
        return jax.random.normal(next(ks), shape, jnp.float32) * s

    return {
        'x': nrm((BATCH, SEQ, D), 1.0),
        'c': nrm((BATCH, D), 1.0),
        'ctx': nrm((BATCH, CTX_LEN, D), 1.0),
        'c_ctx': nrm((D,), 1.0),
        'w_mod': nrm((L, D, 6 * D), 0.5 * D ** -0.5),
        'b_mod': nrm((L, 6 * D), 0.02),
        'g_mix': 1.0 + nrm((L, D), 0.02),
        'g_ffn': 1.0 + nrm((L, D), 0.02),
        'w_in': nrm((L, D, IN_COLS), D ** -0.5),
        'pool_w': nrm((L, POOL_GROUPS, POOL_GW, POOL_GW), POOL_GW ** -0.5),
        'pool_scale': 1.0 + nrm((L, BRANCH_W), 0.02),
        'diff_lambda': nrm((L, 4, DIFF_DQ), 0.1),
        'diff_norm_g': 1.0 + nrm((L, DIFF_DV), 0.02),
        'nat_rpb': nrm((L, NAT_HEADS, 2 * NAT_WIN_R - 1, 2 * NAT_WIN_C - 1), 0.02),
        'gqa_q_norm': 1.0 + nrm((L, HEAD_DIM), 0.02),
        'gqa_k_norm': 1.0 + nrm((L, HEAD_DIM), 0.02),
        'w_branch': nrm((L, N_BRANCH, BRANCH_W, D), BRANCH_W ** -0.5),
        'w_out': nrm((L, D, D), D ** -0.5),
        'w_router_group': nrm((L, D, N_GROUPS), D ** -0.5),
        'b_router_group': nrm((L, N_GROUPS), 0.01),
        'w_router_expert': nrm((L, D, N_EXPERTS), D ** -0.5),
        'b_router_expert': nrm((L, N_EXPERTS), 0.01),
        'w_exp_gate': nrm((L, N_EXPERTS, D, D_EXPERT), D ** -0.5),
        'w_exp_up': nrm((L, N_EXPERTS, D, D_EXPERT), D ** -0.5),
        'w_exp_down': nrm((L, N_EXPERTS, D_EXPERT, D), D_EXPERT ** -0.5),
        'g_final': 1.0 + nrm((D,), 0.02),
    }


def reference(x, c, ctx, c_ctx, w_mod, b_mod, g_mix, g_ffn, w_in, pool_w, pool_scale, diff_lambda, diff_norm_g,
              nat_rpb, gqa_q_norm, gqa_k_norm, w_branch, w_out, w_router_group, b_router_group, w_router_expert,
              b_router_expert, w_exp_gate, w_exp_up, w_exp_down, g_final):
    x_lat, x_ctx = x, ctx
    s_lat = jax.nn.silu(c)
    s_ctx = jax.nn.silu(c_ctx)[None]
    for l in range(DEPTH):
        need_ctx = l < DEPTH - 1
        sh1, sc1, gt1, sh2, sc2, gt2 = jnp.split(s_lat @ w_mod[l] + b_mod[l], 6, axis=-1)
        csh1, csc1, cgt1, csh2, csc2, cgt2 = jnp.split(s_ctx @ w_mod[l] + b_mod[l], 6, axis=-1)
        h_lat = _modulate(x_lat, g_mix[l], sh1, sc1)
        h_ctx = _modulate(x_ctx, g_mix[l], csh1, csc1)
        o_lat, o_ctx = _mixer(h_lat, h_ctx, w_in[l], pool_w[l], pool_scale[l], diff_lambda[l], diff_norm_g[l],
                              nat_rpb[l], gqa_q_norm[l], gqa_k_norm[l], w_branch[l], w_out[l], l, need_ctx)
        x_lat = x_lat + gt1[:, None] * o_lat
        x_lat = x_lat + gt2[:, None] * _moe(_modulate(x_lat, g_ffn[l], sh2, sc2), w_router_group[l], b_router_group[l],
                                            w_router_expert[l], b_router_expert[l], w_exp_gate[l], w_exp_up[l], w_exp_down[l])
        if need_ctx:
            x_ctx = x_ctx + cgt1[:, None] * o_ctx
            x_ctx = x_ctx + cgt2[:, None] * _moe(_modulate(x_ctx, g_ffn[l], csh2, csc2), w_router_group[l], b_router_group[l],
                                                 w_router_expert[l], b_router_expert[l], w_exp_gate[l], w_exp_up[l], w_exp_down[l])
    return _rmsnorm(x_lat, g_final)
```

```python
import math
from contextlib import ExitStack
import numpy as np
import concourse.bass as bass
import concourse.mybir as mybir
from concourse.bass_utils import run_bass_kernel_spmd

F32 = mybir.dt.float32
BF16 = mybir.dt.bfloat16
AF = mybir.ActivationFunctionType
ALU = mybir.AluOpType
AX = mybir.AxisListType

D = 1024
NLAT = 4096
NCTX = 256
NTOK = NLAT + NCTX
NT = NTOK // 128
DEPTH = 2
EPS = 1e-6
NEG = -1e30
SPARSE = True
I32 = mybir.dt.int32
DBG_BARRIER = False


class Sched:
    ENG = ('pe', 'act', 'dve', 'pool', 'sp')

    def __init__(self, nc, es):
        self.nc = nc
        self.es = es
        self.eh = {'pe': nc.tensor, 'act': nc.scalar, 'dve': nc.vector, 'pool': nc.gpsimd, 'sp': nc.sync}
        self.sem = {e: es.enter_context(nc.semaphore("sem_" + e)) for e in self.ENG}
        self.cnt = {e: 0 for e in self.ENG}
        self.seen = {e: {} for e in self.ENG}
        self.lastw = {}
        self.readers = {}
        self.dsem = {}
        self.free_ds = []
        self.nds = 0
        self.keep = set()
        self.keep_res = set()
        self.inputs = set()
        self.nops = 0
        self.nwaits = 0

    def _resolve(self, eng, deps):
        need = {}
        for tok, kind in deps:
            if tok is None:
                continue
            if tok[0] == 'E':
                f, idx = tok[1], tok[2]
                if f == eng and kind != 'raw':
                    continue
                key = ('E', f)
                h = self.sem[f]
            else:
                key = ('D', tok[1])
                idx = tok[2]
                h = tok[3]
            if self.seen[eng].get(key, 0) >= idx:
                continue
            if key not in need or need[key][1] < idx:
                need[key] = (h, idx)
        e = self.eh[eng]
        for key, (h, idx) in need.items():
            e.wait_ge(h, idx)
            self.seen[eng][key] = idx
            self.nwaits += 1

    def _deps(self, r, w):
        deps = []
        for x in r:
            if x in self.lastw:
                deps.append((self.lastw[x], 'raw'))
            elif x[0] not in self.inputs:
                raise KeyError("read of never-written resource %r" % (x,))
        for x in w:
            if x in self.lastw:
                deps.append((self.lastw[x], 'waw'))
            for t in self.readers.get(x, ()):
                deps.append((t, 'war'))
        return deps

    def _commit(self, tok, r, w):
        for x in w:
            self.lastw[x] = tok
            self.readers[x] = []
        for x in r:
            self.readers.setdefault(x, []).append(tok)

    def op(self, eng, fn, r=(), w=()):
        r = list(r)
        w = list(w)
        w += [x for x in r if x[0] == 'ps' and x not in w]
        self._resolve(eng, self._deps(r, w))
        ins = fn(self.eh[eng])
        self.cnt[eng] += 1
        ins.then_inc(self.sem[eng], 1)
        self._commit(('E', eng, self.cnt[eng]), r, w)
        self.nops += 1

    def dma(self, q, out, in_, own, r=(), w=(), chain=True, indirect=None, **kw):
        r = list(r)
        w = list(w)
        if own not in self.dsem:
            fl = [d for d in self.free_ds if d[4] == q]
            if fl:
                self.free_ds.remove(fl[0])
                self.dsem[own] = fl[0]
            else:
                h = self.es.enter_context(self.nc.semaphore("dsem%d" % self.nds))
                self.nds += 1
                self.dsem[own] = [h, 0, None, self.nds, q]
        ds = self.dsem[own]
        deps = self._deps(r, w)
        if chain:
            deps.append((ds[2], 'raw'))
        self._resolve(q, deps)
        if indirect is None:
            self.eh[q].dma_start(out=out, in_=in_, **kw).then_inc(ds[0], 16)
        else:
            kind, idx_ap = indirect
            off = bass.IndirectOffsetOnAxis(ap=idx_ap, axis=0)
            self.nc.gpsimd.indirect_dma_start(out=out, out_offset=(off if kind == 'scatter' else None),
                                              in_=in_, in_offset=(off if kind == 'gather' else None), **kw).then_inc(ds[0], 16)
        ds[1] += 16
        tok = ('D', ds[3], ds[1], ds[0])
        ds[2] = tok
        self._commit(tok, r, w)
        self.nops += 1

    def barrier(self, engines=None):
        engines = engines or self.ENG
        for e in engines:
            deps = [(('E', f, self.cnt[f]), 'raw') for f in self.ENG if f != e and self.cnt[f] > 0]
            for k, ds in list(self.dsem.items()) + [(None, d) for d in self.free_ds]:
                if ds[2] is not None and k not in self.keep:
                    deps.append((ds[2], 'raw'))
            self._resolve(e, deps)

    def phase_end(self):
        self.barrier()
        for k in list(self.dsem.keys()):
            if k not in self.keep:
                self.free_ds.append(self.dsem.pop(k))
        for k in self.lastw:
            if k[0] not in self.keep_res:
                self.lastw[k] = None
        self.readers.clear()


def R(*a):
    return a


def _band_mats():
    out = np.zeros((128, 20, 128), np.float32)
    for g, w in enumerate((2, 4, 8, 16)):
        def mat(J, dl, n):
            m = np.zeros((128, 128), np.float32)
            for q in range(128):
                t = J * 128 + q
                lo = max(t - w // 2, 0)
                hi = min(t + w - w // 2, n)
                for tp in range(lo, hi):
                    p = tp - (J + dl) * 128
                    if 0 <= p < 128:
                        m[p, q] += 1.0 / (hi - lo)
                p = t - (J + dl) * 128
                if 0 <= p < 128:
                    m[p, q] -= 1.0
            return m
        out[:, g * 5 + 0] = mat(5, -1, 1280)
        out[:, g * 5 + 1] = mat(5, 0, 1280)
        out[:, g * 5 + 2] = mat(5, 1, 1280)
        out[:, g * 5 + 3] = mat(0, 0, 1280)
        out[:, g * 5 + 4] = mat(9, 0, 1280)
    return out


def _rope_tabs(half):
    t = np.arange(NLAT)
    inv = (10000.0 ** (-np.arange(half, dtype=np.float32) / half)).astype(np.float32)
    cos = np.zeros((128, 32, 2, half), np.float32)
    sin = np.zeros((128, 32, 2, half), np.float32)
    for a, pos in enumerate((t // 64, t % 64)):
        ang = pos.astype(np.float32)[:, None] * inv[None, :]
        c = np.cos(ang).astype(np.float32).reshape(32, 128, half)
        s = np.sin(ang).astype(np.float32).reshape(32, 128, half)
        cos[:, :, a, :] = c.transpose(1, 0, 2)
        sin[:, :, a, :] = s.transpose(1, 0, 2)
    return cos, sin


def _nat_variant(j, t):
    if 2 <= j <= 29:
        return (t - j) + 2
    if j == 0:
        return 5 + t
    if j == 1:
        return 9 + t
    if j == 30:
        return 13 + (t - 28)
    return 17 + (t - 28)


def _nat_keytiles(j):
    rows = [2 * j, 2 * j + 1]
    lo = min(min(max(r - 4, 0), 56) for r in rows)
    hi = max(min(max(r - 4, 0), 56) + 7 for r in rows)
    return list(range(lo // 2, hi // 2 + 1))


def _nat_plan():
    plan = {}
    mask = np.full((128, 21, 128), NEG, np.float32)
    qc = np.arange(64)
    cs = np.clip(qc - 8, 0, 48)
    kc = np.arange(64)
    colvalid = (kc[None, :] >= cs[:, None]) & (kc[None, :] < cs[:, None] + 16)
    for j in range(32):
        for t in _nat_keytiles(j):
            v = _nat_variant(j, t)
            blocks = {}
            for a in range(2):
                for b in range(2):
                    qr = 2 * j + b
                    kr = 2 * t + a
                    rs = min(max(qr - 4, 0), 56)
                    ok = rs <= kr < rs + 8
                    blocks[(a, b)] = (kr - qr + 7) if ok else None
                    if ok:
                        m = np.where(colvalid, 0.0, NEG).astype(np.float32)
                        mask[b * 64:(b + 1) * 64, v, a * 64:(a + 1) * 64] = m[::-1, :]
            if v in plan:
                assert plan[v] == blocks
            plan[v] = blocks
    return plan, mask


def _host_consts():
    c = {}
    c['identf'] = np.eye(128, dtype=np.float32)
    anti = np.zeros((128, 128), np.float32)
    for b in range(2):
        for q in range(64):
            anti[b * 64 + 63 - q, b * 64 + q] = 1.0
    c['antii'] = anti
    c['band'] = _band_mats()
    c['cosg'], c['sing'] = _rope_tabs(16)
    c['cosd'], c['sind'] = _rope_tabs(8)
    _, c['natmask'] = _nat_plan()
    c['ltri'] = np.triu(np.ones((128, 128), np.float32), 1)
    c['ustrict'] = np.triu(np.ones((32, 32), np.float32), 1)
    c['thr'] = np.tile((128.0 * np.arange(100, dtype=np.float32))[None, :], (128, 1))
    c['iota'] = np.arange(128, dtype=np.float32).reshape(128, 1)
    return c


def build(stop=None, dbg=False):
    nc = bass.Bass("TRN2", target_bir_lowering=False)

    def din(name, shape, dt=F32):
        return nc.dram_tensor(name, list(shape), dt, kind="ExternalInput").ap()

    def dscr(name, shape, dt=F32):
        return nc.dram_tensor(name, list(shape), dt, kind=("ExternalOutput" if dbg else "Internal")).ap()

    x_in = din("x", [NLAT, D])
    ctx_in = din("ctx", [NCTX, D])
    c_in = din("c", [D])
    cctx_in = din("c_ctx", [D])
    w_mod = din("w_mod", [2, D, 6 * D])
    b_mod = din("b_mod", [2, 6 * D])
    g_mix = din("g_mix", [2, D])
    g_ffn = din("g_ffn", [2, D])
    w_in = din("w_in", [2, D, 6400])
    pool_w = din("pool_w", [2, 4, 64, 64])
    pool_scale = din("pool_scale", [2, 256])
    diff_lambda = din("diff_lambda", [2, 128])
    diff_norm_g = din("diff_norm_g", [2, 64])
    nat_rpb = din("nat_rpb", [2, 60, 31])
    gqa_q_norm = din("gqa_q_norm", [2, 64])
    gqa_k_norm = din("gqa_k_norm", [2, 64])
    w_branch = din("w_branch", [2, 4, 256, D])
    w_out = din("w_out", [2, D, D])
    w_rg = din("w_router_group", [2, D, 4])
    b_rg = din("b_router_group", [2, 4])
    w_re = din("w_router_expert", [2, D, 32])
    b_re = din("b_router_expert", [2, 32])
    w_eg = din("w_exp_gate", [2, 32, D, 512])
    w_eu = din("w_exp_up", [2, 32, D, 512])
    w_ed = din("w_exp_down", [2, 32, 512, D])
    g_final = din("g_final", [D])
    identf_in = din("identf", [128, 128])
    antii_in = din("antii", [128, 128])
    band_in = din("band", [128, 20, 128])
    cosg_in = din("cosg", [128, 32, 2, 16])
    sing_in = din("sing", [128, 32, 2, 16])
    cosd_in = din("cosd", [128, 32, 2, 8])
    sind_in = din("sind", [128, 32, 2, 8])
    natmask_in = din("natmask", [128, 21, 128])
    ltri_in = din("ltri", [128, 128])
    ustrict_in = din("ustrict", [32, 32])
    thr_in = din("thr", [128, 100])
    iota_in = din("iota", [128, 1])

    out_d = nc.dram_tensor("out", [NLAT, D], F32, kind="ExternalOutput").ap()
    xs_d = dscr("xs", [NTOK, D])
    hT_d = dscr("hT", [8, 128, NTOK], BF16)
    brT_d = dscr("brT", [4, 4, 64, NTOK], BF16)
    mT_d = dscr("mT", [8, 128, NTOK], BF16)
    modd = dscr("modd", [2, 2, 6 * D])
    rpbpad = dscr("rpbpad", [2, 60, 128])
    wbf_d = [nc.dram_tensor("wbf%d" % i, [32 * 128, 12288], BF16, kind="Internal").ap() for i in range(2)]
    xslot_d = nc.dram_tensor("xslot", [100 * 128, D], BF16, kind="Internal").ap()
    yslot_d = nc.dram_tensor("yslot", [100 * 128, D], F32, kind="Internal").ap()

    nat_plan, _ = _nat_plan()

    def kind_of(tile):
        return 0 if tile < 32 else 1

    GROUPS = [list(range(g * 4, g * 4 + 4)) for g in range(8)] + [[32, 33]]

    def x_src(l, tiles, after_mix=False):
        t0 = tiles[0]
        n = len(tiles)
        if l == 0 and not after_mix:
            if t0 < 32:
                return x_in[t0 * 128:(t0 + n) * 128, :].rearrange("(t p) d -> p t d", p=128)
            return ctx_in[(t0 - 32) * 128:(t0 - 32 + n) * 128, :].rearrange("(t p) d -> p t d", p=128)
        return xs_d[t0 * 128:(t0 + n) * 128, :].rearrange("(t p) d -> p t d", p=128)

    def x_res(l, tiles, after_mix=False):
        if l == 0 and not after_mix:
            return [R('xin', t) for t in tiles]
        return [R('xs', t) for t in tiles]

    with ExitStack() as es:
        S = Sched(nc, es)
        S.inputs.add('xin')

        _tn = [0]

        def T(ctx, name, shape, dt):
            _tn[0] += 1
            return ctx.enter_context(nc.sbuf_tensor("%s_%d" % (name, _tn[0]), list(shape), dt))

        def mk(t, p0, pn, off, dims):
            row = 1
            for s in t.shape[1:]:
                row *= int(s)
            return bass.AP(t, p0 * row + off, [[row, pn]] + [list(d) for d in dims])

        ps = [es.enter_context(nc.psum_tensor("ps%d" % i, [128, 512], F32)) for i in range(8)]
        psb = [p.bitcast(BF16) for p in ps]

        def PR(i):
            return R('ps', i)

        identF = T(es, "identF", [128, 128], F32)
        identB = T(es, "identB", [128, 128], BF16)
        antiB = T(es, "antiB", [128, 128], BF16)
        colsP = T(es, "colsP", [128, 64], F32)
        cols64 = T(es, "cols64", [64, 8], F32)
        sT = T(es, "sT", [128, 8, 2], F32)
        modP = T(es, "modP", [128, 2, 2, 4, 8], F32)
        nlam = T(es, "nlam", [128, 2], F32)
        gainG = T(es, "gainG", [128, 2, 8, 64], F32)
        gB = T(es, "gB", [128, 2, 64], F32)
        epsT = T(es, "epsT", [128, 1], F32)
        mhalf = T(es, "mhalf", [128, 16], F32)

        S.dma('sp', identF[:], identf_in, own=R('identF'), w=[R('identF')])
        S.dma('pool', identB[:], identf_in, own=R('identB'), w=[R('identB')])
        S.dma('pool', antiB[:], antii_in, own=R('antiB'), w=[R('antiB')])

        with ExitStack() as ph:
            rowsA = T(ph, "rowsA", [52, 128], F32)
            rows64 = T(ph, "rows64", [8, 64], F32)
            S.dma('sp', rowsA[0:8, :], c_in.rearrange("(r d) -> r d", d=128), own=R('rowsA'), w=[R('rowsA')])
            S.dma('sp', rowsA[8:16, :], cctx_in.rearrange("(r d) -> r d", d=128), own=R('rowsA'), w=[R('rowsA')])
            for l in range(2):
                b0 = 16 + l * 18
                S.dma('sp', rowsA[b0:b0 + 8, :], g_mix[l].rearrange("(r d) -> r d", d=128), own=R('rowsA'), w=[R('rowsA')])
                S.dma('sp', rowsA[b0 + 8:b0 + 16, :], g_ffn[l].rearrange("(r d) -> r d", d=128), own=R('rowsA'), w=[R('rowsA')])
                S.dma('sp', rowsA[b0 + 16:b0 + 18, :], pool_scale[l].rearrange("(r d) -> r d", d=128), own=R('rowsA'), w=[R('rowsA')])
                S.dma('sp', rows64[l * 4:l * 4 + 4, :], pool_scale[l].rearrange("(r d) -> r d", d=64), own=R('rows64'), w=[R('rows64')])
            S.op('pe', lambda e: e.transpose(ps[0][:, 0:52], rowsA[0:52, :], identF[0:52, 0:52]),
                 r=[R('rowsA'), R('identF')], w=[PR(0)])
            S.op('dve', lambda e: e.tensor_copy(colsP[:, 0:52], ps[0][:, 0:52]), r=[PR(0)], w=[R('colsP')])
            S.op('pe', lambda e: e.transpose(ps[1][0:64, 0:8], rows64[0:8, :], identF[0:8, 0:8]),
                 r=[R('rows64'), R('identF')], w=[PR(1)])
            S.op('dve', lambda e: e.tensor_copy(cols64[:, :], ps[1][0:64, 0:8]), r=[PR(1)], w=[R('cols64')])
            S.op('dve', lambda e: e.memset(epsT[:], EPS), w=[R('epsT')])
            S.op('pool', lambda e: e.memset(mhalf[:], -0.5), w=[R('mhalf')])
            S.op('act', lambda e: e.activation(mk(sT, 0, 128, 0, [[1, 2], [2, 8]]),
                                               colsP[:, 0:16].rearrange("p (a k) -> p a k", a=2), AF.Silu),
                 r=[R('colsP')], w=[R('sT')])

            wm = [T(ph, "wm%d" % i, [128, 8, 512], F32) for i in range(2)]
            bm = T(ph, "bm", [2, 6 * D], F32)
            modsb = T(ph, "modsb", [2, 6 * D], F32)
            raw = T(ph, "raw", [128, 48, 2], F32)
            dl = T(ph, "dl", [128, 128], F32)
            pr = T(ph, "pr", [128, 2, 32], F32)
            sm = T(ph, "sm", [128, 2], F32)
            for l in range(2):
                for kk in range(2):
                    S.dma('sp', bm[kk:kk + 1, :], b_mod[l:l + 1, :], own=R('bm'), w=[R('bm')])
                for n in range(12):
                    wb = wm[n % 2]
                    S.dma('sp', wb[:], w_mod[l][:, n * 512:(n + 1) * 512].rearrange("(k p) n -> p k n", p=128),
                          own=R('wm', n % 2), w=[R('wm', n % 2)])
                    pb = 2 + (n % 2)
                    for k in range(8):
                        S.op('pe', lambda e: e.matmul(ps[pb][0:2, :], lhsT=sT[:, k, :], rhs=wb[:, k, :],
                                                      start=(k == 0), stop=(k == 7)),
                             r=[R('sT'), R('wm', n % 2)], w=[PR(pb)])
                    S.op('dve', lambda e: e.tensor_tensor(modsb[0:2, n * 512:(n + 1) * 512], ps[pb][0:2, :],
                                                          bm[0:2, n * 512:(n + 1) * 512], ALU.add),
                         r=[PR(pb), R('bm')], w=[R('modsb')])
                S.dma('sp', modd[l], modsb[:], own=R('modsb'), r=[R('modsb')], w=[R('modd', l)])
                for j in range(48):
                    S.op('pe', lambda e: e.transpose(ps[4][:, j * 2:(j + 1) * 2], modsb[0:2, j * 128:(j + 1) * 128],
                                                     identF[0:2, 0:2]),
                         r=[R('modsb'), R('identF')], w=[PR(4)])
                S.op('dve', lambda e: e.tensor_copy(raw[:].rearrange("p j k -> p (j k)"), ps[4][:, 0:96]),
                     r=[PR(4)], w=[R('raw')])
                b0 = 16 + l * 18
                for kd in range(2):
                    S.op('dve', lambda e: e.scalar_tensor_tensor(modP[:, l, kd, 0, :], raw[:, 8:16, kd], 1.0,
                                                                 colsP[:, b0:b0 + 8], ALU.add, ALU.mult),
                         r=[R('raw'), R('colsP')], w=[R('modP')])
                    S.op('dve', lambda e: e.tensor_copy(modP[:, l, kd, 1, :], raw[:, 0:8, kd]), r=[R('raw')], w=[R('modP')])
                    S.op('dve', lambda e: e.scalar_tensor_tensor(modP[:, l, kd, 2, :], raw[:, 32:40, kd], 1.0,
                                                                 colsP[:, b0 + 8:b0 + 16], ALU.add, ALU.mult),
                         r=[R('raw'), R('colsP')], w=[R('modP')])
                    S.op('dve', lambda e: e.tensor_copy(modP[:, l, kd, 3, :], raw[:, 24:32, kd]), r=[R('raw')], w=[R('modP')])
                lam_init = 0.8 - 0.6 * math.exp(-0.3 * l)
                S.dma('sp', dl[:], bass.AP(diff_lambda.tensor, diff_lambda[l].offset, [[0, 128], [1, 128]]),
                      own=R('dl'), w=[R('dl')])
                S.op('dve', lambda e: e.tensor_tensor(pr[:], mk(dl, 0, 128, 0, [[64, 2], [1, 32]]),
                                                      mk(dl, 0, 128, 32, [[64, 2], [1, 32]]), ALU.mult),
                     r=[R('dl')], w=[R('pr')])
                S.op('dve', lambda e: e.reduce_sum(sm[:], pr[:], axis=AX.X), r=[R('pr')], w=[R('sm')])
                S.op('act', lambda e: e.activation(sm[:], sm[:], AF.Exp), r=[R('sm')], w=[R('sm')])
                S.op('dve', lambda e: e.tensor_tensor(nlam[:, l:l + 1], sm[:, 1:2], sm[:, 0:1], ALU.subtract),
                     r=[R('sm')], w=[R('nlam')])
                S.op('dve', lambda e: e.tensor_scalar(nlam[:, l:l + 1], nlam[:, l:l + 1], -lam_init, None, ALU.add),
                     r=[R('nlam')], w=[R('nlam')])
                S.dma('sp', gainG[:, l, 0:4, :], bass.AP(gqa_q_norm.tensor, gqa_q_norm[l].offset, [[0, 128], [0, 4], [1, 64]]),
                      own=R('gainG'), w=[R('gainG')])
                S.dma('sp', gainG[:, l, 4:8, :], bass.AP(gqa_k_norm.tensor, gqa_k_norm[l].offset, [[0, 128], [0, 4], [1, 64]]),
                      own=R('gainG'), w=[R('gainG')])
                S.dma('sp', gB[:, l, :], bass.AP(diff_norm_g.tensor, diff_norm_g[l].offset, [[0, 128], [1, 64]]),
                      own=R('gB'), w=[R('gB')])
                S.op('dve', lambda e: e.tensor_scalar(gB[:, l, :], gB[:, l, :], 1.0 - lam_init, None, ALU.mult),
                     r=[R('gB')], w=[R('gB')])
            S.phase_end()

        def norm_mod_T(l, which, tiles, xt, junk, diag, ssb, outB, out32, pbanks, srcap, srcres):
            norm_p1(tiles, xt, junk, diag, ssb, srcap, srcres)
            norm_p2(l, which, tiles, xt, diag, outB, out32, pbanks)

        def norm_p1(tiles, xt, junk, diag, ssb, srcap, srcres):
            n = len(tiles)
            S.dma('sp', xt[:, 0:n, :], srcap, own=R(xt.name), r=srcres, w=[R(xt.name)])
            for t in range(n):
                S.op('act', lambda e: e.activation(junk[:], xt[:, t, :], AF.Square, accum_out=ssb[:, t:t + 1]),
                     r=[R(xt.name)], w=[R(junk.name), R(ssb.name, t)])
            S.op('dve', lambda e: e.tensor_scalar(ssb[:, 4:4 + n], ssb[:, 0:n], 1.0 / D, EPS, ALU.mult, ALU.add),
                 r=[R(ssb.name, t) for t in range(n)], w=[R(ssb.name, 'm')])
            S.op('pool', lambda e: e.tensor_tensor(ssb[:, 8:8 + n], ssb[:, 4:4 + n], mhalf[:, 0:n], ALU.pow),
                 r=[R(ssb.name, 'm'), R('mhalf')], w=[R(ssb.name, 'r')])
            for t in range(n):
                S.op('pool' if t % 2 else 'dve',
                     lambda e: e.tensor_scalar(diag[:, t, :], identF[:], ssb[:, 8 + t:9 + t], None, ALU.mult),
                     r=[R(ssb.name, 'r'), R('identF')], w=[R(diag.name, t)])

        def norm_p2(l, which, tiles, xt, diag, outB, out32, pbanks):
            n = len(tiles)
            ntok = n * 128
            kd = kind_of(tiles[0])
            a_i = 0 if which == 1 else 2
            for c in range(8):
                pb = pbanks[c % len(pbanks)]
                for t in range(n):
                    S.op('pe', lambda e: e.matmul(ps[pb][:, t * 128:(t + 1) * 128], lhsT=xt[:, t, c * 128:(c + 1) * 128],
                                                  rhs=diag[:, t, :], start=True, stop=True),
                         r=[R(xt.name), R(diag.name, t)], w=[PR(pb)])
                A = modP[:, l, kd, a_i, c:c + 1]
                B = modP[:, l, kd, a_i + 1, c:c + 1]
                if outB is None:
                    o32, o32res = out32(c)
                    S.op('act' if c % 2 else 'dve',
                         (lambda e: e.activation(o32, ps[pb][:, 0:ntok], AF.Identity, bias=B, scale=A)) if c % 2 else
                         (lambda e: e.tensor_scalar(o32, ps[pb][:, 0:ntok], A, B, ALU.mult, ALU.add)),
                         r=[PR(pb), R('modP')], w=o32res)
                    continue
                ob, obres = outB(c)
                if out32 is None:
                    if c % 2 == 0:
                        S.op('dve', lambda e: e.tensor_scalar(ob, ps[pb][:, 0:ntok], A, B, ALU.mult, ALU.add),
                             r=[PR(pb), R('modP')], w=obres)
                    else:
                        S.op('act', lambda e: e.activation(ob, ps[pb][:, 0:ntok], AF.Identity, bias=B, scale=A),
                             r=[PR(pb), R('modP')], w=obres)
                else:
                    o32, o32res = out32(c)
                    S.op('dve', lambda e: e.tensor_scalar(ob, ps[pb][:, 0:ntok], A, B, ALU.mult, ALU.add),
                         r=[PR(pb), R('modP')], w=obres)
                    S.op('act', lambda e: e.activation(o32, ps[pb][:, 0:ntok], AF.Identity, bias=B, scale=A),
                         r=[PR(pb), R('modP')], w=o32res)

        def hT_view(tok0, ntok):
            return hT_d[:, :, tok0:tok0 + ntok].rearrange("c p t -> p c t")


        S.keep.add(R('wbfsem'))
        S.keep_res.add('wbf')

        def issue_casts(l):
            for e_ in range(32):
                rows = wbf_d[l][e_ * 128:(e_ + 1) * 128, :]
                S.dma('pool', rows[:, 0:4096].rearrange("p (k n) -> p k n", k=8),
                      w_eg[l, e_].rearrange("(k p) n -> p k n", p=128), own=R('wbfsem'), chain=False, w=[R('wbf', l, e_, 0)])
                S.dma('pool', rows[:, 4096:8192].rearrange("p (k n) -> p k n", k=8),
                      w_eu[l, e_].rearrange("(k p) n -> p k n", p=128), own=R('wbfsem'), chain=False, w=[R('wbf', l, e_, 1)])
                S.dma('pool', rows[:, 8192:12288].rearrange("p (k n) -> p k n", k=4),
                      w_ed[l, e_].rearrange("(k p) n -> p k n", p=128), own=R('wbfsem'), chain=False, w=[R('wbf', l, e_, 2)])

        def moe_sparse(l, need_ctx):
            last = (l == DEPTH - 1)
            moe_tiles = list(range(34)) if need_ctx else list(range(32))
            NTt = len(moe_tiles)
            NS = 100 if need_ctx else 96
            wbf_res = [R('wbf', l, e_, m) for e_ in range(32) for m in range(3)]
            with ExitStack() as ph:
                OHs = T(ph, "OHs", [128, NTt, 2, 32], F32)
                W12 = T(ph, "W12", [128, NTt, 2], F32)
                Pall = T(ph, "Pall", [128, NTt, 32], F32)
                posF = T(ph, "posF", [128, NTt, 2], F32)
                posI = T(ph, "posI", [128, NTt, 2], I32)
                idxW = T(ph, "idxW", [128, NS], I32)
                gt2 = T(ph, "gt2", [128, 2, D], F32)
                for kd in range(2):
                    S.dma('sp', gt2[:, kd, :], bass.AP(modd.tensor, modd[l, kd, 5 * D:6 * D].offset, [[0, 128], [1, D]]),
                          own=R('gt2'), r=[R('modd', l)], w=[R('gt2')])
                with ExitStack() as pa:
                    xt = T(pa, "xtm", [128, 4, D], F32)
                    junk = T(pa, "junkm", [128, D], BF16)
                    diag = T(pa, "diagm", [128, 4, 128], F32)
                    ssb = T(pa, "ssbm", [128, 12], F32)
                    tT32 = T(pa, "tT32", [128, 8, 512], F32)
                    wr32 = T(pa, "wr32", [128, 8, 36], F32)
                    brt = T(pa, "brt", [128, 36], F32)
                    A2b = T(pa, "A2b", [128, 2, D], F32)
                    B2b = T(pa, "B2b", [128, 2, D], F32)
                    ttok = T(pa, "ttok", [128, NTt, D], BF16)
                    tmpf = [T(pa, "tmpf%d" % i, [128, D], F32) for i in range(2)]
                    lg = [T(pa, "lg%d" % i, [128, 36], F32) for i in range(2)]
                    rt = [T(pa, "rt%d" % i, [128, 128], F32) for i in range(2)]
                    Cb = [T(pa, "Cb%d" % i, [128, 32], BF16) for i in range(2)]
                    Ccum = [T(pa, "Ccum%d" % i, [128, 32], BF16) for i in range(2)]
                    ltri = T(pa, "ltri", [128, 128], BF16)
                    onesB = T(pa, "onesB", [128, 128], BF16)
                    ustr = T(pa, "ustr", [32, 32], F32)
                    thr = T(pa, "thr", [128, 100], F32)
                    iota = T(pa, "iota", [128, 1], F32)
                    zt = T(pa, "zt", [128, D], BF16)
                    ncol = T(pa, "ncol", [32, 4], F32)
                    cmp32 = T(pa, "cmp32", [32, 100], F32)
                    npbc = T(pa, "npbc", [32, 128], F32)
                    offs = T(pa, "offs", [128, 32], F32)
                    cmpw = T(pa, "cmpw", [128, NS, 32], F32)
                    cntw = T(pa, "cntw", [128, NS], F32)
                    prod = T(pa, "prod", [128, NTt, 2, 32], F32)
                    onesF = T(pa, "onesF", [32, 1], F32)
                    totc = T(pa, "totc", [128, 1], F32)
                    unused = T(pa, "unused", [128, NS], F32)
                    S.op('dve', lambda e: e.memset(onesF[:], 1.0), w=[R('onesF')])
                    S.dma('sp', wr32[:, :, 0:4], w_rg[l].rearrange("(k p) n -> p k n", p=128), own=R('wr32'), w=[R('wr32')])
                    S.dma('sp', wr32[:, :, 4:36], w_re[l].rearrange("(k p) n -> p k n", p=128), own=R('wr32'), w=[R('wr32')])
                    S.dma('sp', brt[:, 0:4], bass.AP(b_rg.tensor, b_rg[l].offset, [[0, 128], [1, 4]]), own=R('brt'), w=[R('brt')])
                    S.dma('sp', brt[:, 4:36], bass.AP(b_re.tensor, b_re[l].offset, [[0, 128], [1, 32]]), own=R('brt'), w=[R('brt')])
                    S.dma('pool', ltri[:], ltri_in, own=R('ltri'), w=[R('ltri')])
                    S.dma('sp', ustr[:], ustrict_in, own=R('ustr'), w=[R('ustr')])
                    S.dma('sp', thr[:], thr_in, own=R('thr'), w=[R('thr')])
                    S.dma('sp', iota[:], iota_in, own=R('iota'), w=[R('iota')])
                    S.op('pool', lambda e: e.memset(onesB[:], 1.0), w=[R('onesB')])
                    S.op('pool', lambda e: e.memset(zt[:], 0.0), w=[R('zt')])
                    S.op('pool', lambda e: e.memset(Ccum[0][:], 0.0), w=[R(Ccum[0].name)])
                    S.dma('sp', xslot_d[0:NS * 128, :].rearrange("(i p) d -> p i d", p=128),
                          mk(zt, 0, 128, 0, [[0, NS], [1, D]]), own=R('zt'), r=[R('zt')], w=[R('xslot0')])
                    for kd in range(2):
                        S.dma('sp', A2b[:, kd, :], bass.AP(modd.tensor, modd[l, kd, 4 * D:5 * D].offset, [[0, 128], [1, D]]),
                              own=R('A2b'), r=[R('modd', l)], w=[R('A2b')])
                        S.dma('sp', B2b[:, kd, :], bass.AP(modd.tensor, modd[l, kd, 3 * D:4 * D].offset, [[0, 128], [1, D]]),
                              own=R('B2b'), r=[R('modd', l)], w=[R('B2b')])
                    S.dma('sp', tmpf[0][:], bass.AP(g_ffn.tensor, g_ffn[l].offset, [[0, 128], [1, D]]), own=R(tmpf[0].name), w=[R(tmpf[0].name)])
                    for kd in range(2):
                        S.op('dve', lambda e: e.scalar_tensor_tensor(A2b[:, kd, :], A2b[:, kd, :], 1.0, tmpf[0][:], ALU.add, ALU.mult),
                             r=[R('A2b'), R(tmpf[0].name)], w=[R('A2b')])
                    chunks = []
                    cur = []
                    for t in moe_tiles:
                        if cur and (len(cur) == 4 or kind_of(cur[0]) != kind_of(t)):
                            chunks.append(cur)
                            cur = []
                        cur.append(t)
                    chunks.append(cur)
                    for ch in chunks:
                        n = len(ch)
                        ntok = n * 128
                        kd = kind_of(ch[0])
                        norm_mod_T(l, 2, ch, xt, junk, diag, ssb, None,
                                   lambda c: (tT32[:, c, 0:ntok], [R('tT32', c)]),
                                   [0, 1, 2, 3], x_src(l, ch, True), x_res(l, ch, True))
                        for ti, tile in enumerate(ch):
                            lt = tile
                            b = lt % 2
                            S.op('dve', lambda e: e.scalar_tensor_tensor(tmpf[b][:], xt[:, ti, :], ssb[:, 8 + ti:9 + ti], A2b[:, kd, :], ALU.mult, ALU.mult),
                                 r=[R(xt.name), R(ssb.name, 'r'), R('A2b')], w=[R(tmpf[b].name)])
                            S.op('pool', lambda e: e.tensor_tensor(ttok[:, lt, :], tmpf[b][:], B2b[:, kd, :], ALU.add),
                                 r=[R(tmpf[b].name), R('B2b')], w=[R('ttok', lt)])
                            L, Rt = lg[b], rt[b]
                            for k in range(8):
                                S.op('pe', lambda e: e.matmul(ps[4 + b][:, 0:36], lhsT=tT32[:, k, ti * 128:(ti + 1) * 128], rhs=wr32[:, k, :],
                                                              start=(k == 0), stop=(k == 7)),
                                     r=[R('tT32', k), R('wr32')], w=[PR(4 + b)])
                            S.op('dve', lambda e: e.tensor_tensor(L[:], ps[4 + b][:, 0:36], brt[:], ALU.add), r=[PR(4 + b), R('brt')], w=[R(L.name)])
                            oh1 = OHs[:, lt, 0, :]
                            oh2 = OHs[:, lt, 1, :]
                            S.op('dve', lambda e: e.reduce_max(Rt[:, 0:1], L[:, 0:4], axis=AX.X), r=[R(L.name)], w=[R(Rt.name, 0)])
                            S.op('dve', lambda e: e.tensor_scalar(Rt[:, 1:2], Rt[:, 0:1], -1.0, None, ALU.mult), r=[R(Rt.name, 0)], w=[R(Rt.name, 1)])
                            S.op('act', lambda e: e.activation(Rt[:, 12:16], L[:, 0:4], AF.Exp, bias=Rt[:, 1:2], scale=1.0, accum_out=Rt[:, 2:3]),
                                 r=[R(L.name), R(Rt.name, 1)], w=[R(Rt.name, 2)])
                            S.op('dve', lambda e: e.reciprocal(Rt[:, 3:4], Rt[:, 2:3]), r=[R(Rt.name, 2)], w=[R(Rt.name, 3)])
                            S.op('dve', lambda e: e.tensor_scalar(Rt[:, 4:8], L[:, 0:4], Rt[:, 0:1], None, ALU.is_equal),
                                 r=[R(L.name), R(Rt.name, 0)], w=[R(Rt.name, 4)])
                            S.op('dve', lambda e: e.tensor_scalar(Rt[:, 8:12], Rt[:, 4:8], -1.0, 1e30, ALU.add, ALU.mult),
                                 r=[R(Rt.name, 4)], w=[R(Rt.name, 5)])
                            S.op('dve', lambda e: e.tensor_tensor(Rt[:, 16:48].rearrange("p (g k) -> p g k", g=4),
                                                                  L[:, 4:36].rearrange("p (g k) -> p g k", g=4),
                                                                  mk(Rt, 0, 128, 8, [[1, 4], [0, 8]]), ALU.add),
                                 r=[R(L.name), R(Rt.name, 5)], w=[R(Rt.name, 6)])
                            S.op('dve', lambda e: e.reduce_max(Rt[:, 48:49], Rt[:, 16:48], axis=AX.X), r=[R(Rt.name, 6)], w=[R(Rt.name, 7)])
                            S.op('dve', lambda e: e.tensor_scalar(oh1, Rt[:, 16:48], Rt[:, 48:49], None, ALU.is_equal),
                                 r=[R(Rt.name, 6), R(Rt.name, 7)], w=[R('oh', lt, 0)])
                            S.op('dve', lambda e: e.scalar_tensor_tensor(Rt[:, 88:120], oh1, -1e30, Rt[:, 16:48], ALU.mult, ALU.add),
                                 r=[R('oh', lt, 0), R(Rt.name, 6)], w=[R(Rt.name, 9)])
                            S.op('dve', lambda e: e.reduce_max(Rt[:, 49:50], Rt[:, 88:120], axis=AX.X), r=[R(Rt.name, 9)], w=[R(Rt.name, 10)])
                            S.op('dve', lambda e: e.tensor_scalar(oh2, Rt[:, 88:120], Rt[:, 49:50], None, ALU.is_equal),
                                 r=[R(Rt.name, 9), R(Rt.name, 10)], w=[R('oh', lt, 1)])
                            S.op('dve', lambda e: e.tensor_scalar(Rt[:, 50:51], Rt[:, 48:49], -1.0, None, ALU.mult), r=[R(Rt.name, 7)], w=[R(Rt.name, 12)])
                            S.op('act', lambda e: e.activation(Rt[:, 51:52], Rt[:, 49:50], AF.Exp, bias=Rt[:, 50:51], scale=1.0),
                                 r=[R(Rt.name, 10), R(Rt.name, 12)], w=[R(Rt.name, 13)])
                            S.op('dve', lambda e: e.tensor_scalar(Rt[:, 52:53], Rt[:, 51:52], 1.0, None, ALU.add), r=[R(Rt.name, 13)], w=[R(Rt.name, 14)])
                            S.op('dve', lambda e: e.reciprocal(Rt[:, 53:54], Rt[:, 52:53]), r=[R(Rt.name, 14)], w=[R(Rt.name, 15)])
                            S.op('dve', lambda e: e.tensor_tensor(W12[:, lt, 0:1], Rt[:, 53:54], Rt[:, 3:4], ALU.mult),
                                 r=[R(Rt.name, 15), R(Rt.name, 3)], w=[R('w12', lt, 0)])
                            S.op('dve', lambda e: e.tensor_tensor(W12[:, lt, 1:2], W12[:, lt, 0:1], Rt[:, 51:52], ALU.mult),
                                 r=[R('w12', lt, 0), R(Rt.name, 13)], w=[R('w12', lt, 1)])
                            cb = Cb[b]
                            cprev, cnext = Ccum[lt % 2], Ccum[(lt + 1) % 2]
                            S.op('dve', lambda e: e.tensor_tensor(cb[:], oh1, oh2, ALU.add), r=[R('oh', lt, 0), R('oh', lt, 1)], w=[R(cb.name)])
                            S.op('pe', lambda e: e.matmul(ps[6][:, 0:32], lhsT=ltri[:], rhs=cb[:], start=True, stop=False),
                                 r=[R('ltri'), R(cb.name)], w=[PR(6)])
                            S.op('pe', lambda e: e.matmul(ps[6][:, 0:32], lhsT=onesB[:], rhs=cprev[:], start=False, stop=True),
                                 r=[R('onesB'), R(cprev.name)], w=[PR(6)])
                            S.op('act', lambda e: e.activation(Pall[:, lt, :], ps[6][:, 0:32], AF.Copy), r=[PR(6)], w=[R('Pall', lt)])
                            S.op('pool', lambda e: e.tensor_tensor(cnext[:], cprev[:], cb[:], ALU.add),
                                 r=[R(cprev.name), R(cb.name)], w=[R(cnext.name)])
                    cfin = Ccum[NTt % 2]
                    S.op('pe', lambda e: e.matmul(ps[7][0:32, 0:1], lhsT=cfin[:], rhs=onesB[:, 0:1], start=True, stop=True),
                         r=[R(cfin.name), R('onesB')], w=[PR(7)])
                    S.op('dve', lambda e: e.tensor_copy(ncol[:, 0:1], ps[7][0:32, 0:1]), r=[PR(7)], w=[R('ncol', 0)])
                    S.op('dve', lambda e: e.tensor_scalar(cmp32[:], thr[0:32, :], ncol[:, 0:1], None, ALU.is_lt),
                         r=[R('thr'), R('ncol', 0)], w=[R('cmp32')])
                    S.op('dve', lambda e: e.reduce_sum(ncol[:, 1:2], cmp32[:], axis=AX.X), r=[R('cmp32')], w=[R('ncol', 1)])
                    S.op('dve', lambda e: e.tensor_scalar(ncol[:, 2:3], ncol[:, 1:2], 128.0, None, ALU.mult), r=[R('ncol', 1)], w=[R('ncol', 2)])
                    S.op('dve', lambda e: e.tensor_copy(npbc[:], mk(ncol, 0, 32, 2, [[0, 128]])), r=[R('ncol', 2)], w=[R('npbc')])
                    S.op('pe', lambda e: e.matmul(ps[7][:, 32:64], lhsT=npbc[:], rhs=ustr[:], start=True, stop=True),
                         r=[R('npbc'), R('ustr')], w=[PR(7)])
                    S.op('dve', lambda e: e.tensor_copy(offs[:], ps[7][:, 32:64]), r=[PR(7)], w=[R('offs')])
                    S.op('dve', lambda e: e.tensor_tensor(cmpw[:], mk(offs, 0, 128, 0, [[0, NS], [1, 32]]),
                                                          mk(thr, 0, 128, 0, [[1, NS], [0, 32]]), ALU.is_le),
                         r=[R('offs'), R('thr')], w=[R('cmpw')])
                    S.op('dve', lambda e: e.reduce_sum(cntw[:], cmpw[:], axis=AX.X), r=[R('cmpw')], w=[R('cntw')])
                    S.op('dve', lambda e: e.tensor_scalar(cntw[:], cntw[:], -1.0, 128.0, ALU.add, ALU.mult), r=[R('cntw')], w=[R('cntw')])
                    S.op('dve', lambda e: e.tensor_scalar(cntw[:], cntw[:], iota[:, 0:1], None, ALU.add), r=[R('cntw'), R('iota')], w=[R('cntw')])
                    S.op('dve', lambda e: e.tensor_copy(idxW[:], cntw[:]), r=[R('cntw')], w=[R('idxW')])
                    S.op('dve', lambda e: e.tensor_tensor(Pall[:], Pall[:], mk(offs, 0, 128, 0, [[0, NTt], [1, 32]]), ALU.add),
                         r=[R('Pall', t) for t in range(NTt)] + [R('offs')], w=[R('Pall2')])
                    S.op('dve', lambda e: e.tensor_tensor(prod[:], OHs[:], mk(Pall, 0, 128, 0, [[32, NTt], [0, 2], [1, 32]]), ALU.mult),
                         r=[R('Pall2')] + [R('oh', t, k) for t in range(NTt) for k in range(2)], w=[R('prod')])
                    S.op('dve', lambda e: e.reduce_sum(posF[:].rearrange("p t k -> p (t k)"), prod[:].rearrange("p t k e -> p (t k) e"), axis=AX.X),
                         r=[R('prod')], w=[R('posF')])
                    S.op('dve', lambda e: e.tensor_copy(posI[:], posF[:]), r=[R('posF')], w=[R('posI')])
                    for lt in range(NTt):
                        for k in range(2):
                            S.dma('pool', xslot_d[:, :], ttok[:, lt, :], own=R('scat', (lt * 2 + k) % 4),
                                  r=[R('ttok', lt), R('posI'), R('xslot0')], w=[R('xslot', lt, k)],
                                  indirect=('scatter', posI[:, lt, k:k + 1]))
                    S.barrier()
                xslot_res = [R('xslot', lt, k) for lt in range(NTt) for k in range(2)]
                with ExitStack() as pb_:
                    wt = [T(pb_, "wt%d" % i, [128, 12288], BF16) for i in range(2)]
                    xs = [T(pb_, "xs%d" % i, [128, D], BF16) for i in range(2)]
                    xT = [T(pb_, "xT%d" % i, [128, 8, 128], BF16) for i in range(2)]
                    sgb = [T(pb_, "sgb%d" % i, [128, 512], BF16) for i in range(2)]
                    aT = [T(pb_, "aTs%d" % i, [128, 4, 128], BF16) for i in range(2)]
                    ysb = [T(pb_, "ysb%d" % i, [128, D], F32) for i in range(2)]

                    def fetch(i):
                        S.dma('pool', wt[i % 2][:, :], wbf_d[l][:, :], own=R('wt', i % 2),
                              r=[R('idxW')] + wbf_res, w=[R('wt', i % 2)], indirect=('gather', idxW[:, i:i + 1]))
                        S.dma('sp', xs[i % 2][:], xslot_d[i * 128:(i + 1) * 128, :], own=R('xs', i % 2), r=xslot_res, w=[R('xs', i % 2)])

                    fetch(0)
                    for i in range(NS):
                        if i + 1 < NS:
                            fetch(i + 1)
                        w_, x_, xT_, sg_, a_, y_ = wt[i % 2], xs[i % 2], xT[i % 2], sgb[i % 2], aT[i % 2], ysb[i % 2]
                        tb = i % 2
                        for k in range(8):
                            S.op('pe', lambda e: e.transpose(psb[tb][:, k * 128:(k + 1) * 128], x_[:, k * 128:(k + 1) * 128], identB[:]),
                                 r=[R('xs', i % 2), R('identB')], w=[PR(tb)])
                        if i % 2:
                            S.op('dve', lambda e: e.tensor_copy(xT_[:].rearrange("p k s -> p (k s)"), psb[tb][:, 0:1024]), r=[PR(tb)], w=[R(xT_.name)])
                        else:
                            S.op('act', lambda e: e.activation(xT_[:].rearrange("p k s -> p (k s)"), psb[tb][:, 0:1024], AF.Copy), r=[PR(tb)], w=[R(xT_.name)])
                        G = 2 + (i % 2)
                        U = 4 + (i % 2)
                        for fc in range(4):
                            for k in range(8):
                                S.op('pe', lambda e: e.matmul(ps[G][:, fc * 128:(fc + 1) * 128], lhsT=w_[:, k * 512 + fc * 128:k * 512 + (fc + 1) * 128],
                                                              rhs=xT_[:, k, :], start=(k == 0), stop=(k == 7)),
                                     r=[R('wt', i % 2), R(xT_.name)], w=[PR(G)])
                        for fc in range(4):
                            for k in range(8):
                                S.op('pe', lambda e: e.matmul(ps[U][:, fc * 128:(fc + 1) * 128],
                                                              lhsT=w_[:, 4096 + k * 512 + fc * 128:4096 + k * 512 + (fc + 1) * 128],
                                                              rhs=xT_[:, k, :], start=(k == 0), stop=(k == 7)),
                                     r=[R('wt', i % 2), R(xT_.name)], w=[PR(U)])
                        S.op('act', lambda e: e.activation(sg_[:], ps[G][:, :], AF.Silu), r=[PR(G)], w=[R(sg_.name)])
                        S.op('dve', lambda e: e.tensor_tensor(a_[:].rearrange("p f s -> p (f s)"), sg_[:], ps[U][:, :], ALU.mult),
                             r=[R(sg_.name), PR(U)], w=[R(a_.name)])
                        for hf in range(2):
                            Y = 6 + hf
                            for fc in range(4):
                                S.op('pe', lambda e: e.matmul(ps[Y][:, :], lhsT=a_[:, fc, :],
                                                              rhs=w_[:, 8192 + fc * 1024 + hf * 512:8192 + fc * 1024 + (hf + 1) * 512],
                                                              start=(fc == 0), stop=(fc == 3)),
                                     r=[R(a_.name), R('wt', i % 2)], w=[PR(Y)])
                            if hf:
                                S.op('dve', lambda e: e.tensor_copy(y_[:, hf * 512:(hf + 1) * 512], ps[Y][:, :]), r=[PR(Y)], w=[R(y_.name, hf)])
                            else:
                                S.op('act', lambda e: e.activation(y_[:, hf * 512:(hf + 1) * 512], ps[Y][:, :], AF.Copy), r=[PR(Y)], w=[R(y_.name, hf)])
                        S.dma('sp', yslot_d[i * 128:(i + 1) * 128, :], y_[:], own=R(y_.name, 0),
                              r=[R(y_.name, 0), R(y_.name, 1)], w=[R('yslot', i)])
                    S.barrier()
                yslot_res = [R('yslot', i) for i in range(NS)]
                with ExitStack() as pc_:
                    g1 = [T(pc_, "g1%d" % i, [128, D], F32) for i in range(2)]
                    g2 = [T(pc_, "g2%d" % i, [128, D], F32) for i in range(2)]
                    xq = [T(pc_, "xq%d" % i, [128, D], F32) for i in range(2)]
                    junk = T(pc_, "junkc", [128, D], BF16)
                    gfin = T(pc_, "gfin", [128, D], F32)
                    ssc = [T(pc_, "ssc%d" % i, [128, 4], F32) for i in range(2)]
                    S.dma('sp', gfin[:], bass.AP(g_final.tensor, g_final.offset, [[0, 128], [1, D]]), own=R('gfin'), w=[R('gfin')])
                    for lt in range(NTt):
                        tile = moe_tiles[lt]
                        b = lt % 2
                        kd = kind_of(tile)
                        a1, a2, xx, sc_ = g1[b], g2[b], xq[b], ssc[b]
                        S.dma('pool', a1[:, :], yslot_d[:, :], own=R(a1.name), r=[R('posI')] + yslot_res, w=[R(a1.name)],
                              indirect=('gather', posI[:, lt, 0:1]))
                        S.dma('pool', a2[:, :], yslot_d[:, :], own=R(a2.name), r=[R('posI')] + yslot_res, w=[R(a2.name)],
                              indirect=('gather', posI[:, lt, 1:2]))
                        S.dma('sp', xx[:], x_src(l, [tile], True)[:, 0, :], own=R(xx.name), r=x_res(l, [tile], True), w=[R(xx.name)])
                        S.op('dve', lambda e: e.tensor_scalar(a1[:], a1[:], W12[:, lt, 0:1], None, ALU.mult),
                             r=[R(a1.name), R('w12', lt, 0)], w=[R(a1.name)])
                        S.op('dve', lambda e: e.scalar_tensor_tensor(a1[:], a2[:], W12[:, lt, 1:2], a1[:], ALU.mult, ALU.add),
                             r=[R(a1.name), R(a2.name), R('w12', lt, 1)], w=[R(a1.name)])
                        S.op('dve', lambda e: e.tensor_tensor(a1[:], a1[:], gt2[:, kd, :], ALU.mult), r=[R(a1.name), R('gt2')], w=[R(a1.name)])
                        S.op('dve', lambda e: e.tensor_tensor(xx[:], xx[:], a1[:], ALU.add), r=[R(a1.name), R(xx.name)], w=[R(xx.name)])
                        if not last:
                            S.dma('sp', xs_d[tile * 128:(tile + 1) * 128, :], xx[:], own=R(xx.name), r=[R(xx.name)], w=[R('xs', tile)])
                        else:
                            S.op('act', lambda e: e.activation(junk[:], xx[:], AF.Square, accum_out=sc_[:, 0:1]),
                                 r=[R(xx.name)], w=[R(junk.name), R(sc_.name, 0)])
                            S.op('dve', lambda e: e.tensor_scalar(sc_[:, 1:2], sc_[:, 0:1], 1.0 / D, EPS, ALU.mult, ALU.add),
                                 r=[R(sc_.name, 0)], w=[R(sc_.name, 1)])
                            S.op('pool', lambda e: e.tensor_tensor(sc_[:, 2:3], sc_[:, 1:2], mhalf[:, 0:1], ALU.pow),
                                 r=[R(sc_.name, 1), R('mhalf')], w=[R(sc_.name, 2)])
                            S.op('dve', lambda e: e.scalar_tensor_tensor(xx[:], xx[:], sc_[:, 2:3], gfin[:], ALU.mult, ALU.mult),
                                 r=[R(xx.name), R(sc_.name, 2), R('gfin')], w=[R(xx.name)])
                            S.dma('sp', out_d[tile * 128:(tile + 1) * 128, :], xx[:], own=R(xx.name), r=[R(xx.name)], w=[R('out', tile)])
                S.phase_end()

        for l in range(DEPTH):
            need_ctx = l < DEPTH - 1
            lay = ExitStack()
            es.callback(lay.close)
            biasM = T(lay, "biasM", [128, 21, 4, 128], BF16)
            with ExitStack() as ph:
                xts = [T(ph, "xt%d" % i, [128, 4, D], F32) for i in range(2)]
                junk = T(ph, "junk", [128, D], BF16)
                diags = [T(ph, "diag%d" % i, [128, 4, 128], F32) for i in range(2)]
                ssbs = [T(ph, "ssb%d" % i, [128, 12], F32) for i in range(2)]
                hTs = [T(ph, "hTs%d" % i, [128, 8, 512], BF16) for i in range(2)]
                norm_p1(GROUPS[0], xts[0], junk, diags[0], ssbs[0], x_src(l, GROUPS[0]), x_res(l, GROUPS[0]))
                for gi, tiles in enumerate(GROUPS):
                    n = len(tiles)
                    ntok = n * 128
                    hb = hTs[gi % 2]
                    if gi + 1 < len(GROUPS):
                        nt_ = GROUPS[gi + 1]
                        norm_p1(nt_, xts[(gi + 1) % 2], junk, diags[(gi + 1) % 2], ssbs[(gi + 1) % 2], x_src(l, nt_), x_res(l, nt_))
                    norm_p2(l, 1, tiles, xts[gi % 2], diags[gi % 2],
                            lambda c: (hb[:, c, 0:ntok], [R(hb.name, c)]), None,
                            [0, 1, 2, 3] if gi % 2 == 0 else [4, 5, 6, 7])
                    S.dma('sp', hT_view(tiles[0] * 128, ntok), hb[:, :, 0:ntok], own=R(hb.name, 0),
                          r=[R(hb.name, c) for c in range(8)], w=[R('hT', t) for t in tiles])
                S.phase_end()
            if stop == ('p1', l):
                break

            def load_hT(hb, tiles):
                ntok = len(tiles) * 128
                S.dma('sp', hb[:, :, 0:ntok], hT_view(tiles[0] * 128, ntok), own=R(hb.name),
                      r=[R('hT', t) for t in tiles], w=[R(hb.name)])

            def store_br(i, brs, tiles):
                ntok = len(tiles) * 128
                t0 = tiles[0] * 128
                S.dma('sp', brT_d[i, :, :, t0:t0 + ntok].rearrange("g p t -> p g t"), brs[0:64, :, 0:ntok],
                      own=R(brs.name), r=[R(brs.name)], w=[R('br', i, t) for t in tiles])

            def wload(dst, res, cols0, cols1):
                S.dma('pool', dst[:], w_in[l][:, cols0:cols1].rearrange("(k p) n -> p k n", p=128),
                      own=R(res), w=[R(res)])

            with ExitStack() as ph:
                wu = T(ph, "wu", [128, 8, 256], BF16)
                pw = T(ph, "pw", [64, 4, 64], BF16)
                band = T(ph, "band", [128, 20, 128], BF16)
                u_sb = T(ph, "u_sb", [128, NT, 256], BF16)
                hTs = [T(ph, "hTp%d" % i, [128, 8, 512], BF16) for i in range(2)]
                brs2 = [T(ph, "brs%d" % i, [64, 4, 512], BF16) for i in range(2)]
                pooled = [T(ph, "pooled%d" % i, [64, 512], BF16) for i in range(2)]
                wload(wu, 'wu', 0, 256)
                S.dma('pool', pw[:], pool_w[l].rearrange("g c d -> c g d"), own=R('pw'), w=[R('pw')])
                S.dma('pool', band[:], band_in, own=R('band'), w=[R('band')])
                if DBG_BARRIER:
                    S.barrier(DBG_BARRIER)
                groups = GROUPS if need_ctx else GROUPS[:8]
                load_hT(hTs[0], groups[0])
                for gi, tiles in enumerate(groups):
                    hb = hTs[gi % 2]
                    if gi + 1 < len(groups):
                        load_hT(hTs[(gi + 1) % 2], groups[gi + 1])
                    for ti, tile in enumerate(tiles):
                        pb = (gi * 4 + ti) % 4
                        for k in range(8):
                            S.op('pe', lambda e: e.matmul(ps[pb][:, 0:256], lhsT=hb[:, k, ti * 128:(ti + 1) * 128],
                                                          rhs=wu[:, k, :], start=(k == 0), stop=(k == 7)),
                                 r=[R(hb.name), R('wu')], w=[PR(pb)])
                        if ti % 2 == 0:
                            S.op('dve', lambda e: e.tensor_copy(u_sb[:, tile, :], ps[pb][:, 0:256]), r=[PR(pb)], w=[R('u', tile)])
                        else:
                            S.op('act', lambda e: e.activation(u_sb[:, tile, :], ps[pb][:, 0:256], AF.Copy), r=[PR(pb)], w=[R('u', tile)])
                cnt = 0
                for gi, tiles in enumerate(groups):
                    first, last = (0, 31) if tiles[0] < 32 else (32, 33)
                    ntok = len(tiles) * 128
                    brs = brs2[gi % 2]
                    for g in range(4):
                        pa = 4 + (cnt % 2)
                        pbk = 6 + (cnt % 2)
                        pl = pooled[cnt % 2]
                        cnt += 1
                        for jj, j in enumerate(tiles):
                            ins = []
                            if j > first:
                                ins.append((j - 1, 0))
                            ins.append((j, 3 if j == first else (4 if j == last else 1)))
                            if j < last:
                                ins.append((j + 1, 2))
                            for ii, (jin, var) in enumerate(ins):
                                S.op('pe', lambda e: e.matmul(ps[pa][0:64, jj * 128:(jj + 1) * 128],
                                                              lhsT=u_sb[:, jin, g * 64:(g + 1) * 64], rhs=band[:, g * 5 + var, :],
                                                              start=(ii == 0), stop=(ii == len(ins) - 1)),
                                     r=[R('u', jin), R('band')], w=[PR(pa)])
                        S.op('act', lambda e: e.activation(pl[0:64, 0:ntok], ps[pa][0:64, 0:ntok], AF.Copy), r=[PR(pa)], w=[R(pl.name)])
                        S.op('pe', lambda e: e.matmul(ps[pbk][0:64, 0:ntok], lhsT=pw[0:64, g, :], rhs=pl[0:64, 0:ntok],
                                                      start=True, stop=True),
                             r=[R('pw'), R(pl.name)], w=[PR(pbk)])
                        S.op('dve', lambda e: e.tensor_scalar(brs[0:64, g, 0:ntok], ps[pbk][0:64, 0:ntok],
                                                              cols64[0:64, l * 4 + g:l * 4 + g + 1], None, ALU.mult),
                             r=[PR(pbk), R('cols64')], w=[R(brs.name)])
                    store_br(0, brs, tiles)
                S.phase_end()
            if stop == ('pool', l):
                break

            def transpose_out(i, o_grp, tiles, brs, pbank):
                n = len(tiles)
                for t0 in range(0, n, 2):
                    nn = min(2, n - t0)
                    for tt in range(nn):
                        for h in range(4):
                            col = (tt * 4 + h) * 128
                            S.op('pe', lambda e: e.transpose(psb[pbank][0:64, col:col + 128],
                                                             o_grp[:, t0 + tt, h * 64:(h + 1) * 64], identB[:]),
                                 r=[R(o_grp.name), R('identB')], w=[PR(pbank)])
                    S.op('dve', lambda e: e.tensor_copy(
                        brs[0:64, :, t0 * 128:(t0 + nn) * 128].rearrange("p h (t q) -> p t h q", t=nn),
                        psb[pbank][0:64, 0:nn * 512].rearrange("p (t h q) -> p t h q", t=nn, h=4)),
                        r=[PR(pbank)], w=[R(brs.name)])
                store_br(i, brs, tiles)

            def dense_attention(i, pair_list, scale, ph, bg=None, bg_n=0):
                pTs = [T(ph, "pT%d" % k, [128, 512], BF16) for k in range(6)]
                o_grps = [T(ph, "ogrp%d" % k, [128, 4, 256], BF16) for k in range(2)]
                brs2 = [T(ph, "brsA%d" % k, [64, 4, 512], BF16) for k in range(2)]
                qgroups = GROUPS if need_ctx else GROUPS[:8]
                sbanks = [0, 1, 2, 3]
                obanks = [4, 5]
                sc = 0
                for gi, tiles in enumerate(qgroups):
                    nqs = len(tiles)
                    nq = nqs * 128
                    q0 = tiles[0] * 128
                    chunks = list(range(NT)) if tiles[0] < 32 else [32, 33]
                    o_grp = o_grps[gi % 2]
                    for lanes, fin in pair_list:
                        for ob in obanks:
                            S.op('dve', lambda e: e.memset(ps[ob][:, 0:nqs * 65], 0.0), w=[PR(ob)])
                        slots = {}

                        def qk_pair(ci):
                            nonlocal sc
                            c = chunks[ci]
                            for m in range(2):
                                kf, qf, vf = lanes[m]
                                sb = sbanks[sc % 4]
                                pT = pTs[sc % 6]
                                sc += 1
                                slots[(ci, m)] = (sb, pT)
                                S.op('pe', lambda e: e.matmul(ps[sb][:, 0:nq], lhsT=kf(c), rhs=qf(q0, nq), start=True, stop=True),
                                     r=[R('kT', c), R('qT', gi)], w=[PR(sb)])
                            for m in range(2):
                                sb, pT = slots[(ci, m)]
                                S.op('act', lambda e: e.activation(pT[:, 0:nq], ps[sb][:, 0:nq], AF.Exp, scale=scale),
                                     r=[PR(sb)], w=[R(pT.name)])

                        def pv_pair(ci):
                            c = chunks[ci]
                            for m in range(2):
                                kf, qf, vf = lanes[m]
                                sb, pT = slots.pop((ci, m))
                                for qs in range(nqs):
                                    S.op('pe', lambda e: e.matmul(ps[obanks[m]][:, qs * 65:(qs + 1) * 65],
                                                                  lhsT=pT[:, qs * 128:(qs + 1) * 128], rhs=vf(c),
                                                                  start=False, stop=(ci == len(chunks) - 1), skip_group_check=True),
                                         r=[R(pT.name), R('V', c)], w=[PR(obanks[m])])

                        qk_pair(0)
                        for ci in range(len(chunks)):
                            if ci + 1 < len(chunks):
                                qk_pair(ci + 1)
                            pv_pair(ci)
                        fin(nqs, obanks, o_grp)
                        if bg is not None:
                            for _ in range(bg_n):
                                f_ = next(bg, None)
                                if f_ is not None:
                                    f_()
                    transpose_out(i, o_grp, tiles, brs2[gi % 2], 7)

            with ExitStack() as ph:
                wq = T(ph, "wq", [128, 8, 512], BF16)
                qT_sb = T(ph, "qT_sb", [128, 2, NTOK], BF16)
                kT_sb = T(ph, "kT_sb", [128, 2, NTOK], BF16)
                Vaug = T(ph, "Vaug", [128, NT, 2, 65], BF16)
                cosT = T(ph, "cosT", [128, 32, 2, 16], F32)
                sinT = T(ph, "sinT", [128, 32, 2, 16], F32)
                hTs = [T(ph, "hTg%d" % i, [128, 8, 512], BF16) for i in range(2)]
                sqt = [T(ph, "sqt%d" % i, [128, 6, 64], F32) for i in range(4)]
                qn1 = [T(ph, "qn1%d" % i, [128, 8, 64], F32) for i in range(4)]
                qn2 = [T(ph, "qn2%d" % i, [128, 8, 64], F32) for i in range(4)]
                rm = [T(ph, "rm%d" % i, [128, 4, 8, 32], F32) for i in range(4)]
                qr = [T(ph, "qr%d" % i, [128, 8, 64], BF16) for i in range(4)]
                st6 = [T(ph, "st6%d" % i, [128, 18], F32) for i in range(4)]
                rden = [T(ph, "rden%d" % i, [128, 4], F32) for i in range(2)]
                wload(wq, 'wq', 1792, 2304)
                S.dma('sp', cosT[:], cosg_in, own=R('cosT'), w=[R('cosT')])
                S.dma('sp', sinT[:], sing_in, own=R('sinT'), w=[R('sinT')])
                S.op('pool', lambda e: e.memset(Vaug[:], 1.0), w=[R('V', c) for c in range(NT)])
                load_hT(hTs[0], GROUPS[0])
                for gi, tiles in enumerate(GROUPS):
                    hb = hTs[gi % 2]
                    if gi + 1 < len(GROUPS):
                        load_hT(hTs[(gi + 1) % 2], GROUPS[gi + 1])
                    for ti, tile in enumerate(tiles):
                        for k in range(8):
                            S.op('pe', lambda e: e.matmul(ps[ti][:, :], lhsT=hb[:, k, ti * 128:(ti + 1) * 128], rhs=wq[:, k, :],
                                                          start=(k == 0), stop=(k == 7)),
                                 r=[R(hb.name), R('wq')], w=[PR(ti)])
                    for ti, tile in enumerate(tiles):
                        S.op('act', lambda e: e.activation(Vaug[:, tile, :, 0:64], ps[ti][:, 384:512].rearrange("p (h d) -> p h d", h=2), AF.Copy),
                             r=[PR(ti)], w=[R('V', tile)])
                        S.op('act', lambda e: e.activation(sqt[ti][:], ps[ti][:, 0:384].rearrange("p (h d) -> p h d", h=6), AF.Square),
                             r=[PR(ti)], w=[R(sqt[ti].name)])
                    for ti, tile in enumerate(tiles):
                        S.op('dve', lambda e: e.reduce_sum(st6[ti][:, 0:6], sqt[ti][:], axis=AX.X), r=[R(sqt[ti].name)], w=[R(st6[ti].name, 0)])
                        S.op('dve', lambda e: e.tensor_scalar(st6[ti][:, 6:12], st6[ti][:, 0:6], 1.0 / 64, EPS, ALU.mult, ALU.add),
                             r=[R(st6[ti].name, 0)], w=[R(st6[ti].name, 1)])
                    for ti, tile in enumerate(tiles):
                        S.op('pool', lambda e: e.tensor_tensor(st6[ti][:, 12:18], st6[ti][:, 6:12], mhalf[:, 0:6], ALU.pow),
                             r=[R(st6[ti].name, 1), R('mhalf')], w=[R(st6[ti].name, 2)])
                    for ti, tile in enumerate(tiles):
                        S.op('dve', lambda e: e.tensor_tensor(qn1[ti][:, 0:4, :], ps[ti][:, 0:256].rearrange("p (h d) -> p h d", h=4),
                                                              mk(st6[ti], 0, 128, 12, [[1, 4], [0, 64]]), ALU.mult),
                             r=[PR(ti), R(st6[ti].name, 2)], w=[R(qn1[ti].name, 0)])
                        S.op('dve', lambda e: e.tensor_tensor(qn1[ti][:, 4:8, :].rearrange("p (k u) d -> p k u d", u=2),
                                                              mk(ps[ti], 0, 128, 256, [[64, 2], [0, 2], [1, 64]]),
                                                              mk(st6[ti], 0, 128, 16, [[1, 2], [0, 2], [0, 64]]), ALU.mult),
                             r=[PR(ti), R(st6[ti].name, 2)], w=[R(qn1[ti].name, 1)])
                    for ti, tile in enumerate(tiles):
                        if tile < 32:
                            S.op('pool', lambda e: e.tensor_tensor(qn2[ti][:], qn1[ti][:], gainG[:, l, :, :], ALU.mult),
                                 r=[R(qn1[ti].name, 0), R(qn1[ti].name, 1), R('gainG')], w=[R(qn2[ti].name)])
                        else:
                            S.op('pool', lambda e: e.tensor_tensor(qr[ti][:], qn1[ti][:], gainG[:, l, :, :], ALU.mult),
                                 r=[R(qn1[ti].name, 0), R(qn1[ti].name, 1), R('gainG')], w=[R(qr[ti].name, 0), R(qr[ti].name, 1)])
                    for half in range(2):
                        for ti, tile in enumerate(tiles):
                            if tile >= 32:
                                continue
                            X = qn2[ti]
                            x1 = mk(X, 0, 128, 0, [[64, 8], [32, 2], [1, 16]])
                            x2 = mk(X, 0, 128, 16, [[64, 8], [32, 2], [1, 16]])
                            cs = mk(cosT, 0, 128, tile * 32, [[0, 8], [16, 2], [1, 16]])
                            sn = mk(sinT, 0, 128, tile * 32, [[0, 8], [16, 2], [1, 16]])
                            m_ = [mk(rm[ti], 0, 128, q * 256, [[32, 8], [16, 2], [1, 16]]) for q in range(4)]
                            o1 = mk(qr[ti], 0, 128, 0, [[64, 8], [32, 2], [1, 16]])
                            o2 = mk(qr[ti], 0, 128, 16, [[64, 8], [32, 2], [1, 16]])
                            if half == 0:
                                S.op('dve', lambda e: e.tensor_tensor(m_[0], x1, cs, ALU.mult), r=[R(X.name), R('cosT')], w=[R(rm[ti].name, 0)])
                                S.op('dve', lambda e: e.tensor_tensor(m_[1], x2, sn, ALU.mult), r=[R(X.name), R('sinT')], w=[R(rm[ti].name, 1)])
                                S.op('pool', lambda e: e.tensor_tensor(m_[2], x1, sn, ALU.mult), r=[R(X.name), R('sinT')], w=[R(rm[ti].name, 2)])
                                S.op('pool', lambda e: e.tensor_tensor(m_[3], x2, cs, ALU.mult), r=[R(X.name), R('cosT')], w=[R(rm[ti].name, 3)])
                            else:
                                S.op('dve', lambda e: e.tensor_tensor(o1, m_[0], m_[1], ALU.subtract),
                                     r=[R(rm[ti].name, 0), R(rm[ti].name, 1)], w=[R(qr[ti].name, 0)])
                                S.op('pool', lambda e: e.tensor_tensor(o2, m_[2], m_[3], ALU.add),
                                     r=[R(rm[ti].name, 2), R(rm[ti].name, 3)], w=[R(qr[ti].name, 1)])
                    for ti, tile in enumerate(tiles):
                        tb = 4 + ti
                        for jj in range(4):
                            S.op('pe', lambda e: e.transpose(psb[tb][:, jj * 128:(jj + 1) * 128],
                                                             qr[ti][:, 2 * jj:2 * jj + 2, :].rearrange("p h d -> p (h d)"), identB[:]),
                                 r=[R(qr[ti].name, 0), R(qr[ti].name, 1), R('identB')], w=[PR(tb)])
                    for ti, tile in enumerate(tiles):
                        tb = 4 + ti
                        S.op('act', lambda e: e.activation(qT_sb[:, :, tile * 128:(tile + 1) * 128],
                                                           psb[tb][:, 0:256].rearrange("p (h q) -> p h q", h=2), AF.Copy),
                             r=[PR(tb)], w=[R('qT', tile // 4)])
                        S.op('dve', lambda e: e.tensor_copy(kT_sb[:, :, tile * 128:(tile + 1) * 128],
                                                            psb[tb][:, 256:512].rearrange("p (h q) -> p h q", h=2)),
                             r=[PR(tb)], w=[R('kT', tile)])

                def fin_gqa(h, nqs, ob, o_grp):
                    rd = rden[h % 2]
                    S.op('dve', lambda e: e.reciprocal(rd[:, 0:nqs], mk(ps[ob], 0, 128, 64, [[65, nqs]])), r=[PR(ob)], w=[R(rd.name)])
                    S.op('dve', lambda e: e.tensor_tensor(o_grp[:, 0:nqs, h * 64:(h + 1) * 64],
                                                          mk(ps[ob], 0, 128, 0, [[65, nqs], [1, 64]]),
                                                          mk(rd, 0, 128, 0, [[1, nqs], [0, 64]]), ALU.mult),
                         r=[PR(ob), R(rd.name)], w=[R(o_grp.name)])

                rp = T(ph, "rp", [60, 128], F32)
                Tall = T(ph, "Tall", [128, 60, 64], F32)
                maskc = T(ph, "maskc", [128, 21, 128], F32)
                S.op('pool', lambda e: e.memset(rp[:], 0.0), w=[R('rp')])
                S.dma('sp', rp[:, 48:79], nat_rpb[l], own=R('rp'), w=[R('rp')])
                S.dma('sp', rpbpad[l], rp[:], own=R('rp'), r=[R('rp')], w=[R('rpbpad')])
                S.dma('sp', maskc[:], natmask_in, own=R('maskc'), w=[R('maskc')])
                for b in range(2):
                    S.dma('sp', Tall[b * 64:(b + 1) * 64, :, :],
                          bass.AP(rpbpad.tensor, rpbpad[l].offset, [[1, 64], [128, 60], [1, 64]]),
                          own=R('Tall', b), r=[R('rpbpad')], w=[R('Tall', b)])
                bg_ops = []
                for h in range(4):
                    bg_ops.append(lambda h=h: S.op('dve', lambda e: e.tensor_copy(biasM[:, :, h, :], maskc[:]), r=[R('maskc')], w=[R('biasM')]))
                for v in range(21):
                    for (a, b), dr in nat_plan[v].items():
                        if dr is None:
                            continue
                        for h in range(4):
                            bg_ops.append(lambda v=v, a=a, b=b, dr=dr, h=h: S.op(
                                'dve', lambda e: e.scalar_tensor_tensor(biasM[b * 64:(b + 1) * 64, v, h, a * 64:(a + 1) * 64],
                                                                        Tall[b * 64:(b + 1) * 64, h * 15 + dr, :], 8.0,
                                                                        maskc[b * 64:(b + 1) * 64, v, a * 64:(a + 1) * 64], ALU.mult, ALU.add),
                                r=[R('Tall', b), R('maskc'), R('biasM')], w=[R('biasM')]))
                bg_iter = iter(bg_ops)

                def gqa_pair(j):
                    def lane(r):
                        return (lambda c: kT_sb[r * 64:(r + 1) * 64, j, c * 128:(c + 1) * 128],
                                lambda q0, nq: qT_sb[r * 64:(r + 1) * 64, j, q0:q0 + nq],
                                lambda c: Vaug[:, c, j, :])

                    def fin(nqs, obanks, o_grp):
                        for r in range(2):
                            fin_gqa(2 * j + r, nqs, obanks[r], o_grp)
                    return ([lane(0), lane(1)], fin)

                dense_attention(3, [gqa_pair(0), gqa_pair(1)], 0.125, ph, bg=bg_iter, bg_n=24)
                for f_ in bg_iter:
                    f_()
                S.phase_end()
            if stop == ('gqa', l):
                break

            with ExitStack() as ph:
                wq = T(ph, "wqd", [128, 8, 768], BF16)
                qT_sb = T(ph, "qT_sbd", [64, 4, NTOK], BF16)
                kT_sb = T(ph, "kT_sbd", [64, 4, NTOK], BF16)
                Vaug = T(ph, "Vaugd", [128, NT, 4, 65], BF16)
                cosT = T(ph, "cosTd", [128, 32, 2, 8], F32)
                sinT = T(ph, "sinTd", [128, 32, 2, 8], F32)
                hTs = [T(ph, "hTd%d" % i, [128, 8, 512], BF16) for i in range(2)]
                qf = [T(ph, "qf%d" % i, [128, 16, 32], F32) for i in range(2)]
                rm = [T(ph, "rmd%d" % i, [128, 4, 16, 16], F32) for i in range(2)]
                qr = [T(ph, "qrd%d" % i, [128, 16, 32], BF16) for i in range(2)]
                rden = [T(ph, "rdend%d" % i, [128, 16], F32) for i in range(2)]
                t0s = [T(ph, "t0s%d" % i, [128, 4, 64], F32) for i in range(2)]
                t1s = [T(ph, "t1s%d" % i, [128, 4, 64], F32) for i in range(2)]
                tsq = [T(ph, "tsq%d" % i, [128, 4, 64], F32) for i in range(2)]
                wload(wq, 'wq', 256, 1024)
                S.dma('sp', cosT[:], cosd_in, own=R('cosT'), w=[R('cosT')])
                S.dma('sp', sinT[:], sind_in, own=R('sinT'), w=[R('sinT')])
                S.op('pool', lambda e: e.memset(Vaug[:], 1.0), w=[R('V', c) for c in range(NT)])
                load_hT(hTs[0], GROUPS[0])
                bcnt = 0
                for gi, tiles in enumerate(GROUPS):
                    hb = hTs[gi % 2]
                    if gi + 1 < len(GROUPS):
                        load_hT(hTs[(gi + 1) % 2], GROUPS[gi + 1])
                    for t0_ in range(0, len(tiles), 2):
                        batch = [(t0_ + u, tiles[t0_ + u]) for u in range(min(2, len(tiles) - t0_))]
                        tbs = [4, 5] if bcnt % 2 == 0 else [6, 7]
                        bcnt += 1
                        for u, (ti, tile) in enumerate(batch):
                            pb = 2 * u
                            for k in range(8):
                                S.op('pe', lambda e: e.matmul(ps[pb][:, :], lhsT=hb[:, k, ti * 128:(ti + 1) * 128], rhs=wq[:, k, 0:512],
                                                              start=(k == 0), stop=(k == 7)),
                                     r=[R(hb.name), R('wq')], w=[PR(pb)])
                            for k in range(8):
                                S.op('pe', lambda e: e.matmul(ps[pb + 1][:, 0:256], lhsT=hb[:, k, ti * 128:(ti + 1) * 128], rhs=wq[:, k, 512:768],
                                                              start=(k == 0), stop=(k == 7)),
                                     r=[R(hb.name), R('wq')], w=[PR(pb + 1)])
                        for u, (ti, tile) in enumerate(batch):
                            pb = 2 * u
                            S.op('act', lambda e: e.activation(Vaug[:, tile, :, 0:64], ps[pb + 1][:, 0:256].rearrange("p (h d) -> p h d", h=4), AF.Copy),
                                 r=[PR(pb + 1)], w=[R('V', tile)])
                            if tile < 32:
                                S.op('act', lambda e: e.activation(qf[u][:].rearrange("p a b -> p (a b)"), ps[pb][:, :], AF.Copy),
                                     r=[PR(pb)], w=[R(qf[u].name)])
                            else:
                                S.op('act', lambda e: e.activation(qr[u][:].rearrange("p a b -> p (a b)"), ps[pb][:, :], AF.Copy),
                                     r=[PR(pb)], w=[R(qr[u].name, 0), R(qr[u].name, 1)])
                        for half in range(2):
                            for u, (ti, tile) in enumerate(batch):
                                if tile >= 32:
                                    continue
                                X = qf[u]
                                x1 = mk(X, 0, 128, 0, [[32, 16], [16, 2], [1, 8]])
                                x2 = mk(X, 0, 128, 8, [[32, 16], [16, 2], [1, 8]])
                                cs = mk(cosT, 0, 128, tile * 16, [[0, 16], [8, 2], [1, 8]])
                                sn = mk(sinT, 0, 128, tile * 16, [[0, 16], [8, 2], [1, 8]])
                                m_ = [mk(rm[u], 0, 128, q * 256, [[16, 16], [8, 2], [1, 8]]) for q in range(4)]
                                o1 = mk(qr[u], 0, 128, 0, [[32, 16], [16, 2], [1, 8]])
                                o2 = mk(qr[u], 0, 128, 8, [[32, 16], [16, 2], [1, 8]])
                                if half == 0:
                                    S.op('dve', lambda e: e.tensor_tensor(m_[0], x1, cs, ALU.mult), r=[R(X.name), R('cosT')], w=[R(rm[u].name, 0)])
                                    S.op('dve', lambda e: e.tensor_tensor(m_[1], x2, sn, ALU.mult), r=[R(X.name), R('sinT')], w=[R(rm[u].name, 1)])
                                    S.op('pool', lambda e: e.tensor_tensor(m_[2], x1, sn, ALU.mult), r=[R(X.name), R('sinT')], w=[R(rm[u].name, 2)])
                                    S.op('pool', lambda e: e.tensor_tensor(m_[3], x2, cs, ALU.mult), r=[R(X.name), R('cosT')], w=[R(rm[u].name, 3)])
                                else:
                                    S.op('dve', lambda e: e.tensor_tensor(o1, m_[0], m_[1], ALU.subtract),
                                         r=[R(rm[u].name, 0), R(rm[u].name, 1)], w=[R(qr[u].name, 0)])
                                    S.op('pool', lambda e: e.tensor_tensor(o2, m_[2], m_[3], ALU.add),
                                         r=[R(rm[u].name, 2), R(rm[u].name, 3)], w=[R(qr[u].name, 1)])
                        for u, (ti, tile) in enumerate(batch):
                            tb = tbs[u]
                            qrf = qr[u][:].rearrange("p a b -> p (a b)")
                            for hh in range(8):
                                S.op('pe', lambda e: e.transpose(psb[tb][0:64, hh * 128:(hh + 1) * 128], qrf[:, hh * 64:(hh + 1) * 64], identB[:]),
                                     r=[R(qr[u].name, 0), R(qr[u].name, 1), R('identB')], w=[PR(tb)])
                        for u, (ti, tile) in enumerate(batch):
                            tb = tbs[u]
                            S.op('act', lambda e: e.activation(qT_sb[0:64, :, tile * 128:(tile + 1) * 128],
                                                               psb[tb][0:64, 0:512].rearrange("p (h q) -> p h q", h=4), AF.Copy),
                                 r=[PR(tb)], w=[R('qT', tile // 4)])
                            S.op('dve', lambda e: e.tensor_copy(kT_sb[0:64, :, tile * 128:(tile + 1) * 128],
                                                                psb[tb][0:64, 512:1024].rearrange("p (h q) -> p h q", h=4)),
                                 r=[PR(tb)], w=[R('kT', tile)])

                def fin_diff(h, nqs, obanks, o_grp):
                    o0, o1 = obanks
                    rd = rden[h % 2]
                    t0, t1, tq = t0s[h % 2], t1s[h % 2], tsq[h % 2]
                    S.op('dve', lambda e: e.reciprocal(rd[:, 0:nqs], mk(ps[o0], 0, 128, 64, [[65, nqs]])), r=[PR(o0)], w=[R(rd.name, 0)])
                    S.op('dve', lambda e: e.reciprocal(rd[:, 4:4 + nqs], mk(ps[o1], 0, 128, 64, [[65, nqs]])), r=[PR(o1)], w=[R(rd.name, 1)])
                    S.op('dve', lambda e: e.tensor_scalar(rd[:, 4:4 + nqs], rd[:, 4:4 + nqs], nlam[:, l:l + 1], None, ALU.mult),
                         r=[R(rd.name, 1), R('nlam')], w=[R(rd.name, 1)])
                    S.op('dve', lambda e: e.tensor_tensor(t0[:, 0:nqs, :], mk(ps[o0], 0, 128, 0, [[65, nqs], [1, 64]]),
                                                          mk(rd, 0, 128, 0, [[1, nqs], [0, 64]]), ALU.mult),
                         r=[PR(o0), R(rd.name, 0)], w=[R(t0.name)])
                    S.op('dve', lambda e: e.tensor_tensor(t1[:, 0:nqs, :], mk(ps[o1], 0, 128, 0, [[65, nqs], [1, 64]]),
                                                          mk(rd, 0, 128, 4, [[1, nqs], [0, 64]]), ALU.mult),
                         r=[PR(o1), R(rd.name, 1)], w=[R(t1.name)])
                    S.op('dve', lambda e: e.tensor_tensor(t0[:, 0:nqs, :], t0[:, 0:nqs, :], t1[:, 0:nqs, :], ALU.add),
                         r=[R(t0.name), R(t1.name)], w=[R(t0.name)])
                    S.op('dve', lambda e: e.tensor_tensor(tq[:, 0:nqs, :], t0[:, 0:nqs, :], t0[:, 0:nqs, :], ALU.mult),
                         r=[R(t0.name)], w=[R(tq.name)])
                    S.op('dve', lambda e: e.reduce_sum(rd[:, 8:8 + nqs], tq[:, 0:nqs, :], axis=AX.X), r=[R(tq.name)], w=[R(rd.name, 2)])
                    S.op('dve', lambda e: e.tensor_scalar(rd[:, 8:8 + nqs], rd[:, 8:8 + nqs], 1.0 / 64, EPS, ALU.mult, ALU.add),
                         r=[R(rd.name, 2)], w=[R(rd.name, 2)])
                    S.op('pool', lambda e: e.tensor_tensor(rd[:, 12:12 + nqs], rd[:, 8:8 + nqs], mhalf[:, 0:nqs], ALU.pow),
                         r=[R(rd.name, 2), R('mhalf')], w=[R(rd.name, 3)])
                    S.op('dve', lambda e: e.tensor_tensor(t1[:, 0:nqs, :], t0[:, 0:nqs, :], mk(rd, 0, 128, 12, [[1, nqs], [0, 64]]), ALU.mult),
                         r=[R(t0.name), R(rd.name, 3)], w=[R(t1.name)])
                    S.op('dve', lambda e: e.tensor_tensor(o_grp[:, 0:nqs, h * 64:(h + 1) * 64], t1[:, 0:nqs, :],
                                                           mk(gB, 0, 128, l * 64, [[0, nqs], [1, 64]]), ALU.mult),
                         r=[R(t1.name), R('gB')], w=[R(o_grp.name)])

                def diff_pair(h):
                    def lane(m):
                        return (lambda c: kT_sb[m * 32:(m + 1) * 32, h, c * 128:(c + 1) * 128],
                                lambda q0, nq: qT_sb[m * 32:(m + 1) * 32, h, q0:q0 + nq],
                                lambda c: Vaug[:, c, h, :])
                    return ([lane(0), lane(1)], lambda nqs, obanks, o_grp: fin_diff(h, nqs, obanks, o_grp))

                if SPARSE:
                    issue_casts(l)
                dense_attention(1, [diff_pair(h) for h in range(4)], 32 ** -0.5, ph)
                S.phase_end()
            if stop == ('diff', l):
                break

            with ExitStack() as ph:
                wq = T(ph, "wqn", [128, 8, 768], BF16)
                qT_sb = T(ph, "qT_sbn", [64, 4, NTOK], BF16)
                kT_sb = T(ph, "kT_sbn", [64, 4, NTOK], BF16)
                Vaug = T(ph, "Vaugn", [128, NT, 4, 65], BF16)
                hTs = [T(ph, "hTn%d" % i, [128, 8, 512], BF16) for i in range(2)]
                pTa = [T(ph, "pTa%d" % i, [128, 512], BF16) for i in range(2)]
                pTb = [T(ph, "pTb%d" % i, [128, 512], BF16) for i in range(2)]
                o_grps = [T(ph, "ogn%d" % k, [128, 4, 256], BF16) for k in range(2)]
                brs2 = [T(ph, "brsn%d" % k, [64, 4, 512], BF16) for k in range(2)]
                rden = [T(ph, "rdenn%d" % i, [128, 4], F32) for i in range(2)]
                wload(wq, 'wq', 1024, 1792)
                S.op('pool', lambda e: e.memset(Vaug[:], 1.0), w=[R('V', c) for c in range(NT)])
                load_hT(hTs[0], GROUPS[0])
                pc = 0
                for gi, tiles in enumerate(GROUPS):
                    hb = hTs[gi % 2]
                    ntok = len(tiles) * 128
                    t0 = tiles[0] * 128
                    if gi + 1 < len(GROUPS):
                        load_hT(hTs[(gi + 1) % 2], GROUPS[gi + 1])
                    for qk in range(2):
                        for h in range(4):
                            pb = pc % 4
                            pc += 1
                            c0 = qk * 256 + h * 64
                            for k in range(8):
                                S.op('pe', lambda e: e.matmul(ps[pb][0:64, 0:ntok], lhsT=wq[:, k, c0:c0 + 64], rhs=hb[:, k, 0:ntok],
                                                              start=(k == 0), stop=(k == 7)),
                                     r=[R(hb.name), R('wq')], w=[PR(pb)])
                            dst = (qT_sb if qk == 0 else kT_sb)[0:64, h, t0:t0 + ntok]
                            wres = [R('qT', gi)] if qk == 0 else [R('kT', t) for t in tiles]
                            if pc % 2:
                                S.op('dve', lambda e: e.tensor_copy(dst, ps[pb][0:64, 0:ntok]), r=[PR(pb)], w=wres)
                            else:
                                S.op('act', lambda e: e.activation(dst, ps[pb][0:64, 0:ntok], AF.Copy), r=[PR(pb)], w=wres)
                    for ti, tile in enumerate(tiles):
                        pb = pc % 4
                        pc += 1
                        for k in range(8):
                            S.op('pe', lambda e: e.matmul(ps[pb][:, 0:256], lhsT=hb[:, k, ti * 128:(ti + 1) * 128], rhs=wq[:, k, 512:768],
                                                          start=(k == 0), stop=(k == 7)),
                                 r=[R(hb.name), R('wq')], w=[PR(pb)])
                        S.op('act', lambda e: e.activation(Vaug[:, tile, :, 0:64], ps[pb][:, 0:256].rearrange("p (h d) -> p h d", h=4), AF.Copy),
                             r=[PR(pb)], w=[R('V', tile)])
                qgroups = GROUPS if need_ctx else GROUPS[:8]
                sc = 0
                for gi, tiles in enumerate(qgroups):
                    o_grp = o_grps[gi % 2]
                    for ti, j in enumerate(tiles):
                        if j < 32:
                            kts = [(t, _nat_variant(j, t)) for t in _nat_keytiles(j)] + [(32, None), (33, None)]
                        else:
                            kts = [(32, None), (33, None)]
                        ob = 4 + (ti % 2)
                        hs = {}

                        def nat_a(h):
                            nonlocal sc
                            sa = (sc % 2) * 2
                            pa, pbb = pTa[sc % 2], pTb[sc % 2]
                            sc += 1
                            hs[h] = (pa, pbb)
                            for ii, (t, v) in enumerate(kts):
                                bank = sa + ii // 4
                                col = (ii % 4) * 128
                                S.op('pe', lambda e: e.matmul(ps[bank][:, col:col + 128], lhsT=kT_sb[0:64, h, t * 128:(t + 1) * 128],
                                                              rhs=qT_sb[0:64, h, j * 128:(j + 1) * 128], start=True, stop=(v is None)),
                                     r=[R('kT', t), R('qT', gi)], w=[PR(bank)])
                                if v is not None:
                                    S.op('pe', lambda e: e.matmul(ps[bank][:, col:col + 128], lhsT=biasM[:, v, h, :], rhs=antiB[:],
                                                                  start=False, stop=True),
                                         r=[R('biasM'), R('antiB')], w=[PR(bank)])
                            na = min(4, len(kts))
                            nb = len(kts) - na
                            S.op('act', lambda e: e.activation(pa[:, 0:na * 128], ps[sa][:, 0:na * 128], AF.Exp, scale=0.125),
                                 r=[PR(sa)], w=[R(pa.name)])
                            if nb:
                                S.op('act', lambda e: e.activation(pbb[:, 0:nb * 128], ps[sa + 1][:, 0:nb * 128], AF.Exp, scale=0.125),
                                     r=[PR(sa + 1)], w=[R(pbb.name)])

                        def nat_b(h):
                            pa, pbb = hs.pop(h)
                            for ii, (t, v) in enumerate(kts):
                                src = pa if ii < 4 else pbb
                                col = (ii % 4) * 128
                                S.op('pe', lambda e: e.matmul(ps[ob][:, h * 65:(h + 1) * 65], lhsT=src[:, col:col + 128], rhs=Vaug[:, t, h, :],
                                                              start=(ii == 0), stop=(ii == len(kts) - 1)),
                                     r=[R(src.name), R('V', t)], w=[PR(ob)])

                        nat_a(0)
                        for h in range(4):
                            if h + 1 < 4:
                                nat_a(h + 1)
                            nat_b(h)
                        rd = rden[ti % 2]
                        S.op('dve', lambda e: e.reciprocal(rd[:, 0:4], mk(ps[ob], 0, 128, 64, [[65, 4]])), r=[PR(ob)], w=[R(rd.name)])
                        S.op('dve', lambda e: e.tensor_tensor(o_grp[:, ti, :].rearrange("p (h d) -> p h d", h=4),
                                                              mk(ps[ob], 0, 128, 0, [[65, 4], [1, 64]]),
                                                              mk(rd, 0, 128, 0, [[1, 4], [0, 64]]), ALU.mult),
                             r=[PR(ob), R(rd.name)], w=[R(o_grp.name)])
                    transpose_out(2, o_grp, tiles, brs2[gi % 2], 7)
                S.phase_end()
            lay.close()
            if stop == ('nat', l):
                break

            up_groups = GROUPS if need_ctx else GROUPS[:8]
            with ExitStack() as ph:
                wgate = T(ph, "wgate", [128, 8, 4096], BF16)
                wbr = T(ph, "wbr", [128, 4, 2, D], BF16)
                hTs = [T(ph, "hTm%d" % i, [128, 8, 512], BF16) for i in range(2)]
                brTs = [T(ph, "brTm%d" % i, [128, 4, 2, 512], BF16) for i in range(2)]
                mTs = [T(ph, "mTs%d" % i, [128, 8, 512], BF16) for i in range(2)]
                sgs = [T(ph, "sg%d" % i, [128, 512], F32) for i in range(3)]
                accs = [T(ph, "acc%d" % i, [128, 512], F32) for i in range(2)]
                tmps = [T(ph, "tmpm%d" % i, [128, 512], F32) for i in range(2)]
                for q4 in range(4):
                    S.dma('pool', wgate[:, :, q4 * 1024:(q4 + 1) * 1024],
                          w_in[l][:, 2304 + q4 * 1024:2304 + (q4 + 1) * 1024].rearrange("(k p) n -> p k n", p=128),
                          own=R('wgate', q4), w=[R('wgate', q4)])
                S.dma('pool', wbr[:], w_branch[l].rearrange("b (m p) n -> p b m n", p=128), own=R('wbr'), w=[R('wbr')])

                def load_br(bt, tiles):
                    ntok = len(tiles) * 128
                    t0 = tiles[0] * 128
                    for hh in range(2):
                        S.dma('sp', bt[hh * 64:(hh + 1) * 64, :, :, 0:ntok],
                              brT_d[:, :, :, t0:t0 + ntok].rearrange("b (m hh) d t -> hh d b m t", hh=2)[hh],
                              own=R(bt.name, hh), r=[R('br', i, t) for i in range(4) for t in tiles], w=[R(bt.name, hh)])

                load_hT(hTs[0], up_groups[0])
                load_br(brTs[0], up_groups[0])
                cnt = 0
                for gi, tiles in enumerate(up_groups):
                    ntok = len(tiles) * 128
                    hb, bt, mt = hTs[gi % 2], brTs[gi % 2], mTs[gi % 2]
                    if gi + 1 < len(up_groups):
                        load_hT(hTs[(gi + 1) % 2], up_groups[gi + 1])
                        load_br(brTs[(gi + 1) % 2], up_groups[gi + 1])
                    for dc in range(8):
                        acc = accs[dc % 2]
                        for i in range(4):
                            pg = (cnt % 3)
                            pp = 3 + (cnt % 3)
                            sg = sgs[cnt % 3]
                            cnt += 1
                            c0 = i * 1024 + dc * 128
                            for k in range(8):
                                S.op('pe', lambda e: e.matmul(ps[pg][:, 0:ntok], lhsT=wgate[:, k, c0:c0 + 128], rhs=hb[:, k, 0:ntok],
                                                              start=(k == 0), stop=(k == 7)),
                                     r=[R(hb.name), R('wgate', i)], w=[PR(pg)])
                            for m in range(2):
                                S.op('pe', lambda e: e.matmul(ps[pp][:, 0:ntok], lhsT=wbr[:, i, m, dc * 128:(dc + 1) * 128], rhs=bt[:, i, m, 0:ntok],
                                                              start=(m == 0), stop=(m == 1)),
                                     r=[R(bt.name, 0), R(bt.name, 1), R('wbr')], w=[PR(pp)])
                            S.op('act', lambda e: e.activation(sg[:, 0:ntok], ps[pg][:, 0:ntok], AF.Sigmoid), r=[PR(pg)], w=[R(sg.name)])
                            if i == 0:
                                S.op('dve', lambda e: e.tensor_tensor(acc[:, 0:ntok], sg[:, 0:ntok], ps[pp][:, 0:ntok], ALU.mult),
                                     r=[R(sg.name), PR(pp)], w=[R(acc.name)])
                            else:
                                tm = tmps[i % 2]
                                S.op('dve', lambda e: e.tensor_tensor(tm[:, 0:ntok], sg[:, 0:ntok], ps[pp][:, 0:ntok], ALU.mult),
                                     r=[R(sg.name), PR(pp)], w=[R(tm.name)])
                                if i < 3:
                                    S.op('pool', lambda e: e.tensor_tensor(acc[:, 0:ntok], acc[:, 0:ntok], tm[:, 0:ntok], ALU.add),
                                         r=[R(acc.name), R(tm.name)], w=[R(acc.name)])
                                else:
                                    S.op('pool', lambda e: e.tensor_tensor(mt[:, dc, 0:ntok], acc[:, 0:ntok], tm[:, 0:ntok], ALU.add),
                                         r=[R(acc.name), R(tm.name)], w=[R(mt.name, dc)])
                    t0 = tiles[0] * 128
                    S.dma('sp', mT_d[:, :, t0:t0 + ntok].rearrange("c p t -> p c t"), mt[:, :, 0:ntok], own=R(mt.name, 0),
                          r=[R(mt.name, c) for c in range(8)], w=[R('mT', t) for t in tiles])
                S.phase_end()
            if stop == ('merge', l):
                break

            with ExitStack() as ph:
                wout = T(ph, "wout", [128, 8, D], BF16)
                mTs = [T(ph, "mTo%d" % i, [128, 8, 512], BF16) for i in range(2)]
                xts = [T(ph, "xto%d" % i, [128, 4, D], F32) for i in range(2)]
                gtb = T(ph, "gtb", [128, 2, D], F32)
                tmps = [T(ph, "tmpo%d" % i, [128, 512], F32) for i in range(3)]
                S.dma('pool', wout[:], w_out[l].rearrange("(k p) n -> p k n", p=128), own=R('wout'), w=[R('wout')])
                for kd in range(2):
                    S.dma('sp', gtb[:, kd, :], bass.AP(modd.tensor, modd[l, kd, 2 * D:3 * D].offset, [[0, 128], [1, D]]),
                          own=R('gtb'), r=[R('modd', l)], w=[R('gtb')])

                def load_m(mt, xt, tiles):
                    ntok = len(tiles) * 128
                    t0 = tiles[0] * 128
                    S.dma('sp', mt[:, :, 0:ntok], mT_d[:, :, t0:t0 + ntok].rearrange("c p t -> p c t"), own=R(mt.name),
                          r=[R('mT', t) for t in tiles], w=[R(mt.name)])
                    S.dma('sp', xt[:, 0:len(tiles), :], x_src(l, tiles), own=R(xt.name), r=x_res(l, tiles), w=[R(xt.name)])

                load_m(mTs[0], xts[0], up_groups[0])
                cnt = 0
                for gi, tiles in enumerate(up_groups):
                    n = len(tiles)
                    mt, xt = mTs[gi % 2], xts[gi % 2]
                    kd = kind_of(tiles[0])
                    if gi + 1 < len(up_groups):
                        load_m(mTs[(gi + 1) % 2], xts[(gi + 1) % 2], up_groups[gi + 1])
                    for ts in range(n):
                        for hf in range(2):
                            pb = cnt % 4
                            tm = tmps[cnt % 3]
                            cnt += 1
                            for k in range(8):
                                S.op('pe', lambda e: e.matmul(ps[pb][:, :], lhsT=mt[:, k, ts * 128:(ts + 1) * 128], rhs=wout[:, k, hf * 512:(hf + 1) * 512],
                                                              start=(k == 0), stop=(k == 7)),
                                     r=[R(mt.name), R('wout')], w=[PR(pb)])
                            S.op('dve', lambda e: e.tensor_tensor(tm[:], ps[pb][:, :], gtb[:, kd, hf * 512:(hf + 1) * 512], ALU.mult),
                                 r=[PR(pb), R('gtb')], w=[R(tm.name)])
                            S.op('pool', lambda e: e.tensor_tensor(xt[:, ts, hf * 512:(hf + 1) * 512], xt[:, ts, hf * 512:(hf + 1) * 512], tm[:], ALU.add),
                                 r=[R(tm.name), R(xt.name)], w=[R(xt.name)])
                    S.dma('sp', xs_d[tiles[0] * 128:(tiles[0] + n) * 128, :].rearrange("(t p) d -> p t d", p=128), xt[:, 0:n, :],
                          own=R(xt.name), r=[R(xt.name)], w=[R('xs', t) for t in tiles])
                S.phase_end()
            if stop == ('oproj', l):
                break

            if SPARSE:
                moe_sparse(l, need_ctx)
                if stop == ('moe', l):
                    break
                continue
            last = (l == DEPTH - 1)
            moe_tiles = list(range(34)) if need_ctx else list(range(32))
            nsg = 3
            per = (len(moe_tiles) + nsg - 1) // nsg
            SGS = [moe_tiles[i * per:(i + 1) * per] for i in range(nsg)]
            with ExitStack() as ph:
                tT_sb = T(ph, "tT_sb", [128, 8, per * 128], BF16)
                yacc = T(ph, "yacc", [128, per, D], F32)
                wgu = [T(ph, "wgu%d" % i, [128, 8, 1024], BF16) for i in range(2)]
                wdn = [T(ph, "wdn%d" % i, [128, 4, 1024], BF16) for i in range(2)]
                aT = [T(ph, "aT%d" % i, [128, 4, 512], BF16) for i in range(2)]
                sgt = [T(ph, "sgm%d" % i, [128, 512], BF16) for i in range(3)]
                comb = T(ph, "comb", [128, per, 32], F32)
                xt = T(ph, "xtm", [128, 4, D], F32)
                junk = T(ph, "junkm", [128, D], BF16)
                diag = T(ph, "diagm", [128, 4, 128], F32)
                ssb = T(ph, "ssbm", [128, 12], F32)
                tT32 = T(ph, "tT32", [128, 8, 512], F32)
                wr32 = T(ph, "wr32", [128, 8, 36], F32)
                brt = T(ph, "brt", [128, 36], F32)
                gt2 = T(ph, "gt2", [128, 2, D], F32)
                gfin = T(ph, "gfin", [128, D], F32)
                lg = [T(ph, "lg%d" % i, [128, 36], F32) for i in range(2)]
                rt = [T(ph, "rt%d" % i, [128, 128], F32) for i in range(2)]
                S.dma('sp', wr32[:, :, 0:4], w_rg[l].rearrange("(k p) n -> p k n", p=128), own=R('wr32'), w=[R('wr32')])
                S.dma('sp', wr32[:, :, 4:36], w_re[l].rearrange("(k p) n -> p k n", p=128), own=R('wr32'), w=[R('wr32')])
                S.dma('sp', brt[:, 0:4], bass.AP(b_rg.tensor, b_rg[l].offset, [[0, 128], [1, 4]]), own=R('brt'), w=[R('brt')])
                S.dma('sp', brt[:, 4:36], bass.AP(b_re.tensor, b_re[l].offset, [[0, 128], [1, 32]]), own=R('brt'), w=[R('brt')])
                for kd in range(2):
                    S.dma('sp', gt2[:, kd, :], bass.AP(modd.tensor, modd[l, kd, 5 * D:6 * D].offset, [[0, 128], [1, D]]),
                          own=R('gt2'), r=[R('modd', l)], w=[R('gt2')])
                S.dma('sp', gfin[:], bass.AP(g_final.tensor, g_final.offset, [[0, 128], [1, D]]), own=R('gfin'), w=[R('gfin')])

                def load_exp(e_, slot):
                    S.dma('pool', wgu[slot][:, :, 0:512], w_eg[l, e_].rearrange("(k p) n -> p k n", p=128),
                          own=R('wgu', slot, 0), w=[R('wgu', slot)])
                    S.dma('pool', wgu[slot][:, :, 512:1024], w_eu[l, e_].rearrange("(k p) n -> p k n", p=128),
                          own=R('wgu', slot, 1), w=[R('wgu', slot)])
                    S.dma('pool', wdn[slot][:], w_ed[l, e_].rearrange("(k p) n -> p k n", p=128),
                          own=R('wdn', slot), w=[R('wdn', slot)])

                ecnt = 0
                for sgi, sgt_tiles in enumerate(SGS):
                    chunks = []
                    cur = []
                    for t in sgt_tiles:
                        if cur and (len(cur) == 4 or kind_of(cur[0]) != kind_of(t)):
                            chunks.append(cur)
                            cur = []
                        cur.append(t)
                    if cur:
                        chunks.append(cur)
                    base = sgt_tiles[0]
                    load_exp(0, ecnt % 2)
                    for ch in chunks:
                        n = len(ch)
                        ntok = n * 128
                        off = (ch[0] - base) * 128
                        norm_mod_T(l, 2, ch, xt, junk, diag, ssb,
                                   lambda c: (tT_sb[:, c, off:off + ntok], [R('tT', t) for t in ch]),
                                   lambda c: (tT32[:, c, 0:ntok], [R('tT32', c)]),
                                   [0, 1, 2, 3], x_src(l, ch, True), x_res(l, ch, True))
                        for ti, tile in enumerate(ch):
                            lt = tile - base
                            b = lt % 2
                            L, Rt = lg[b], rt[b]
                            for k in range(8):
                                S.op('pe', lambda e: e.matmul(ps[4 + b][:, 0:36], lhsT=tT32[:, k, ti * 128:(ti + 1) * 128], rhs=wr32[:, k, :],
                                                              start=(k == 0), stop=(k == 7)),
                                     r=[R('tT32', k), R('wr32')], w=[PR(4 + b)])
                            S.op('dve', lambda e: e.tensor_tensor(L[:], ps[4 + b][:, 0:36], brt[:], ALU.add), r=[PR(4 + b), R('brt')], w=[R(L.name)])
                            S.op('dve', lambda e: e.reduce_max(Rt[:, 0:1], L[:, 0:4], axis=AX.X), r=[R(L.name)], w=[R(Rt.name, 0)])
                            S.op('dve', lambda e: e.tensor_scalar(Rt[:, 1:2], Rt[:, 0:1], -1.0, None, ALU.mult), r=[R(Rt.name, 0)], w=[R(Rt.name, 1)])
                            S.op('act', lambda e: e.activation(Rt[:, 12:16], L[:, 0:4], AF.Exp, bias=Rt[:, 1:2], scale=1.0, accum_out=Rt[:, 2:3]),
                                 r=[R(L.name), R(Rt.name, 1)], w=[R(Rt.name, 2)])
                            S.op('dve', lambda e: e.reciprocal(Rt[:, 3:4], Rt[:, 2:3]), r=[R(Rt.name, 2)], w=[R(Rt.name, 3)])
                            S.op('dve', lambda e: e.tensor_scalar(Rt[:, 4:8], L[:, 0:4], Rt[:, 0:1], None, ALU.is_equal),
                                 r=[R(L.name), R(Rt.name, 0)], w=[R(Rt.name, 4)])
                            S.op('dve', lambda e: e.tensor_scalar(Rt[:, 8:12], Rt[:, 4:8], -1.0, 1e30, ALU.add, ALU.mult),
                                 r=[R(Rt.name, 4)], w=[R(Rt.name, 5)])
                            S.op('dve', lambda e: e.tensor_tensor(Rt[:, 16:48].rearrange("p (g k) -> p g k", g=4),
                                                                  L[:, 4:36].rearrange("p (g k) -> p g k", g=4),
                                                                  mk(Rt, 0, 128, 8, [[1, 4], [0, 8]]), ALU.add),
                                 r=[R(L.name), R(Rt.name, 5)], w=[R(Rt.name, 6)])
                            S.op('dve', lambda e: e.reduce_max(Rt[:, 48:49], Rt[:, 16:48], axis=AX.X), r=[R(Rt.name, 6)], w=[R(Rt.name, 7)])
                            S.op('dve', lambda e: e.tensor_scalar(Rt[:, 56:88], Rt[:, 16:48], Rt[:, 48:49], None, ALU.is_equal),
                                 r=[R(Rt.name, 6), R(Rt.name, 7)], w=[R(Rt.name, 8)])
                            S.op('dve', lambda e: e.scalar_tensor_tensor(Rt[:, 88:120], Rt[:, 56:88], -1e30, Rt[:, 16:48], ALU.mult, ALU.add),
                                 r=[R(Rt.name, 8), R(Rt.name, 6)], w=[R(Rt.name, 9)])
                            S.op('dve', lambda e: e.reduce_max(Rt[:, 49:50], Rt[:, 88:120], axis=AX.X), r=[R(Rt.name, 9)], w=[R(Rt.name, 10)])
                            S.op('dve', lambda e: e.tensor_scalar(Rt[:, 88:120], Rt[:, 88:120], Rt[:, 49:50], None, ALU.is_equal),
                                 r=[R(Rt.name, 9), R(Rt.name, 10)], w=[R(Rt.name, 11)])
                            S.op('dve', lambda e: e.tensor_scalar(Rt[:, 50:51], Rt[:, 48:49], -1.0, None, ALU.mult), r=[R(Rt.name, 7)], w=[R(Rt.name, 12)])
                            S.op('act', lambda e: e.activation(Rt[:, 51:52], Rt[:, 49:50], AF.Exp, bias=Rt[:, 50:51], scale=1.0),
                                 r=[R(Rt.name, 10), R(Rt.name, 12)], w=[R(Rt.name, 13)])
                            S.op('dve', lambda e: e.tensor_scalar(Rt[:, 52:53], Rt[:, 51:52], 1.0, None, ALU.add), r=[R(Rt.name, 13)], w=[R(Rt.name, 14)])
                            S.op('dve', lambda e: e.reciprocal(Rt[:, 53:54], Rt[:, 52:53]), r=[R(Rt.name, 14)], w=[R(Rt.name, 15)])
                            S.op('dve', lambda e: e.tensor_tensor(Rt[:, 53:54], Rt[:, 53:54], Rt[:, 3:4], ALU.mult),
                                 r=[R(Rt.name, 15), R(Rt.name, 3)], w=[R(Rt.name, 15)])
                            S.op('dve', lambda e: e.tensor_tensor(Rt[:, 54:55], Rt[:, 53:54], Rt[:, 51:52], ALU.mult),
                                 r=[R(Rt.name, 15), R(Rt.name, 13)], w=[R(Rt.name, 16)])
                            S.op('dve', lambda e: e.tensor_scalar(Rt[:, 56:88], Rt[:, 56:88], Rt[:, 53:54], None, ALU.mult),
                                 r=[R(Rt.name, 8), R(Rt.name, 15)], w=[R(Rt.name, 8)])
                            S.op('dve', lambda e: e.scalar_tensor_tensor(comb[:, lt, :], Rt[:, 88:120], Rt[:, 54:55], Rt[:, 56:88], ALU.mult, ALU.add),
                                 r=[R(Rt.name, 11), R(Rt.name, 16), R(Rt.name, 8)], w=[R('comb', lt)])
                    nsgt = len(sgt_tiles)
                    S.op('pool', lambda e: e.memset(yacc[:, 0:nsgt, :], 0.0), w=[R('yacc', t) for t in range(nsgt)])
                    pcnt = 0
                    for e_ in range(32):
                        slot = ecnt % 2
                        ecnt += 1
                        if e_ + 1 < 32:
                            load_exp(e_ + 1, ecnt % 2)
                        for ch in chunks:
                            n = len(ch)
                            ntok = n * 128
                            off = (ch[0] - base) * 128
                            a_ = aT[pcnt % 2]
                            for fc in range(4):
                                pg = (pcnt * 4 + fc) % 2
                                pu = 2 + (pcnt * 4 + fc) % 2
                                sgb = sgt[(pcnt * 4 + fc) % 3]
                                for k in range(8):
                                    S.op('pe', lambda e: e.matmul(ps[pg][:, 0:ntok], lhsT=wgu[slot][:, k, fc * 128:(fc + 1) * 128],
                                                                  rhs=tT_sb[:, k, off:off + ntok], start=(k == 0), stop=(k == 7)),
                                         r=[R('wgu', slot)] + [R('tT', t) for t in ch], w=[PR(pg)])
                                for k in range(8):
                                    S.op('pe', lambda e: e.matmul(ps[pu][:, 0:ntok], lhsT=wgu[slot][:, k, 512 + fc * 128:512 + (fc + 1) * 128],
                                                                  rhs=tT_sb[:, k, off:off + ntok], start=(k == 0), stop=(k == 7)),
                                         r=[R('wgu', slot)] + [R('tT', t) for t in ch], w=[PR(pu)])
                                S.op('act', lambda e: e.activation(sgb[:, 0:ntok], ps[pg][:, 0:ntok], AF.Silu), r=[PR(pg)], w=[R(sgb.name)])
                                S.op('dve', lambda e: e.tensor_tensor(a_[:, fc, 0:ntok], sgb[:, 0:ntok], ps[pu][:, 0:ntok], ALU.mult),
                                     r=[R(sgb.name), PR(pu)], w=[R(a_.name, fc)])
                            for ti, tile in enumerate(ch):
                                lt = tile - base
                                for hf in range(2):
                                    py = 4 + (pcnt * 8 + ti * 2 + hf) % 4
                                    for fc in range(4):
                                        S.op('pe', lambda e: e.matmul(ps[py][:, :], lhsT=a_[:, fc, ti * 128:(ti + 1) * 128],
                                                                      rhs=wdn[slot][:, fc, hf * 512:(hf + 1) * 512], start=(fc == 0), stop=(fc == 3)),
                                             r=[R(a_.name, fc), R('wdn', slot)], w=[PR(py)])
                                    S.op('dve', lambda e: e.scalar_tensor_tensor(yacc[:, lt, hf * 512:(hf + 1) * 512], ps[py][:, :],
                                                                                 comb[:, lt, e_:e_ + 1], yacc[:, lt, hf * 512:(hf + 1) * 512],
                                                                                 ALU.mult, ALU.add),
                                         r=[PR(py), R('comb', lt), R('yacc', lt)], w=[R('yacc', lt)])
                            pcnt += 1
                    for ch in chunks:
                        n = len(ch)
                        kd = kind_of(ch[0])
                        S.dma('sp', xt[:, 0:n, :], x_src(l, ch, True), own=R(xt.name), r=x_res(l, ch, True), w=[R(xt.name)])
                        for ti, tile in enumerate(ch):
                            lt = tile - base
                            S.op('pool', lambda e: e.tensor_tensor(yacc[:, lt, :], yacc[:, lt, :], gt2[:, kd, :], ALU.mult),
                                 r=[R('yacc', lt), R('gt2')], w=[R('yacc', lt)])
                            S.op('pool', lambda e: e.tensor_tensor(xt[:, ti, :], xt[:, ti, :], yacc[:, lt, :], ALU.add),
                                 r=[R('yacc', lt), R(xt.name)], w=[R(xt.name)])
                        if not last:
                            S.dma('sp', xs_d[ch[0] * 128:(ch[0] + n) * 128, :].rearrange("(t p) d -> p t d", p=128), xt[:, 0:n, :],
                                  own=R(xt.name), r=[R(xt.name)], w=[R('xs', t) for t in ch])
                        else:
                            for ti in range(n):
                                S.op('act', lambda e: e.activation(junk[:], xt[:, ti, :], AF.Square, accum_out=ssb[:, ti:ti + 1]),
                                     r=[R(xt.name)], w=[R(junk.name), R(ssb.name, ti)])
                            S.op('dve', lambda e: e.tensor_scalar(ssb[:, 4:4 + n], ssb[:, 0:n], 1.0 / D, EPS, ALU.mult, ALU.add),
                                 r=[R(ssb.name, t) for t in range(n)], w=[R(ssb.name, 'm')])
                            S.op('pool', lambda e: e.tensor_tensor(ssb[:, 8:8 + n], ssb[:, 4:4 + n], mhalf[:, 0:n], ALU.pow),
                                 r=[R(ssb.name, 'm'), R('mhalf')], w=[R(ssb.name, 'r')])
                            for ti in range(n):
                                S.op('dve', lambda e: e.scalar_tensor_tensor(xt[:, ti, :], xt[:, ti, :], ssb[:, 8 + ti:9 + ti], gfin[:], ALU.mult, ALU.mult),
                                     r=[R(xt.name), R(ssb.name, 'r'), R('gfin')], w=[R(xt.name)])
                            S.dma('sp', out_d[ch[0] * 128:(ch[0] + n) * 128, :].rearrange("(t p) d -> p t d", p=128), xt[:, 0:n, :],
                                  own=R(xt.name), r=[R(xt.name)], w=[R('out', t) for t in ch])
                S.phase_end()
            if stop == ('moe', l):
                break
        S.barrier()
        build.stats = (S.nops, S.nwaits, S.nds)
    return nc


_CACHE = {}


def kernel(**inputs):
    consts = _host_consts()
    if 'nc' not in _CACHE:
        _CACHE['nc'] = build()
    nc = _CACHE['nc']
    f32 = lambda a: np.ascontiguousarray(np.asarray(a, dtype=np.float32))
    shared = {}
    for k in ('c_ctx', 'w_mod', 'b_mod', 'g_mix', 'g_ffn', 'w_in', 'pool_w', 'pool_scale', 'diff_norm_g',
              'gqa_q_norm', 'gqa_k_norm', 'w_branch', 'w_out', 'w_router_group', 'b_router_group',
              'w_router_expert', 'b_router_expert', 'w_exp_gate', 'w_exp_up', 'w_exp_down', 'g_final'):
        shared[k] = f32(inputs[k])
    shared['diff_lambda'] = f32(inputs['diff_lambda']).reshape(2, 128)
    shared['nat_rpb'] = f32(inputs['nat_rpb']).reshape(2, 60, 31)
    for k, v in consts.items():
        shared[k] = v
    x = f32(inputs['x'])
    ctx = f32(inputs['ctx'])
    c = f32(inputs['c'])
    in_maps = []
    for b in range(8):
        m = dict(shared)
        m['x'] = x[b]
        m['ctx'] = ctx[b]
        m['c'] = c[b]
        in_maps.append(m)
    res = run_bass_kernel_spmd(nc, in_maps, core_ids=list(range(8)))
    return np.stack([np.asarray(r['out'], dtype=np.float32) for r in res.results], axis=0)
```

```python
import math
from contextlib import ExitStack
import numpy as np
import concourse.bass as bass
import concourse.mybir as mybir
from concourse.bass_utils import run_bass_kernel_spmd

F32 = mybir.dt.float32
BF16 = mybir.dt.bfloat16
AF = mybir.ActivationFunctionType
ALU = mybir.AluOpType
AX = mybir.AxisListType

D = 1024
NLAT = 4096
NCTX = 256
NTOK = NLAT + NCTX
NT = NTOK // 128
DEPTH = 2
EPS = 1e-6
NEG = -1e30
SPARSE = True
I32 = mybir.dt.int32
DBG_BARRIER = False


class Sched:
    ENG = ('pe', 'act', 'dve', 'pool', 'sp')

    def __init__(self, nc, es):
        self.nc = nc
        self.es = es
        self.eh = {'pe': nc.tensor, 'act': nc.scalar, 'dve': nc.vector, 'pool': nc.gpsimd, 'sp': nc.sync}
        self.sem = {e: es.enter_context(nc.semaphore("sem_" + e)) for e in self.ENG}
        self.cnt = {e: 0 for e in self.ENG}
        self.seen = {e: {} for e in self.ENG}
        self.lastw = {}
        self.readers = {}
        self.dsem = {}
        self.free_ds = []
        self.nds = 0
        self.keep = set()
        self.keep_res = set()
        self.inputs = set()
        self.nops = 0
        self.nwaits = 0

    def _resolve(self, eng, deps):
        need = {}
        for tok, kind in deps:
            if tok is None:
                continue
            if tok[0] == 'E':
                f, idx = tok[1], tok[2]
                if f == eng and kind != 'raw':
                    continue
                key = ('E', f)
                h = self.sem[f]
            else:
                key = ('D', tok[1])
                idx = tok[2]
                h = tok[3]
            if self.seen[eng].get(key, 0) >= idx:
                continue
            if key not in need or need[key][1] < idx:
                need[key] = (h, idx)
        e = self.eh[eng]
        for key, (h, idx) in need.items():
            e.wait_ge(h, idx)
            self.seen[eng][key] = idx
            self.nwaits += 1

    def _deps(self, r, w):
        deps = []
        for x in r:
            if x in self.lastw:
                deps.append((self.lastw[x], 'raw'))
            elif x[0] not in self.inputs:
                raise KeyError("read of never-written resource %r" % (x,))
        for x in w:
            if x in self.lastw:
                deps.append((self.lastw[x], 'waw'))
            for t in self.readers.get(x, ()):
                deps.append((t, 'war'))
        return deps

    def _commit(self, tok, r, w):
        for x in w:
            self.lastw[x] = tok
            self.readers[x] = []
        for x in r:
            self.readers.setdefault(x, []).append(tok)

    def op(self, eng, fn, r=(), w=()):
        r = list(r)
        w = list(w)
        w += [x for x in r if x[0] == 'ps' and x not in w]
        self._resolve(eng, self._deps(r, w))
        ins = fn(self.eh[eng])
        self.cnt[eng] += 1
        ins.then_inc(self.sem[eng], 1)
        self._commit(('E', eng, self.cnt[eng]), r, w)
        self.nops += 1

    def dma(self, q, out, in_, own, r=(), w=(), chain=True, indirect=None, **kw):
        r = list(r)
        w = list(w)
        if own not in self.dsem:
            fl = [d for d in self.free_ds if d[4] == q]
            if fl:
                self.free_ds.remove(fl[0])
                self.dsem[own] = fl[0]
            else:
                h = self.es.enter_context(self.nc.semaphore("dsem%d" % self.nds))
                self.nds += 1
                self.dsem[own] = [h, 0, None, self.nds, q]
        ds = self.dsem[own]
        deps = self._deps(r, w)
        if chain:
            deps.append((ds[2], 'raw'))
        self._resolve(q, deps)
        if indirect is None:
            self.eh[q].dma_start(out=out, in_=in_, **kw).then_inc(ds[0], 16)
        else:
            kind, idx_ap = indirect
            off = bass.IndirectOffsetOnAxis(ap=idx_ap, axis=0)
            self.nc.gpsimd.indirect_dma_start(out=out, out_offset=(off if kind == 'scatter' else None),
                                              in_=in_, in_offset=(off if kind == 'gather' else None), **kw).then_inc(ds[0], 16)
        ds[1] += 16
        tok = ('D', ds[3], ds[1], ds[0])
        ds[2] = tok
        self._commit(tok, r, w)
        self.nops += 1

    def barrier(self, engines=None):
        engines = engines or self.ENG
        for e in engines:
            deps = [(('E', f, self.cnt[f]), 'raw') for f in self.ENG if f != e and self.cnt[f] > 0]
            for k, ds in list(self.dsem.items()) + [(None, d) for d in self.free_ds]:
                if ds[2] is not None and k not in self.keep:
                    deps.append((ds[2], 'raw'))
            self._resolve(e, deps)

    def phase_end(self):
        self.barrier()
        for k in list(self.dsem.keys()):
            if k not in self.keep:
                self.free_ds.append(self.dsem.pop(k))
        for k in self.lastw:
            if k[0] not in self.keep_res:
                self.lastw[k] = None
        self.readers.clear()


def R(*a):
    return a


def _band_mats():
    out = np.zeros((128, 20, 128), np.float32)
    for g, w in enumerate((2, 4, 8, 16)):
        def mat(J, dl, n):
            m = np.zeros((128, 128), np.float32)
            for q in range(128):
                t = J * 128 + q
                lo = max(t - w // 2, 0)
                hi = min(t + w - w // 2, n)
                for tp in range(lo, hi):
                    p = tp - (J + dl) * 128
                    if 0 <= p < 128:
                        m[p, q] += 1.0 / (hi - lo)
                p = t - (J + dl) * 128
                if 0 <= p < 128:
                    m[p, q] -= 1.0
            return m
        out[:, g * 5 + 0] = mat(5, -1, 1280)
        out[:, g * 5 + 1] = mat(5, 0, 1280)
        out[:, g * 5 + 2] = mat(5, 1, 1280)
        out[:, g * 5 + 3] = mat(0, 0, 1280)
        out[:, g * 5 + 4] = mat(9, 0, 1280)
    return out


def _rope_tabs(half):
    t = np.arange(NLAT)
    inv = (10000.0 ** (-np.arange(half, dtype=np.float32) / half)).astype(np.float32)
    cos = np.zeros((128, 32, 2, half), np.float32)
    sin = np.zeros((128, 32, 2, half), np.float32)
    for a, pos in enumerate((t // 64, t % 64)):
        ang = pos.astype(np.float32)[:, None] * inv[None, :]
        c = np.cos(ang).astype(np.float32).reshape(32, 128, half)
        s = np.sin(ang).astype(np.float32).reshape(32, 128, half)
        cos[:, :, a, :] = c.transpose(1, 0, 2)
        sin[:, :, a, :] = s.transpose(1, 0, 2)
    return cos, sin


def _nat_variant(j, t):
    if 2 <= j <= 29:
        return (t - j) + 2
    if j == 0:
        return 5 + t
    if j == 1:
        return 9 + t
    if j == 30:
        return 13 + (t - 28)
    return 17 + (t - 28)


def _nat_keytiles(j):
    rows = [2 * j, 2 * j + 1]
    lo = min(min(max(r - 4, 0), 56) for r in rows)
    hi = max(min(max(r - 4, 0), 56) + 7 for r in rows)
    return list(range(lo // 2, hi // 2 + 1))


def _nat_plan():
    plan = {}
    mask = np.full((128, 21, 128), NEG, np.float32)
    qc = np.arange(64)
    cs = np.clip(qc - 8, 0, 48)
    kc = np.arange(64)
    colvalid = (kc[None, :] >= cs[:, None]) & (kc[None, :] < cs[:, None] + 16)
    for j in range(32):
        for t in _nat_keytiles(j):
            v = _nat_variant(j, t)
            blocks = {}
            for a in range(2):
                for b in range(2):
                    qr = 2 * j + b
                    kr = 2 * t + a
                    rs = min(max(qr - 4, 0), 56)
                    ok = rs <= kr < rs + 8
                    blocks[(a, b)] = (kr - qr + 7) if ok else None
                    if ok:
                        m = np.where(colvalid, 0.0, NEG).astype(np.float32)
                        mask[b * 64:(b + 1) * 64, v, a * 64:(a + 1) * 64] = m[::-1, :]
            if v in plan:
                assert plan[v] == blocks
            plan[v] = blocks
    return plan, mask


def _host_consts():
    c = {}
    c['identf'] = np.eye(128, dtype=np.float32)
    anti = np.zeros((128, 128), np.float32)
    for b in range(2):
        for q in range(64):
            anti[b * 64 + 63 - q, b * 64 + q] = 1.0
    c['antii'] = anti
    c['band'] = _band_mats()
    c['cosg'], c['sing'] = _rope_tabs(16)
    c['cosd'], c['sind'] = _rope_tabs(8)
    _, c['natmask'] = _nat_plan()
    c['ltri'] = np.triu(np.ones((128, 128), np.float32), 1)
    c['ustrict'] = np.triu(np.ones((32, 32), np.float32), 1)
    c['thr'] = np.tile((128.0 * np.arange(100, dtype=np.float32))[None, :], (128, 1))
    c['iota'] = np.arange(128, dtype=np.float32).reshape(128, 1)
    return c


def build(stop=None, dbg=False):
    nc = bass.Bass("TRN2", target_bir_lowering=False)

    def din(name, shape, dt=F32):
        return nc.dram_tensor(name, list(shape), dt, kind="ExternalInput").ap()

    def dscr(name, shape, dt=F32):
        return nc.dram_tensor(name, list(shape), dt, kind=("ExternalOutput" if dbg else "Internal")).ap()

    x_in = din("x", [NLAT, D])
    ctx_in = din("ctx", [NCTX, D])
    c_in = din("c", [D])
    cctx_in = din("c_ctx", [D])
    w_mod = din("w_mod", [2, D, 6 * D])
    b_mod = din("b_mod", [2, 6 * D])
    g_mix = din("g_mix", [2, D])
    g_ffn = din("g_ffn", [2, D])
    w_in = din("w_in", [2, D, 6400])
    pool_w = din("pool_w", [2, 4, 64, 64])
    pool_scale = din("pool_scale", [2, 256])
    diff_lambda = din("diff_lambda", [2, 128])
    diff_norm_g = din("diff_norm_g", [2, 64])
    nat_rpb = din("nat_rpb", [2, 60, 31])
    gqa_q_norm = din("gqa_q_norm", [2, 64])
    gqa_k_norm = din("gqa_k_norm", [2, 64])
    w_branch = din("w_branch", [2, 4, 256, D])
    w_out = din("w_out", [2, D, D])
    w_rg = din("w_router_group", [2, D, 4])
    b_rg = din("b_router_group", [2, 4])
    w_re = din("w_router_expert", [2, D, 32])
    b_re = din("b_router_expert", [2, 32])
    w_eg = din("w_exp_gate", [2, 32, D, 512])
    w_eu = din("w_exp_up", [2, 32, D, 512])
    w_ed = din("w_exp_down", [2, 32, 512, D])
    g_final = din("g_final", [D])
    identf_in = din("identf", [128, 128])
    antii_in = din("antii", [128, 128])
    band_in = din("band", [128, 20, 128])
    cosg_in = din("cosg", [128, 32, 2, 16])
    sing_in = din("sing", [128, 32, 2, 16])
    cosd_in = din("cosd", [128, 32, 2, 8])
    sind_in = din("sind", [128, 32, 2, 8])
    natmask_in = din("natmask", [128, 21, 128])
    ltri_in = din("ltri", [128, 128])
    ustrict_in = din("ustrict", [32, 32])
    thr_in = din("thr", [128, 100])
    iota_in = din("iota", [128, 1])

    out_d = nc.dram_tensor("out", [NLAT, D], F32, kind="ExternalOutput").ap()
    xs_d = dscr("xs", [NTOK, D])
    hT_d = dscr("hT", [8, 128, NTOK], BF16)
    brT_d = dscr("brT", [4, 4, 64, NTOK], BF16)
    mT_d = dscr("mT", [8, 128, NTOK], BF16)
    modd = dscr("modd", [2, 2, 6 * D])
    rpbpad = dscr("rpbpad", [2, 60, 128])
    wbf_d = [nc.dram_tensor("wbf%d" % i, [32 * 128, 12288], BF16, kind="Internal").ap() for i in range(2)]
    xslot_d = nc.dram_tensor("xslot", [100 * 128, D], BF16, kind="Internal").ap()
    yslot_d = nc.dram_tensor("yslot", [100 * 128, D], F32, kind="Internal").ap()

    nat_plan, _ = _nat_plan()

    def kind_of(tile):
        return 0 if tile < 32 else 1

    GROUPS = [list(range(g * 4, g * 4 + 4)) for g in range(8)] + [[32, 33]]

    def x_src(l, tiles, after_mix=False):
        t0 = tiles[0]
        n = len(tiles)
        if l == 0 and not after_mix:
            if t0 < 32:
                return x_in[t0 * 128:(t0 + n) * 128, :].rearrange("(t p) d -> p t d", p=128)
            return ctx_in[(t0 - 32) * 128:(t0 - 32 + n) * 128, :].rearrange("(t p) d -> p t d", p=128)
        return xs_d[t0 * 128:(t0 + n) * 128, :].rearrange("(t p) d -> p t d", p=128)

    def x_res(l, tiles, after_mix=False):
        if l == 0 and not after_mix:
            return [R('xin', t) for t in tiles]
        return [R('xs', t) for t in tiles]

    with ExitStack() as es:
        S = Sched(nc, es)
        S.inputs.add('xin')

        _tn = [0]

        def T(ctx, name, shape, dt):
            _tn[0] += 1
            return ctx.enter_context(nc.sbuf_tensor("%s_%d" % (name, _tn[0]), list(shape), dt))

        def mk(t, p0, pn, off, dims):
            row = 1
            for s in t.shape[1:]:
                row *= int(s)
            return bass.AP(t, p0 * row + off, [[row, pn]] + [list(d) for d in dims])

        ps = [es.enter_context(nc.psum_tensor("ps%d" % i, [128, 512], F32)) for i in range(8)]
        psb = [p.bitcast(BF16) for p in ps]

        def PR(i):
            return R('ps', i)

        identF = T(es, "identF", [128, 128], F32)
        identB = T(es, "identB", [128, 128], BF16)
        antiB = T(es, "antiB", [128, 128], BF16)
        colsP = T(es, "colsP", [128, 64], F32)
        cols64 = T(es, "cols64", [64, 8], F32)
        sT = T(es, "sT", [128, 8, 2], F32)
        modP = T(es, "modP", [128, 2, 2, 4, 8], F32)
        nlam = T(es, "nlam", [128, 2], F32)
        gainG = T(es, "gainG", [128, 2, 8, 64], F32)
        gB = T(es, "gB", [128, 2, 64], F32)
        epsT = T(es, "epsT", [128, 1], F32)
        mhalf = T(es, "mhalf", [128, 16], F32)

        S.dma('sp', identF[:], identf_in, own=R('identF'), w=[R('identF')])
        S.dma('pool', identB[:], identf_in, own=R('identB'), w=[R('identB')])
        S.dma('pool', antiB[:], antii_in, own=R('antiB'), w=[R('antiB')])

        with ExitStack() as ph:
            rowsA = T(ph, "rowsA", [52, 128], F32)
            rows64 = T(ph, "rows64", [8, 64], F32)
            S.dma('sp', rowsA[0:8, :], c_in.rearrange("(r d) -> r d", d=128), own=R('rowsA'), w=[R('rowsA')])
            S.dma('sp', rowsA[8:16, :], cctx_in.rearrange("(r d) -> r d", d=128), own=R('rowsA'), w=[R('rowsA')])
            for l in range(2):
                b0 = 16 + l * 18
                S.dma('sp', rowsA[b0:b0 + 8, :], g_mix[l].rearrange("(r d) -> r d", d=128), own=R('rowsA'), w=[R('rowsA')])
                S.dma('sp', rowsA[b0 + 8:b0 + 16, :], g_ffn[l].rearrange("(r d) -> r d", d=128), own=R('rowsA'), w=[R('rowsA')])
                S.dma('sp', rowsA[b0 + 16:b0 + 18, :], pool_scale[l].rearrange("(r d) -> r d", d=128), own=R('rowsA'), w=[R('rowsA')])
                S.dma('sp', rows64[l * 4:l * 4 + 4, :], pool_scale[l].rearrange("(r d) -> r d", d=64), own=R('rows64'), w=[R('rows64')])
            S.op('pe', lambda e: e.transpose(ps[0][:, 0:52], rowsA[0:52, :], identF[0:52, 0:52]),
                 r=[R('rowsA'), R('identF')], w=[PR(0)])
            S.op('dve', lambda e: e.tensor_copy(colsP[:, 0:52], ps[0][:, 0:52]), r=[PR(0)], w=[R('colsP')])
            S.op('pe', lambda e: e.transpose(ps[1][0:64, 0:8], rows64[0:8, :], identF[0:8, 0:8]),
                 r=[R('rows64'), R('identF')], w=[PR(1)])
            S.op('dve', lambda e: e.tensor_copy(cols64[:, :], ps[1][0:64, 0:8]), r=[PR(1)], w=[R('cols64')])
            S.op('dve', lambda e: e.memset(epsT[:], EPS), w=[R('epsT')])
            S.op('pool', lambda e: e.memset(mhalf[:], -0.5), w=[R('mhalf')])
            S.op('act', lambda e: e.activation(mk(sT, 0, 128, 0, [[1, 2], [2, 8]]),
                                               colsP[:, 0:16].rearrange("p (a k) -> p a k", a=2), AF.Silu),
                 r=[R('colsP')], w=[R('sT')])

            wm = [T(ph, "wm%d" % i, [128, 8, 512], F32) for i in range(2)]
            bm = T(ph, "bm", [2, 6 * D], F32)
            modsb = T(ph, "modsb", [2, 6 * D], F32)
            raw = T(ph, "raw", [128, 48, 2], F32)
            dl = T(ph, "dl", [128, 128], F32)
            pr = T(ph, "pr", [128, 2, 32], F32)
            sm = T(ph, "sm", [128, 2], F32)
            for l in range(2):
                for kk in range(2):
                    S.dma('sp', bm[kk:kk + 1, :], b_mod[l:l + 1, :], own=R('bm'), w=[R('bm')])
                for n in range(12):
                    wb = wm[n % 2]
                    S.dma('sp', wb[:], w_mod[l][:, n * 512:(n + 1) * 512].rearrange("(k p) n -> p k n", p=128),
                          own=R('wm', n % 2), w=[R('wm', n % 2)])
                    pb = 2 + (n % 2)
                    for k in range(8):
                        S.op('pe', lambda e: e.matmul(ps[pb][0:2, :], lhsT=sT[:, k, :], rhs=wb[:, k, :],
                                                      start=(k == 0), stop=(k == 7)),
                             r=[R('sT'), R('wm', n % 2)], w=[PR(pb)])
                    S.op('dve', lambda e: e.tensor_tensor(modsb[0:2, n * 512:(n + 1) * 512], ps[pb][0:2, :],
                                                          bm[0:2, n * 512:(n + 1) * 512], ALU.add),
                         r=[PR(pb), R('bm')], w=[R('modsb')])
                S.dma('sp', modd[l], modsb[:], own=R('modsb'), r=[R('modsb')], w=[R('modd', l)])
                for j in range(48):
                    S.op('pe', lambda e: e.transpose(ps[4][:, j * 2:(j + 1) * 2], modsb[0:2, j * 128:(j + 1) * 128],
                                                     identF[0:2, 0:2]),
                         r=[R('modsb'), R('identF')], w=[PR(4)])
                S.op('dve', lambda e: e.tensor_copy(raw[:].rearrange("p j k -> p (j k)"), ps[4][:, 0:96]),
                     r=[PR(4)], w=[R('raw')])
                b0 = 16 + l * 18
                for kd in range(2):
                    S.op('dve', lambda e: e.scalar_tensor_tensor(modP[:, l, kd, 0, :], raw[:, 8:16, kd], 1.0,
                                                                 colsP[:, b0:b0 + 8], ALU.add, ALU.mult),
                         r=[R('raw'), R('colsP')], w=[R('modP')])
                    S.op('dve', lambda e: e.tensor_copy(modP[:, l, kd, 1, :], raw[:, 0:8, kd]), r=[R('raw')], w=[R('modP')])
                    S.op('dve', lambda e: e.scalar_tensor_tensor(modP[:, l, kd, 2, :], raw[:, 32:40, kd], 1.0,
                                                                 colsP[:, b0 + 8:b0 + 16], ALU.add, ALU.mult),
                         r=[R('raw'), R('colsP')], w=[R('modP')])
                    S.op('dve', lambda e: e.tensor_copy(modP[:, l, kd, 3, :], raw[:, 24:32, kd]), r=[R('raw')], w=[R('modP')])
                lam_init = 0.8 - 0.6 * math.exp(-0.3 * l)
                S.dma('sp', dl[:], bass.AP(diff_lambda.tensor, diff_lambda[l].offset, [[0, 128], [1, 128]]),
                      own=R('dl'), w=[R('dl')])
                S.op('dve', lambda e: e.tensor_tensor(pr[:], mk(dl, 0, 128, 0, [[64, 2], [1, 32]]),
                                                      mk(dl, 0, 128, 32, [[64, 2], [1, 32]]), ALU.mult),
                     r=[R('dl')], w=[R('pr')])
                S.op('dve', lambda e: e.reduce_sum(sm[:], pr[:], axis=AX.X), r=[R('pr')], w=[R('sm')])
                S.op('act', lambda e: e.activation(sm[:], sm[:], AF.Exp), r=[R('sm')], w=[R('sm')])
                S.op('dve', lambda e: e.tensor_tensor(nlam[:, l:l + 1], sm[:, 1:2], sm[:, 0:1], ALU.subtract),
                     r=[R('sm')], w=[R('nlam')])
                S.op('dve', lambda e: e.tensor_scalar(nlam[:, l:l + 1], nlam[:, l:l + 1], -lam_init, None, ALU.add),
                     r=[R('nlam')], w=[R('nlam')])
                S.dma('sp', gainG[:, l, 0:4, :], bass.AP(gqa_q_norm.tensor, gqa_q_norm[l].offset, [[0, 128], [0, 4], [1, 64]]),
                      own=R('gainG'), w=[R('gainG')])
                S.dma('sp', gainG[:, l, 4:8, :], bass.AP(gqa_k_norm.tensor, gqa_k_norm[l].offset, [[0, 128], [0, 4], [1, 64]]),
                      own=R('gainG'), w=[R('gainG')])
                S.dma('sp', gB[:, l, :], bass.AP(diff_norm_g.tensor, diff_norm_g[l].offset, [[0, 128], [1, 64]]),
                      own=R('gB'), w=[R('gB')])
                S.op('dve', lambda e: e.tensor_scalar(gB[:, l, :], gB[:, l, :], 1.0 - lam_init, None, ALU.mult),
                     r=[R('gB')], w=[R('gB')])
            S.phase_end()

        def norm_mod_T(l, which, tiles, xt, junk, diag, ssb, outB, out32, pbanks, srcap, srcres):
            norm_p1(tiles, xt, junk, diag, ssb, srcap, srcres)
            norm_p2(l, which, tiles, xt, diag, outB, out32, pbanks)

        def norm_p1(tiles, xt, junk, diag, ssb, srcap, srcres):
            n = len(tiles)
            S.dma('sp', xt[:, 0:n, :], srcap, own=R(xt.name), r=srcres, w=[R(xt.name)])
            for t in range(n):
                S.op('act', lambda e: e.activation(junk[:], xt[:, t, :], AF.Square, accum_out=ssb[:, t:t + 1]),
                     r=[R(xt.name)], w=[R(junk.name), R(ssb.name, t)])
            S.op('dve', lambda e: e.tensor_scalar(ssb[:, 4:4 + n], ssb[:, 0:n], 1.0 / D, EPS, ALU.mult, ALU.add),
                 r=[R(ssb.name, t) for t in range(n)], w=[R(ssb.name, 'm')])
            S.op('pool', lambda e: e.tensor_tensor(ssb[:, 8:8 + n], ssb[:, 4:4 + n], mhalf[:, 0:n], ALU.pow),
                 r=[R(ssb.name, 'm'), R('mhalf')], w=[R(ssb.name, 'r')])
            for t in range(n):
                S.op('pool' if t % 2 else 'dve',
                     lambda e: e.tensor_scalar(diag[:, t, :], identF[:], ssb[:, 8 + t:9 + t], None, ALU.mult),
                     r=[R(ssb.name, 'r'), R('identF')], w=[R(diag.name, t)])

        def norm_p2(l, which, tiles, xt, diag, outB, out32, pbanks):
            n = len(tiles)
            ntok = n * 128
            kd = kind_of(tiles[0])
            a_i = 0 if which == 1 else 2
            for c in range(8):
                pb = pbanks[c % len(pbanks)]
                for t in range(n):
                    S.op('pe', lambda e: e.matmul(ps[pb][:, t * 128:(t + 1) * 128], lhsT=xt[:, t, c * 128:(c + 1) * 128],
                                                  rhs=diag[:, t, :], start=True, stop=True),
                         r=[R(xt.name), R(diag.name, t)], w=[PR(pb)])
                A = modP[:, l, kd, a_i, c:c + 1]
                B = modP[:, l, kd, a_i + 1, c:c + 1]
                if outB is None:
                    o32, o32res = out32(c)
                    S.op('act' if c % 2 else 'dve',
                         (lambda e: e.activation(o32, ps[pb][:, 0:ntok], AF.Identity, bias=B, scale=A)) if c % 2 else
                         (lambda e: e.tensor_scalar(o32, ps[pb][:, 0:ntok], A, B, ALU.mult, ALU.add)),
                         r=[PR(pb), R('modP')], w=o32res)
                    continue
                ob, obres = outB(c)
                if out32 is None:
                    if c % 2 == 0:
                        S.op('dve', lambda e: e.tensor_scalar(ob, ps[pb][:, 0:ntok], A, B, ALU.mult, ALU.add),
                             r=[PR(pb), R('modP')], w=obres)
                    else:
                        S.op('act', lambda e: e.activation(ob, ps[pb][:, 0:ntok], AF.Identity, bias=B, scale=A),
                             r=[PR(pb), R('modP')], w=obres)
                else:
                    o32, o32res = out32(c)
                    S.op('dve', lambda e: e.tensor_scalar(ob, ps[pb][:, 0:ntok], A, B, ALU.mult, ALU.add),
                         r=[PR(pb), R('modP')], w=obres)
                    S.op('act', lambda e: e.activation(o32, ps[pb][:, 0:ntok], AF.Identity, bias=B, scale=A),
                         r=[PR(pb), R('modP')], w=o32res)

        def hT_view(tok0, ntok):
            return hT_d[:, :, tok0:tok0 + ntok].rearrange("c p t -> p c t")


        S.keep.add(R('wbfsem'))
        S.keep_res.add('wbf')

        def issue_casts(l):
            for e_ in range(32):
                rows = wbf_d[l][e_ * 128:(e_ + 1) * 128, :]
                S.dma('pool', rows[:, 0:4096].rearrange("p (k n) -> p k n", k=8),
                      w_eg[l, e_].rearrange("(k p) n -> p k n", p=128), own=R('wbfsem'), chain=False, w=[R('wbf', l, e_, 0)])
                S.dma('pool', rows[:, 4096:8192].rearrange("p (k n) -> p k n", k=8),
                      w_eu[l, e_].rearrange("(k p) n -> p k n", p=128), own=R('wbfsem'), chain=False, w=[R('wbf', l, e_, 1)])
                S.dma('pool', rows[:, 8192:12288].rearrange("p (k n) -> p k n", k=4),
                      w_ed[l, e_].rearrange("(k p) n -> p k n", p=128), own=R('wbfsem'), chain=False, w=[R('wbf', l, e_, 2)])

        def moe_sparse(l, need_ctx):
            last = (l == DEPTH - 1)
            moe_tiles = list(range(34)) if need_ctx else list(range(32))
            NTt = len(moe_tiles)
            NS = 100 if need_ctx else 96
            wbf_res = [R('wbf', l, e_, m) for e_ in range(32) for m in range(3)]
            with ExitStack() as ph:
                OHs = T(ph, "OHs", [128, NTt, 2, 32], F32)
                W12 = T(ph, "W12", [128, NTt, 2], F32)
                Pall = T(ph, "Pall", [128, NTt, 32], F32)
                posF = T(ph, "posF", [128, NTt, 2], F32)
                posI = T(ph, "posI", [128, NTt, 2], I32)
                idxW = T(ph, "idxW", [128, NS], I32)
                gt2 = T(ph, "gt2", [128, 2, D], F32)
                for kd in range(2):
                    S.dma('sp', gt2[:, kd, :], bass.AP(modd.tensor, modd[l, kd, 5 * D:6 * D].offset, [[0, 128], [1, D]]),
                          own=R('gt2'), r=[R('modd', l)], w=[R('gt2')])
                with ExitStack() as pa:
                    xt = T(pa, "xtm", [128, 4, D], F32)
                    junk = T(pa, "junkm", [128, D], BF16)
                    diag = T(pa, "diagm", [128, 4, 128], F32)
                    ssb = T(pa, "ssbm", [128, 12], F32)
                    tT32 = T(pa, "tT32", [128, 8, 512], F32)
                    wr32 = T(pa, "wr32", [128, 8, 36], F32)
                    brt = T(pa, "brt", [128, 36], F32)
                    A2b = T(pa, "A2b", [128, 2, D], F32)
                    B2b = T(pa, "B2b", [128, 2, D], F32)
                    ttok = T(pa, "ttok", [128, NTt, D], BF16)
                    tmpf = [T(pa, "tmpf%d" % i, [128, D], F32) for i in range(2)]
                    lg = [T(pa, "lg%d" % i, [128, 36], F32) for i in range(2)]
                    rt = [T(pa, "rt%d" % i, [128, 128], F32) for i in range(2)]
                    Cb = [T(pa, "Cb%d" % i, [128, 32], BF16) for i in range(2)]
                    Ccum = [T(pa, "Ccum%d" % i, [128, 32], BF16) for i in range(2)]
                    ltri = T(pa, "ltri", [128, 128], BF16)
                    onesB = T(pa, "onesB", [128, 128], BF16)
                    ustr = T(pa, "ustr", [32, 32], F32)
                    thr = T(pa, "thr", [128, 100], F32)
                    iota = T(pa, "iota", [128, 1], F32)
                    zt = T(pa, "zt", [128, D], BF16)
                    ncol = T(pa, "ncol", [32, 4], F32)
                    cmp32 = T(pa, "cmp32", [32, 100], F32)
                    npbc = T(pa, "npbc", [32, 128], F32)
                    offs = T(pa, "offs", [128, 32], F32)
                    cmpw = T(pa, "cmpw", [128, NS, 32], F32)
                    cntw = T(pa, "cntw", [128, NS], F32)
                    prod = T(pa, "prod", [128, NTt, 2, 32], F32)
                    onesF = T(pa, "onesF", [32, 1], F32)
                    totc = T(pa, "totc", [128, 1], F32)
                    unused = T(pa, "unused", [128, NS], F32)
                    S.op('dve', lambda e: e.memset(onesF[:], 1.0), w=[R('onesF')])
                    S.dma('sp', wr32[:, :, 0:4], w_rg[l].rearrange("(k p) n -> p k n", p=128), own=R('wr32'), w=[R('wr32')])
                    S.dma('sp', wr32[:, :, 4:36], w_re[l].rearrange("(k p) n -> p k n", p=128), own=R('wr32'), w=[R('wr32')])
                    S.dma('sp', brt[:, 0:4], bass.AP(b_rg.tensor, b_rg[l].offset, [[0, 128], [1, 4]]), own=R('brt'), w=[R('brt')])
                    S.dma('sp', brt[:, 4:36], bass.AP(b_re.tensor, b_re[l].offset, [[0, 128], [1, 32]]), own=R('brt'), w=[R('brt')])
                    S.dma('pool', ltri[:], ltri_in, own=R('ltri'), w=[R('ltri')])
                    S.dma('sp', ustr[:], ustrict_in, own=R('ustr'), w=[R('ustr')])
                    S.dma('sp', thr[:], thr_in, own=R('thr'), w=[R('thr')])
                    S.dma('sp', iota[:], iota_in, own=R('iota'), w=[R('iota')])
                    S.op('pool', lambda e: e.memset(onesB[:], 1.0), w=[R('onesB')])
                    S.op('pool', lambda e: e.memset(zt[:], 0.0), w=[R('zt')])
                    S.op('pool', lambda e: e.memset(Ccum[0][:], 0.0), w=[R(Ccum[0].name)])
                    S.dma('sp', xslot_d[0:NS * 128, :].rearrange("(i p) d -> p i d", p=128),
                          mk(zt, 0, 128, 0, [[0, NS], [1, D]]), own=R('zt'), r=[R('zt')], w=[R('xslot0')])
                    for kd in range(2):
                        S.dma('sp', A2b[:, kd, :], bass.AP(modd.tensor, modd[l, kd, 4 * D:5 * D].offset, [[0, 128], [1, D]]),
                              own=R('A2b'), r=[R('modd', l)], w=[R('A2b')])
                        S.dma('sp', B2b[:, kd, :], bass.AP(modd.tensor, modd[l, kd, 3 * D:4 * D].offset, [[0, 128], [1, D]]),
                              own=R('B2b'), r=[R('modd', l)], w=[R('B2b')])
                    S.dma('sp', tmpf[0][:], bass.AP(g_ffn.tensor, g_ffn[l].offset, [[0, 128], [1, D]]), own=R(tmpf[0].name), w=[R(tmpf[0].name)])
                    for kd in range(2):
                        S.op('dve', lambda e: e.scalar_tensor_tensor(A2b[:, kd, :], A2b[:, kd, :], 1.0, tmpf[0][:], ALU.add, ALU.mult),
                             r=[R('A2b'), R(tmpf[0].name)], w=[R('A2b')])
                    chunks = []
                    cur = []
                    for t in moe_tiles:
                        if cur and (len(cur) == 4 or kind_of(cur[0]) != kind_of(t)):
                            chunks.append(cur)
                            cur = []
                        cur.append(t)
                    chunks.append(cur)
                    for ch in chunks:
                        n = len(ch)
                        ntok = n * 128
                        kd = kind_of(ch[0])
                        norm_mod_T(l, 2, ch, xt, junk, diag, ssb, None,
                                   lambda c: (tT32[:, c, 0:ntok], [R('tT32', c)]),
                                   [0, 1, 2, 3], x_src(l, ch, True), x_res(l, ch, True))
                        for ti, tile in enumerate(ch):
                            lt = tile
                            b = lt % 2
                            S.op('dve', lambda e: e.scalar_tensor_tensor(tmpf[b][:], xt[:, ti, :], ssb[:, 8 + ti:9 + ti], A2b[:, kd, :], ALU.mult, ALU.mult),
                                 r=[R(xt.name), R(ssb.name, 'r'), R('A2b')], w=[R(tmpf[b].name)])
                            S.op('pool', lambda e: e.tensor_tensor(ttok[:, lt, :], tmpf[b][:], B2b[:, kd, :], ALU.add),
                                 r=[R(tmpf[b].name), R('B2b')], w=[R('ttok', lt)])
                            L, Rt = lg[b], rt[b]
                            for k in range(8):
                                S.op('pe', lambda e: e.matmul(ps[4 + b][:, 0:36], lhsT=tT32[:, k, ti * 128:(ti + 1) * 128], rhs=wr32[:, k, :],
                                                              start=(k == 0), stop=(k == 7)),
                                     r=[R('tT32', k), R('wr32')], w=[PR(4 + b)])
                            S.op('dve', lambda e: e.tensor_tensor(L[:], ps[4 + b][:, 0:36], brt[:], ALU.add), r=[PR(4 + b), R('brt')], w=[R(L.name)])
                            oh1 = OHs[:, lt, 0, :]
                            oh2 = OHs[:, lt, 1, :]
                            S.op('dve', lambda e: e.reduce_max(Rt[:, 0:1], L[:, 0:4], axis=AX.X), r=[R(L.name)], w=[R(Rt.name, 0)])
                            S.op('dve', lambda e: e.tensor_scalar(Rt[:, 1:2], Rt[:, 0:1], -1.0, None, ALU.mult), r=[R(Rt.name, 0)], w=[R(Rt.name, 1)])
                            S.op('act', lambda e: e.activation(Rt[:, 12:16], L[:, 0:4], AF.Exp, bias=Rt[:, 1:2], scale=1.0, accum_out=Rt[:, 2:3]),
                                 r=[R(L.name), R(Rt.name, 1)], w=[R(Rt.name, 2)])
                            S.op('dve', lambda e: e.reciprocal(Rt[:, 3:4], Rt[:, 2:3]), r=[R(Rt.name, 2)], w=[R(Rt.name, 3)])
                            S.op('dve', lambda e: e.tensor_scalar(Rt[:, 4:8], L[:, 0:4], Rt[:, 0:1], None, ALU.is_equal),
                                 r=[R(L.name), R(Rt.name, 0)], w=[R(Rt.name, 4)])
                            S.op('dve', lambda e: e.tensor_scalar(Rt[:, 8:12], Rt[:, 4:8], -1.0, 1e30, ALU.add, ALU.mult),
                                 r=[R(Rt.name, 4)], w=[R(Rt.name, 5)])
                            S.op('dve', lambda e: e.tensor_tensor(Rt[:, 16:48].rearrange("p (g k) -> p g k", g=4),
                                                                  L[:, 4:36].rearrange("p (g k) -> p g k", g=4),
                                                                  mk(Rt, 0, 128, 8, [[1, 4], [0, 8]]), ALU.add),
                                 r=[R(L.name), R(Rt.name, 5)], w=[R(Rt.name, 6)])
                            S.op('dve', lambda e: e.reduce_max(Rt[:, 48:49], Rt[:, 16:48], axis=AX.X), r=[R(Rt.name, 6)], w=[R(Rt.name, 7)])
                            S.op('dve', lambda e: e.tensor_scalar(oh1, Rt[:, 16:48], Rt[:, 48:49], None, ALU.is_equal),
                                 r=[R(Rt.name, 6), R(Rt.name, 7)], w=[R('oh', lt, 0)])
                            S.op('dve', lambda e: e.scalar_tensor_tensor(Rt[:, 88:120], oh1, -1e30, Rt[:, 16:48], ALU.mult, ALU.add),
                                 r=[R('oh', lt, 0), R(Rt.name, 6)], w=[R(Rt.name, 9)])
                            S.op('dve', lambda e: e.reduce_max(Rt[:, 49:50], Rt[:, 88:120], axis=AX.X), r=[R(Rt.name, 9)], w=[R(Rt.name, 10)])
                            S.op('dve', lambda e: e.tensor_scalar(oh2, Rt[:, 88:120], Rt[:, 49:50], None, ALU.is_equal),
                                 r=[R(Rt.name, 9), R(Rt.name, 10)], w=[R('oh', lt, 1)])
                            S.op('dve', lambda e: e.tensor_scalar(Rt[:, 50:51], Rt[:, 48:49], -1.0, None, ALU.mult), r=[R(Rt.name, 7)], w=[R(Rt.name, 12)])
                            S.op('act', lambda e: e.activation(Rt[:, 51:52], Rt[:, 49:50], AF.Exp, bias=Rt[:, 50:51], scale=1.0),
                                 r=[R(Rt.name, 10), R(Rt.name, 12)], w=[R(Rt.name, 13)])
                            S.op('dve', lambda e: e.tensor_scalar(Rt[:, 52:53], Rt[:, 51:52], 1.0, None, ALU.add), r=[R(Rt.name, 13)], w=[R(Rt.name, 14)])
                            S.op('dve', lambda e: e.reciprocal(Rt[:, 53:54], Rt[:, 52:53]), r=[R(Rt.name, 14)], w=[R(Rt.name, 15)])
                            S.op('dve', lambda e: e.tensor_tensor(W12[:, lt, 0:1], Rt[:, 53:54], Rt[:, 3:4], ALU.mult),
                                 r=[R(Rt.name, 15), R(Rt.name, 3)], w=[R('w12', lt, 0)])
                            S.op('dve', lambda e: e.tensor_tensor(W12[:, lt, 1:2], W12[:, lt, 0:1], Rt[:, 51:52], ALU.mult),
                                 r=[R('w12', lt, 0), R(Rt.name, 13)], w=[R('w12', lt, 1)])
                            cb = Cb[b]
                            cprev, cnext = Ccum[lt % 2], Ccum[(lt + 1) % 2]
                            S.op('dve', lambda e: e.tensor_tensor(cb[:], oh1, oh2, ALU.add), r=[R('oh', lt, 0), R('oh', lt, 1)], w=[R(cb.name)])
                            S.op('pe', lambda e: e.matmul(ps[6][:, 0:32], lhsT=ltri[:], rhs=cb[:], start=True, stop=False),
                                 r=[R('ltri'), R(cb.name)], w=[PR(6)])
                            S.op('pe', lambda e: e.matmul(ps[6][:, 0:32], lhsT=onesB[:], rhs=cprev[:], start=False, stop=True),
                                 r=[R('onesB'), R(cprev.name)], w=[PR(6)])
                            S.op('act', lambda e: e.activation(Pall[:, lt, :], ps[6][:, 0:32], AF.Copy), r=[PR(6)], w=[R('Pall', lt)])
                            S.op('pool', lambda e: e.tensor_tensor(cnext[:], cprev[:], cb[:], ALU.add),
                                 r=[R(cprev.name), R(cb.name)], w=[R(cnext.name)])
                    cfin = Ccum[NTt % 2]
                    S.op('pe', lambda e: e.matmul(ps[7][0:32, 0:1], lhsT=cfin[:], rhs=onesB[:, 0:1], start=True, stop=True),
                         r=[R(cfin.name), R('onesB')], w=[PR(7)])
                    S.op('dve', lambda e: e.tensor_copy(ncol[:, 0:1], ps[7][0:32, 0:1]), r=[PR(7)], w=[R('ncol', 0)])
                    S.op('dve', lambda e: e.tensor_scalar(cmp32[:], thr[0:32, :], ncol[:, 0:1], None, ALU.is_lt),
                         r=[R('thr'), R('ncol', 0)], w=[R('cmp32')])
                    S.op('dve', lambda e: e.reduce_sum(ncol[:, 1:2], cmp32[:], axis=AX.X), r=[R('cmp32')], w=[R('ncol', 1)])
                    S.op('dve', lambda e: e.tensor_scalar(ncol[:, 2:3], ncol[:, 1:2], 128.0, None, ALU.mult), r=[R('ncol', 1)], w=[R('ncol', 2)])
                    S.op('dve', lambda e: e.tensor_copy(npbc[:], mk(ncol, 0, 32, 2, [[0, 128]])), r=[R('ncol', 2)], w=[R('npbc')])
                    S.op('pe', lambda e: e.matmul(ps[7][:, 32:64], lhsT=npbc[:], rhs=ustr[:], start=True, stop=True),
                         r=[R('npbc'), R('ustr')], w=[PR(7)])
                    S.op('dve', lambda e: e.tensor_copy(offs[:], ps[7][:, 32:64]), r=[PR(7)], w=[R('offs')])
                    S.op('dve', lambda e: e.tensor_tensor(cmpw[:], mk(offs, 0, 128, 0, [[0, NS], [1, 32]]),
                                                          mk(thr, 0, 128, 0, [[1, NS], [0, 32]]), ALU.is_le),
                         r=[R('offs'), R('thr')], w=[R('cmpw')])
                    S.op('dve', lambda e: e.reduce_sum(cntw[:], cmpw[:], axis=AX.X), r=[R('cmpw')], w=[R('cntw')])
                    S.op('dve', lambda e: e.tensor_scalar(cntw[:], cntw[:], -1.0, 128.0, ALU.add, ALU.mult), r=[R('cntw')], w=[R('cntw')])
                    S.op('dve', lambda e: e.tensor_scalar(cntw[:], cntw[:], iota[:, 0:1], None, ALU.add), r=[R('cntw'), R('iota')], w=[R('cntw')])
                    S.op('dve', lambda e: e.tensor_copy(idxW[:], cntw[:]), r=[R('cntw')], w=[R('idxW')])
                    S.op('dve', lambda e: e.tensor_tensor(Pall[:], Pall[:], mk(offs, 0, 128, 0, [[0, NTt], [1, 32]]), ALU.add),
                         r=[R('Pall', t) for t in range(NTt)] + [R('offs')], w=[R('Pall2')])
                    S.op('dve', lambda e: e.tensor_tensor(prod[:], OHs[:], mk(Pall, 0, 128, 0, [[32, NTt], [0, 2], [1, 32]]), ALU.mult),
                         r=[R('Pall2')] + [R('oh', t, k) for t in range(NTt) for k in range(2)], w=[R('prod')])
                    S.op('dve', lambda e: e.reduce_sum(posF[:].rearrange("p t k -> p (t k)"), prod[:].rearrange("p t k e -> p (t k) e"), axis=AX.X),
                         r=[R('prod')], w=[R('posF')])
                    S.op('dve', lambda e: e.tensor_copy(posI[:], posF[:]), r=[R('posF')], w=[R('posI')])
                    for lt in range(NTt):
                        for k in range(2):
                            S.dma('pool', xslot_d[:, :], ttok[:, lt, :], own=R('scat', (lt * 2 + k) % 4),
                                  r=[R('ttok', lt), R('posI'), R('xslot0')], w=[R('xslot', lt, k)],
                                  indirect=('scatter', posI[:, lt, k:k + 1]))
                    S.barrier()
                xslot_res = [R('xslot', lt, k) for lt in range(NTt) for k in range(2)]
                with ExitStack() as pb_:
                    wt = [T(pb_, "wt%d" % i, [128, 12288], BF16) for i in range(2)]
                    xs = [T(pb_, "xs%d" % i, [128, D], BF16) for i in range(2)]
                    xT = [T(pb_, "xT%d" % i, [128, 8, 128], BF16) for i in range(2)]
                    sgb = [T(pb_, "sgb%d" % i, [128, 512], BF16) for i in range(2)]
                    aT = [T(pb_, "aTs%d" % i, [128, 4, 128], BF16) for i in range(2)]
                    ysb = [T(pb_, "ysb%d" % i, [128, D], F32) for i in range(2)]

                    def fetch(i):
                        S.dma('pool', wt[i % 2][:, :], wbf_d[l][:, :], own=R('wt', i % 2),
                              r=[R('idxW')] + wbf_res, w=[R('wt', i % 2)], indirect=('gather', idxW[:, i:i + 1]))
                        S.dma('sp', xs[i % 2][:], xslot_d[i * 128:(i + 1) * 128, :], own=R('xs', i % 2), r=xslot_res, w=[R('xs', i % 2)])

                    fetch(0)
                    for i in range(NS):
                        if i + 1 < NS:
                            fetch(i + 1)
                        w_, x_, xT_, sg_, a_, y_ = wt[i % 2], xs[i % 2], xT[i % 2], sgb[i % 2], aT[i % 2], ysb[i % 2]
                        tb = i % 2
                        for k in range(8):
                            S.op('pe', lambda e: e.transpose(psb[tb][:, k * 128:(k + 1) * 128], x_[:, k * 128:(k + 1) * 128], identB[:]),
                                 r=[R('xs', i % 2), R('identB')], w=[PR(tb)])
                        if i % 2:
                            S.op('dve', lambda e: e.tensor_copy(xT_[:].rearrange("p k s -> p (k s)"), psb[tb][:, 0:1024]), r=[PR(tb)], w=[R(xT_.name)])
                        else:
                            S.op('act', lambda e: e.activation(xT_[:].rearrange("p k s -> p (k s)"), psb[tb][:, 0:1024], AF.Copy), r=[PR(tb)], w=[R(xT_.name)])
                        G = 2 + (i % 2)
                        U = 4 + (i % 2)
                        for fc in range(4):
                            for k in range(8):
                                S.op('pe', lambda e: e.matmul(ps[G][:, fc * 128:(fc + 1) * 128], lhsT=w_[:, k * 512 + fc * 128:k * 512 + (fc + 1) * 128],
                                                              rhs=xT_[:, k, :], start=(k == 0), stop=(k == 7)),
                                     r=[R('wt', i % 2), R(xT_.name)], w=[PR(G)])
                        for fc in range(4):
                            for k in range(8):
                                S.op('pe', lambda e: e.matmul(ps[U][:, fc * 128:(fc + 1) * 128],
                                                              lhsT=w_[:, 4096 + k * 512 + fc * 128:4096 + k * 512 + (fc + 1) * 128],
                                                              rhs=xT_[:, k, :], start=(k == 0), stop=(k == 7)),
                                     r=[R('wt', i % 2), R(xT_.name)], w=[PR(U)])
                        S.op('act', lambda e: e.activation(sg_[:], ps[G][:, :], AF.Silu), r=[PR(G)], w=[R(sg_.name)])
                        S.op('dve', lambda e: e.tensor_tensor(a_[:].rearrange("p f s -> p (f s)"), sg_[:], ps[U][:, :], ALU.mult),
                             r=[R(sg_.name), PR(U)], w=[R(a_.name)])
                        for hf in range(2):
                            Y = 6 + hf
                            for fc in range(4):
                                S.op('pe', lambda e: e.matmul(ps[Y][:, :], lhsT=a_[:, fc, :],
                                                              rhs=w_[:, 8192 + fc * 1024 + hf * 512:8192 + fc * 1024 + (hf + 1) * 512],
                                                              start=(fc == 0), stop=(fc == 3)),
                                     r=[R(a_.name), R('wt', i % 2)], w=[PR(Y)])
                            if hf:
                                S.op('dve', lambda e: e.tensor_copy(y_[:, hf * 512:(hf + 1) * 512], ps[Y][:, :]), r=[PR(Y)], w=[R(y_.name, hf)])
                            else:
                                S.op('act', lambda e: e.activation(y_[:, hf * 512:(hf + 1) * 512], ps[Y][:, :], AF.Copy), r=[PR(Y)], w=[R(y_.name, hf)])
                        S.dma('sp', yslot_d[i * 128:(i + 1) * 128, :], y_[:], own=R(y_.name, 0),
                              r=[R(y_.name, 0), R(y_.name, 1)], w=[R('yslot', i)])
                    S.barrier()
                yslot_res = [R('yslot', i) for i in range(NS)]
                with ExitStack() as pc_:
                    g1 = [T(pc_, "g1%d" % i, [128, D], F32) for i in range(2)]
                    g2 = [T(pc_, "g2%d" % i, [128, D], F32) for i in range(2)]
                    xq = [T(pc_, "xq%d" % i, [128, D], F32) for i in range(2)]
                    junk = T(pc_, "junkc", [128, D], BF16)
                    gfin = T(pc_, "gfin", [128, D], F32)
                    ssc = [T(pc_, "ssc%d" % i, [128, 4], F32) for i in range(2)]
                    S.dma('sp', gfin[:], bass.AP(g_final.tensor, g_final.offset, [[0, 128], [1, D]]), own=R('gfin'), w=[R('gfin')])
                    for lt in range(NTt):
                        tile = moe_tiles[lt]
                        b = lt % 2
                        kd = kind_of(tile)
                        a1, a2, xx, sc_ = g1[b], g2[b], xq[b], ssc[b]
                        S.dma('pool', a1[:, :], yslot_d[:, :], own=R(a1.name), r=[R('posI')] + yslot_res, w=[R(a1.name)],
                              indirect=('gather', posI[:, lt, 0:1]))
                        S.dma('pool', a2[:, :], yslot_d[:, :], own=R(a2.name), r=[R('posI')] + yslot_res, w=[R(a2.name)],
                              indirect=('gather', posI[:, lt, 1:2]))
                        S.dma('sp', xx[:], x_src(l, [tile], True)[:, 0, :], own=R(xx.name), r=x_res(l, [tile], True), w=[R(xx.name)])
                        S.op('dve', lambda e: e.tensor_scalar(a1[:], a1[:], W12[:, lt, 0:1], None, ALU.mult),
                             r=[R(a1.name), R('w12', lt, 0)], w=[R(a1.name)])
                        S.op('dve', lambda e: e.scalar_tensor_tensor(a1[:], a2[:], W12[:, lt, 1:2], a1[:], ALU.mult, ALU.add),
                             r=[R(a1.name), R(a2.name), R('w12', lt, 1)], w=[R(a1.name)])
                        S.op('dve', lambda e: e.tensor_tensor(a1[:], a1[:], gt2[:, kd, :], ALU.mult), r=[R(a1.name), R('gt2')], w=[R(a1.name)])
                        S.op('dve', lambda e: e.tensor_tensor(xx[:], xx[:], a1[:], ALU.add), r=[R(a1.name), R(xx.name)], w=[R(xx.name)])
                        if not last:
                            S.dma('sp', xs_d[tile * 128:(tile + 1) * 128, :], xx[:], own=R(xx.name), r=[R(xx.name)], w=[R('xs', tile)])
                        else:
                            S.op('act', lambda e: e.activation(junk[:], xx[:], AF.Square, accum_out=sc_[:, 0:1]),
                                 r=[R(xx.name)], w=[R(junk.name), R(sc_.name, 0)])
                            S.op('dve', lambda e: e.tensor_scalar(sc_[:, 1:2], sc_[:, 0:1], 1.0 / D, EPS, ALU.mult, ALU.add),
                                 r=[R(sc_.name, 0)], w=[R(sc_.name, 1)])
                            S.op('pool', lambda e: e.tensor_tensor(sc_[:, 2:3], sc_[:, 1:2], mhalf[:, 0:1], ALU.pow),
                                 r=[R(sc_.name, 1), R('mhalf')], w=[R(sc_.name, 2)])
                            S.op('dve', lambda e: e.scalar_tensor_tensor(xx[:], xx[:], sc_[:, 2:3], gfin[:], ALU.mult, ALU.mult),
                                 r=[R(xx.name), R(sc_.name, 2), R('gfin')], w=[R(xx.name)])
                            S.dma('sp', out_d[tile * 128:(tile + 1) * 128, :], xx[:], own=R(xx.name), r=[R(xx.name)], w=[R('out', tile)])
                S.phase_end()

        for l in range(DEPTH):
            need_ctx = l < DEPTH - 1
            lay = ExitStack()
            es.callback(lay.close)
            biasM = T(lay, "biasM", [128, 21, 4, 128], BF16)
            wqd_next = T(lay, "wqd_next", [128, 8, 768], BF16)
            with ExitStack() as ph:
                xts = [T(ph, "xt%d" % i, [128, 4, D], F32) for i in range(2)]
                junk = T(ph, "junk", [128, D], BF16)
                diags = [T(ph, "diag%d" % i, [128, 4, 128], F32) for i in range(2)]
                ssbs = [T(ph, "ssb%d" % i, [128, 12], F32) for i in range(2)]
                hTs = [T(ph, "hTs%d" % i, [128, 8, 512], BF16) for i in range(2)]
                norm_p1(GROUPS[0], xts[0], junk, diags[0], ssbs[0], x_src(l, GROUPS[0]), x_res(l, GROUPS[0]))
                for gi, tiles in enumerate(GROUPS):
                    n = len(tiles)
                    ntok = n * 128
                    hb = hTs[gi % 2]
                    if gi + 1 < len(GROUPS):
                        nt_ = GROUPS[gi + 1]
                        norm_p1(nt_, xts[(gi + 1) % 2], junk, diags[(gi + 1) % 2], ssbs[(gi + 1) % 2], x_src(l, nt_), x_res(l, nt_))
                    norm_p2(l, 1, tiles, xts[gi % 2], diags[gi % 2],
                            lambda c: (hb[:, c, 0:ntok], [R(hb.name, c)]), None,
                            [0, 1, 2, 3] if gi % 2 == 0 else [4, 5, 6, 7])
                    S.dma('sp', hT_view(tiles[0] * 128, ntok), hb[:, :, 0:ntok], own=R(hb.name, 0),
                          r=[R(hb.name, c) for c in range(8)], w=[R('hT', t) for t in tiles])
                S.phase_end()
            if stop == ('p1', l):
                break

            def load_hT(hb, tiles):
                ntok = len(tiles) * 128
                S.dma('sp', hb[:, :, 0:ntok], hT_view(tiles[0] * 128, ntok), own=R(hb.name),
                      r=[R('hT', t) for t in tiles], w=[R(hb.name)])

            def store_br(i, brs, tiles):
                ntok = len(tiles) * 128
                t0 = tiles[0] * 128
                S.dma('sp', brT_d[i, :, :, t0:t0 + ntok].rearrange("g p t -> p g t"), brs[0:64, :, 0:ntok],
                      own=R(brs.name), r=[R(brs.name)], w=[R('br', i, t) for t in tiles])

            def wload(dst, res, cols0, cols1):
                S.dma('pool', dst[:], w_in[l][:, cols0:cols1].rearrange("(k p) n -> p k n", p=128),
                      own=R(res), w=[R(res)])

            with ExitStack() as ph:
                wu = T(ph, "wu", [128, 8, 256], BF16)
                pw = T(ph, "pw", [64, 4, 64], BF16)
                band = T(ph, "band", [128, 20, 128], BF16)
                u_sb = T(ph, "u_sb", [128, NT, 256], BF16)
                hTs = [T(ph, "hTp%d" % i, [128, 8, 512], BF16) for i in range(2)]
                brs2 = [T(ph, "brs%d" % i, [64, 4, 512], BF16) for i in range(2)]
                pooled = [T(ph, "pooled%d" % i, [64, 512], BF16) for i in range(2)]
                wload(wu, 'wu', 0, 256)
                S.dma('pool', pw[:], pool_w[l].rearrange("g c d -> c g d"), own=R('pw'), w=[R('pw')])
                S.dma('pool', band[:], band_in, own=R('band'), w=[R('band')])
                if DBG_BARRIER:
                    S.barrier(DBG_BARRIER)
                groups = GROUPS if need_ctx else GROUPS[:8]
                load_hT(hTs[0], groups[0])
                for gi, tiles in enumerate(groups):
                    hb = hTs[gi % 2]
                    if gi + 1 < len(groups):
                        load_hT(hTs[(gi + 1) % 2], groups[gi + 1])
                    for ti, tile in enumerate(tiles):
                        pb = (gi * 4 + ti) % 4
                        for k in range(8):
                            S.op('pe', lambda e: e.matmul(ps[pb][:, 0:256], lhsT=hb[:, k, ti * 128:(ti + 1) * 128],
                                                          rhs=wu[:, k, :], start=(k == 0), stop=(k == 7)),
                                 r=[R(hb.name), R('wu')], w=[PR(pb)])
                        if ti % 2 == 0:
                            S.op('dve', lambda e: e.tensor_copy(u_sb[:, tile, :], ps[pb][:, 0:256]), r=[PR(pb)], w=[R('u', tile)])
                        else:
                            S.op('act', lambda e: e.activation(u_sb[:, tile, :], ps[pb][:, 0:256], AF.Copy), r=[PR(pb)], w=[R('u', tile)])
                cnt = 0
                for gi, tiles in enumerate(groups):
                    first, last = (0, 31) if tiles[0] < 32 else (32, 33)
                    ntok = len(tiles) * 128
                    brs = brs2[gi % 2]
                    for g in range(4):
                        pa = 4 + (cnt % 2)
                        pbk = 6 + (cnt % 2)
                        pl = pooled[cnt % 2]
                        cnt += 1
                        for jj, j in enumerate(tiles):
                            ins = []
                            if j > first:
                                ins.append((j - 1, 0))
                            ins.append((j, 3 if j == first else (4 if j == last else 1)))
                            if j < last:
                                ins.append((j + 1, 2))
                            for ii, (jin, var) in enumerate(ins):
                                S.op('pe', lambda e: e.matmul(ps[pa][0:64, jj * 128:(jj + 1) * 128],
                                                              lhsT=u_sb[:, jin, g * 64:(g + 1) * 64], rhs=band[:, g * 5 + var, :],
                                                              start=(ii == 0), stop=(ii == len(ins) - 1)),
                                     r=[R('u', jin), R('band')], w=[PR(pa)])
                        S.op('act', lambda e: e.activation(pl[0:64, 0:ntok], ps[pa][0:64, 0:ntok], AF.Copy), r=[PR(pa)], w=[R(pl.name)])
                        S.op('pe', lambda e: e.matmul(ps[pbk][0:64, 0:ntok], lhsT=pw[0:64, g, :], rhs=pl[0:64, 0:ntok],
                                                      start=True, stop=True),
                             r=[R('pw'), R(pl.name)], w=[PR(pbk)])
                        S.op('dve', lambda e: e.tensor_scalar(brs[0:64, g, 0:ntok], ps[pbk][0:64, 0:ntok],
                                                              cols64[0:64, l * 4 + g:l * 4 + g + 1], None, ALU.mult),
                             r=[PR(pbk), R('cols64')], w=[R(brs.name)])
                    store_br(0, brs, tiles)
                S.phase_end()
            if stop == ('pool', l):
                break

            def transpose_out(i, o_grp, tiles, brs, pbank):
                n = len(tiles)
                for t0 in range(0, n, 2):
                    nn = min(2, n - t0)
                    for tt in range(nn):
                        for h in range(4):
                            col = (tt * 4 + h) * 128
                            S.op('pe', lambda e: e.transpose(psb[pbank][0:64, col:col + 128],
                                                             o_grp[:, t0 + tt, h * 64:(h + 1) * 64], identB[:]),
                                 r=[R(o_grp.name), R('identB')], w=[PR(pbank)])
                    S.op('dve', lambda e: e.tensor_copy(
                        brs[0:64, :, t0 * 128:(t0 + nn) * 128].rearrange("p h (t q) -> p t h q", t=nn),
                        psb[pbank][0:64, 0:nn * 512].rearrange("p (t h q) -> p t h q", t=nn, h=4)),
                        r=[PR(pbank)], w=[R(brs.name)])
                store_br(i, brs, tiles)

            def dense_attention(i, pair_list, scale, ph, bg=None, bg_n=0):
                pTs = [T(ph, "pT%d" % k, [128, 512], BF16) for k in range(6)]
                o_grps = [T(ph, "ogrp%d" % k, [128, 4, 256], BF16) for k in range(2)]
                brs2 = [T(ph, "brsA%d" % k, [64, 4, 512], BF16) for k in range(2)]
                qgroups = GROUPS if need_ctx else GROUPS[:8]
                sbanks = [0, 1, 2, 3]
                obanks = [4, 5]
                sc = 0
                for gi, tiles in enumerate(qgroups):
                    nqs = len(tiles)
                    nq = nqs * 128
                    q0 = tiles[0] * 128
                    chunks = list(range(NT)) if tiles[0] < 32 else [32, 33]
                    o_grp = o_grps[gi % 2]
                    for lanes, fin in pair_list:
                        for ob in obanks:
                            S.op('dve', lambda e: e.memset(ps[ob][:, 0:nqs * 65], 0.0), w=[PR(ob)])
                        if bg is not None:
                            for _ in range(bg_n):
                                f_ = next(bg, None)
                                if f_ is not None:
                                    f_()
                        slots = {}

                        def qk_pair(ci):
                            nonlocal sc
                            c = chunks[ci]
                            for m in range(2):
                                kf, qf, vf = lanes[m]
                                sb = sbanks[sc % 4]
                                pT = pTs[sc % 6]
                                sc += 1
                                slots[(ci, m)] = (sb, pT)
                                S.op('pe', lambda e: e.matmul(ps[sb][:, 0:nq], lhsT=kf(c), rhs=qf(q0, nq), start=True, stop=True),
                                     r=[R('kT', c), R('qT', gi)], w=[PR(sb)])
                            for m in range(2):
                                sb, pT = slots[(ci, m)]
                                S.op('act', lambda e: e.activation(pT[:, 0:nq], ps[sb][:, 0:nq], AF.Exp, scale=scale),
                                     r=[PR(sb)], w=[R(pT.name)])

                        def pv_pair(ci):
                            c = chunks[ci]
                            for m in range(2):
                                kf, qf, vf = lanes[m]
                                sb, pT = slots.pop((ci, m))
                                for qs in range(nqs):
                                    S.op('pe', lambda e: e.matmul(ps[obanks[m]][:, qs * 65:(qs + 1) * 65],
                                                                  lhsT=pT[:, qs * 128:(qs + 1) * 128], rhs=vf(c),
                                                                  start=False, stop=(ci == len(chunks) - 1), skip_group_check=True),
                                         r=[R(pT.name), R('V', c)], w=[PR(obanks[m])])

                        qk_pair(0)
                        for ci in range(len(chunks)):
                            if ci + 1 < len(chunks):
                                qk_pair(ci + 1)
                            pv_pair(ci)
                        fin(nqs, obanks, o_grp)
                    transpose_out(i, o_grp, tiles, brs2[gi % 2], 7)

            with ExitStack() as ph:
                wq = T(ph, "wq", [128, 8, 512], BF16)
                qT_sb = T(ph, "qT_sb", [128, 2, NTOK], BF16)
                kT_sb = T(ph, "kT_sb", [128, 2, NTOK], BF16)
                Vaug = T(ph, "Vaug", [128, NT, 2, 65], BF16)
                cosT = T(ph, "cosT", [128, 32, 2, 16], F32)
                sinT = T(ph, "sinT", [128, 32, 2, 16], F32)
                hTs = [T(ph, "hTg%d" % i, [128, 8, 512], BF16) for i in range(2)]
                sqt = [T(ph, "sqt%d" % i, [128, 6, 64], F32) for i in range(4)]
                qn1 = [T(ph, "qn1%d" % i, [128, 8, 64], F32) for i in range(4)]
                qn2 = [T(ph, "qn2%d" % i, [128, 8, 64], F32) for i in range(4)]
                rm = [T(ph, "rm%d" % i, [128, 4, 8, 32], F32) for i in range(4)]
                qr = [T(ph, "qr%d" % i, [128, 8, 64], BF16) for i in range(4)]
                st6 = [T(ph, "st6%d" % i, [128, 18], F32) for i in range(4)]
                rden = [T(ph, "rden%d" % i, [128, 4], F32) for i in range(2)]
                wload(wq, 'wq', 1792, 2304)
                S.dma('sp', cosT[:], cosg_in, own=R('cosT'), w=[R('cosT')])
                S.dma('sp', sinT[:], sing_in, own=R('sinT'), w=[R('sinT')])
                S.op('pool', lambda e: e.memset(Vaug[:], 1.0), w=[R('V', c) for c in range(NT)])
                load_hT(hTs[0], GROUPS[0])
                for gi, tiles in enumerate(GROUPS):
                    hb = hTs[gi % 2]
                    if gi + 1 < len(GROUPS):
                        load_hT(hTs[(gi + 1) % 2], GROUPS[gi + 1])
                    for ti, tile in enumerate(tiles):
                        for k in range(8):
                            S.op('pe', lambda e: e.matmul(ps[ti][:, :], lhsT=hb[:, k, ti * 128:(ti + 1) * 128], rhs=wq[:, k, :],
                                                          start=(k == 0), stop=(k == 7)),
                                 r=[R(hb.name), R('wq')], w=[PR(ti)])
                    for ti, tile in enumerate(tiles):
                        S.op('act', lambda e: e.activation(Vaug[:, tile, :, 0:64], ps[ti][:, 384:512].rearrange("p (h d) -> p h d", h=2), AF.Copy),
                             r=[PR(ti)], w=[R('V', tile)])
                        S.op('act', lambda e: e.activation(sqt[ti][:], ps[ti][:, 0:384].rearrange("p (h d) -> p h d", h=6), AF.Square),
                             r=[PR(ti)], w=[R(sqt[ti].name)])
                    for ti, tile in enumerate(tiles):
                        S.op('dve', lambda e: e.reduce_sum(st6[ti][:, 0:6], sqt[ti][:], axis=AX.X), r=[R(sqt[ti].name)], w=[R(st6[ti].name, 0)])
                        S.op('dve', lambda e: e.tensor_scalar(st6[ti][:, 6:12], st6[ti][:, 0:6], 1.0 / 64, EPS, ALU.mult, ALU.add),
                             r=[R(st6[ti].name, 0)], w=[R(st6[ti].name, 1)])
                    for ti, tile in enumerate(tiles):
                        S.op('pool', lambda e: e.tensor_tensor(st6[ti][:, 12:18], st6[ti][:, 6:12], mhalf[:, 0:6], ALU.pow),
                             r=[R(st6[ti].name, 1), R('mhalf')], w=[R(st6[ti].name, 2)])
                    for ti, tile in enumerate(tiles):
                        S.op('dve', lambda e: e.tensor_tensor(qn1[ti][:, 0:4, :], ps[ti][:, 0:256].rearrange("p (h d) -> p h d", h=4),
                                                              mk(st6[ti], 0, 128, 12, [[1, 4], [0, 64]]), ALU.mult),
                             r=[PR(ti), R(st6[ti].name, 2)], w=[R(qn1[ti].name, 0)])
                        S.op('dve', lambda e: e.tensor_tensor(qn1[ti][:, 4:8, :].rearrange("p (k u) d -> p k u d", u=2),
                                                              mk(ps[ti], 0, 128, 256, [[64, 2], [0, 2], [1, 64]]),
                                                              mk(st6[ti], 0, 128, 16, [[1, 2], [0, 2], [0, 64]]), ALU.mult),
                             r=[PR(ti), R(st6[ti].name, 2)], w=[R(qn1[ti].name, 1)])
                    for ti, tile in enumerate(tiles):
                        if tile < 32:
                            S.op('pool', lambda e: e.tensor_tensor(qn2[ti][:], qn1[ti][:], gainG[:, l, :, :], ALU.mult),
                                 r=[R(qn1[ti].name, 0), R(qn1[ti].name, 1), R('gainG')], w=[R(qn2[ti].name)])
                        else:
                            S.op('pool', lambda e: e.tensor_tensor(qr[ti][:], qn1[ti][:], gainG[:, l, :, :], ALU.mult),
                                 r=[R(qn1[ti].name, 0), R(qn1[ti].name, 1), R('gainG')], w=[R(qr[ti].name, 0), R(qr[ti].name, 1)])
                    for half in range(2):
                        for ti, tile in enumerate(tiles):
                            if tile >= 32:
                                continue
                            X = qn2[ti]
                            x1 = mk(X, 0, 128, 0, [[64, 8], [32, 2], [1, 16]])
                            x2 = mk(X, 0, 128, 16, [[64, 8], [32, 2], [1, 16]])
                            cs = mk(cosT, 0, 128, tile * 32, [[0, 8], [16, 2], [1, 16]])
                            sn = mk(sinT, 0, 128, tile * 32, [[0, 8], [16, 2], [1, 16]])
                            m_ = [mk(rm[ti], 0, 128, q * 256, [[32, 8], [16, 2], [1, 16]]) for q in range(4)]
                            o1 = mk(qr[ti], 0, 128, 0, [[64, 8], [32, 2], [1, 16]])
                            o2 = mk(qr[ti], 0, 128, 16, [[64, 8], [32, 2], [1, 16]])
                            if half == 0:
                                S.op('dve', lambda e: e.tensor_tensor(m_[0], x1, cs, ALU.mult), r=[R(X.name), R('cosT')], w=[R(rm[ti].name, 0)])
                                S.op('dve', lambda e: e.tensor_tensor(m_[1], x2, sn, ALU.mult), r=[R(X.name), R('sinT')], w=[R(rm[ti].name, 1)])
                                S.op('pool', lambda e: e.tensor_tensor(m_[2], x1, sn, ALU.mult), r=[R(X.name), R('sinT')], w=[R(rm[ti].name, 2)])
                                S.op('pool', lambda e: e.tensor_tensor(m_[3], x2, cs, ALU.mult), r=[R(X.name), R('cosT')], w=[R(rm[ti].name, 3)])
                            else:
                                S.op('dve', lambda e: e.tensor_tensor(o1, m_[0], m_[1], ALU.subtract),
                                     r=[R(rm[ti].name, 0), R(rm[ti].name, 1)], w=[R(qr[ti].name, 0)])
                                S.op('pool', lambda e: e.tensor_tensor(o2, m_[2], m_[3], ALU.add),
                                     r=[R(rm[ti].name, 2), R(rm[ti].name, 3)], w=[R(qr[ti].name, 1)])
                    for ti, tile in enumerate(tiles):
                        tb = 4 + ti
                        for jj in range(4):
                            S.op('pe', lambda e: e.transpose(psb[tb][:, jj * 128:(jj + 1) * 128],
                                                             qr[ti][:, 2 * jj:2 * jj + 2, :].rearrange("p h d -> p (h d)"), identB[:]),
                                 r=[R(qr[ti].name, 0), R(qr[ti].name, 1), R('identB')], w=[PR(tb)])
                    for ti, tile in enumerate(tiles):
                        tb = 4 + ti
                        S.op('act', lambda e: e.activation(qT_sb[:, :, tile * 128:(tile + 1) * 128],
                                                           psb[tb][:, 0:256].rearrange("p (h q) -> p h q", h=2), AF.Copy),
                             r=[PR(tb)], w=[R('qT', tile // 4)])
                        S.op('dve', lambda e: e.tensor_copy(kT_sb[:, :, tile * 128:(tile + 1) * 128],
                                                            psb[tb][:, 256:512].rearrange("p (h q) -> p h q", h=2)),
                             r=[PR(tb)], w=[R('kT', tile)])

                def fin_gqa(h, nqs, ob, o_grp):
                    rd = rden[h % 2]
                    S.op('dve', lambda e: e.reciprocal(rd[:, 0:nqs], mk(ps[ob], 0, 128, 64, [[65, nqs]])), r=[PR(ob)], w=[R(rd.name)])
                    S.op('dve', lambda e: e.tensor_tensor(o_grp[:, 0:nqs, h * 64:(h + 1) * 64],
                                                          mk(ps[ob], 0, 128, 0, [[65, nqs], [1, 64]]),
                                                          mk(rd, 0, 128, 0, [[1, nqs], [0, 64]]), ALU.mult),
                         r=[PR(ob), R(rd.name)], w=[R(o_grp.name)])

                rp = T(ph, "rp", [60, 128], F32)
                Tall = T(ph, "Tall", [128, 60, 64], F32)
                maskc = T(ph, "maskc", [128, 21, 128], F32)
                S.op('pool', lambda e: e.memset(rp[:], 0.0), w=[R('rp')])
                S.dma('sp', rp[:, 48:79], nat_rpb[l], own=R('rp'), w=[R('rp')])
                S.dma('sp', rpbpad[l], rp[:], own=R('rp'), r=[R('rp')], w=[R('rpbpad')])
                S.dma('sp', maskc[:], natmask_in, own=R('maskc'), w=[R('maskc')])
                for b in range(2):
                    S.dma('sp', Tall[b * 64:(b + 1) * 64, :, :],
                          bass.AP(rpbpad.tensor, rpbpad[l].offset, [[1, 64], [128, 60], [1, 64]]),
                          own=R('Tall', b), r=[R('rpbpad')], w=[R('Tall', b)])
                wload(wqd_next, 'wqd', 256, 1024)
                if SPARSE:
                    S.barrier(['pool'])
                    issue_casts(l)
                bg_ops = []
                for h in range(4):
                    bg_ops.append(lambda h=h: S.op('dve', lambda e: e.tensor_copy(biasM[:, :, h, :], maskc[:]), r=[R('maskc')], w=[R('biasM')]))
                for v in range(21):
                    for (a, b), dr in nat_plan[v].items():
                        if dr is None:
                            continue
                        for h in range(4):
                            bg_ops.append(lambda v=v, a=a, b=b, dr=dr, h=h: S.op(
                                'dve', lambda e: e.scalar_tensor_tensor(biasM[b * 64:(b + 1) * 64, v, h, a * 64:(a + 1) * 64],
                                                                        Tall[b * 64:(b + 1) * 64, h * 15 + dr, :], 8.0,
                                                                        maskc[b * 64:(b + 1) * 64, v, a * 64:(a + 1) * 64], ALU.mult, ALU.add),
                                r=[R('Tall', b), R('maskc'), R('biasM')], w=[R('biasM')]))
                bg_iter = iter(bg_ops)

                def gqa_pair(j):
                    def lane(r):
                        return (lambda c: kT_sb[r * 64:(r + 1) * 64, j, c * 128:(c + 1) * 128],
                                lambda q0, nq: qT_sb[r * 64:(r + 1) * 64, j, q0:q0 + nq],
                                lambda c: Vaug[:, c, j, :])

                    def fin(nqs, obanks, o_grp):
                        for r in range(2):
                            fin_gqa(2 * j + r, nqs, obanks[r], o_grp)
                    return ([lane(0), lane(1)], fin)

                dense_attention(3, [gqa_pair(0), gqa_pair(1)], 0.125, ph, bg=bg_iter, bg_n=24)
                for f_ in bg_iter:
                    f_()
                S.phase_end()
            if stop == ('gqa', l):
                break

            with ExitStack() as ph:
                wq = wqd_next
                qT_sb = T(ph, "qT_sbd", [64, 4, NTOK], BF16)
                kT_sb = T(ph, "kT_sbd", [64, 4, NTOK], BF16)
                Vaug = T(ph, "Vaugd", [128, NT, 4, 65], BF16)
                cosT = T(ph, "cosTd", [128, 32, 2, 8], F32)
                sinT = T(ph, "sinTd", [128, 32, 2, 8], F32)
                hTs = [T(ph, "hTd%d" % i, [128, 8, 512], BF16) for i in range(2)]
                qf = [T(ph, "qf%d" % i, [128, 16, 32], F32) for i in range(2)]
                rm = [T(ph, "rmd%d" % i, [128, 4, 16, 16], F32) for i in range(2)]
                qr = [T(ph, "qrd%d" % i, [128, 16, 32], BF16) for i in range(2)]
                rden = [T(ph, "rdend%d" % i, [128, 16], F32) for i in range(2)]
                t0s = [T(ph, "t0s%d" % i, [128, 4, 64], F32) for i in range(2)]
                t1s = [T(ph, "t1s%d" % i, [128, 4, 64], F32) for i in range(2)]
                tsq = [T(ph, "tsq%d" % i, [128, 4, 64], F32) for i in range(2)]
                S.dma('sp', cosT[:], cosd_in, own=R('cosT'), w=[R('cosT')])
                S.dma('sp', sinT[:], sind_in, own=R('sinT'), w=[R('sinT')])
                S.op('pool', lambda e: e.memset(Vaug[:], 1.0), w=[R('V', c) for c in range(NT)])
                load_hT(hTs[0], GROUPS[0])
                bcnt = 0
                for gi, tiles in enumerate(GROUPS):
                    hb = hTs[gi % 2]
                    if gi + 1 < len(GROUPS):
                        load_hT(hTs[(gi + 1) % 2], GROUPS[gi + 1])
                    for t0_ in range(0, len(tiles), 2):
                        batch = [(t0_ + u, tiles[t0_ + u]) for u in range(min(2, len(tiles) - t0_))]
                        tbs = [4, 5] if bcnt % 2 == 0 else [6, 7]
                        bcnt += 1
                        for u, (ti, tile) in enumerate(batch):
                            pb = 2 * u
                            for k in range(8):
                                S.op('pe', lambda e: e.matmul(ps[pb][:, :], lhsT=hb[:, k, ti * 128:(ti + 1) * 128], rhs=wq[:, k, 0:512],
                                                              start=(k == 0), stop=(k == 7)),
                                     r=[R(hb.name), R('wqd')], w=[PR(pb)])
                            for k in range(8):
                                S.op('pe', lambda e: e.matmul(ps[pb + 1][:, 0:256], lhsT=hb[:, k, ti * 128:(ti + 1) * 128], rhs=wq[:, k, 512:768],
                                                              start=(k == 0), stop=(k == 7)),
                                     r=[R(hb.name), R('wqd')], w=[PR(pb + 1)])
                        for u, (ti, tile) in enumerate(batch):
                            pb = 2 * u
                            S.op('act', lambda e: e.activation(Vaug[:, tile, :, 0:64], ps[pb + 1][:, 0:256].rearrange("p (h d) -> p h d", h=4), AF.Copy),
                                 r=[PR(pb + 1)], w=[R('V', tile)])
                            if tile < 32:
                                S.op('act', lambda e: e.activation(qf[u][:].rearrange("p a b -> p (a b)"), ps[pb][:, :], AF.Copy),
                                     r=[PR(pb)], w=[R(qf[u].name)])
                            else:
                                S.op('act', lambda e: e.activation(qr[u][:].rearrange("p a b -> p (a b)"), ps[pb][:, :], AF.Copy),
                                     r=[PR(pb)], w=[R(qr[u].name, 0), R(qr[u].name, 1)])
                        for half in range(2):
                            for u, (ti, tile) in enumerate(batch):
                                if tile >= 32:
                                    continue
                                X = qf[u]
                                x1 = mk(X, 0, 128, 0, [[32, 16], [16, 2], [1, 8]])
                                x2 = mk(X, 0, 128, 8, [[32, 16], [16, 2], [1, 8]])
                                cs = mk(cosT, 0, 128, tile * 16, [[0, 16], [8, 2], [1, 8]])
                                sn = mk(sinT, 0, 128, tile * 16, [[0, 16], [8, 2], [1, 8]])
                                m_ = [mk(rm[u], 0, 128, q * 256, [[16, 16], [8, 2], [1, 8]]) for q in range(4)]
                                o1 = mk(qr[u], 0, 128, 0, [[32, 16], [16, 2], [1, 8]])
                                o2 = mk(qr[u], 0, 128, 8, [[32, 16], [16, 2], [1, 8]])
                                if half == 0:
                                    S.op('dve', lambda e: e.tensor_tensor(m_[0], x1, cs, ALU.mult), r=[R(X.name), R('cosT')], w=[R(rm[u].name, 0)])
                                    S.op('dve', lambda e: e.tensor_tensor(m_[1], x2, sn, ALU.mult), r=[R(X.name), R('sinT')], w=[R(rm[u].name, 1)])
                                    S.op('pool', lambda e: e.tensor_tensor(m_[2], x1, sn, ALU.mult), r=[R(X.name), R('sinT')], w=[R(rm[u].name, 2)])
                                    S.op('pool', lambda e: e.tensor_tensor(m_[3], x2, cs, ALU.mult), r=[R(X.name), R('cosT')], w=[R(rm[u].name, 3)])
                                else:
                                    S.op('dve', lambda e: e.tensor_tensor(o1, m_[0], m_[1], ALU.subtract),
                                         r=[R(rm[u].name, 0), R(rm[u].name, 1)], w=[R(qr[u].name, 0)])
                                    S.op('pool', lambda e: e.tensor_tensor(o2, m_[2], m_[3], ALU.add),
                                         r=[R(rm[u].name, 2), R(rm[u].name, 3)], w=[R(qr[u].name, 1)])
                        for u, (ti, tile) in enumerate(batch):
                            tb = tbs[u]
                            qrf = qr[u][:].rearrange("p a b -> p (a b)")
                            for hh in range(8):
                                S.op('pe', lambda e: e.transpose(psb[tb][0:64, hh * 128:(hh + 1) * 128], qrf[:, hh * 64:(hh + 1) * 64], identB[:]),
                                     r=[R(qr[u].name, 0), R(qr[u].name, 1), R('identB')], w=[PR(tb)])
                        for u, (ti, tile) in enumerate(batch):
                            tb = tbs[u]
                            S.op('act', lambda e: e.activation(qT_sb[0:64, :, tile * 128:(tile + 1) * 128],
                                                               psb[tb][0:64, 0:512].rearrange("p (h q) -> p h q", h=4), AF.Copy),
                                 r=[PR(tb)], w=[R('qT', tile // 4)])
                            S.op('dve', lambda e: e.tensor_copy(kT_sb[0:64, :, tile * 128:(tile + 1) * 128],
                                                                psb[tb][0:64, 512:1024].rearrange("p (h q) -> p h q", h=4)),
                                 r=[PR(tb)], w=[R('kT', tile)])

                def fin_diff(h, nqs, obanks, o_grp):
                    o0, o1 = obanks
                    rd = rden[h % 2]
                    t0, t1, tq = t0s[h % 2], t1s[h % 2], tsq[h % 2]
                    S.op('dve', lambda e: e.reciprocal(rd[:, 0:nqs], mk(ps[o0], 0, 128, 64, [[65, nqs]])), r=[PR(o0)], w=[R(rd.name, 0)])
                    S.op('dve', lambda e: e.reciprocal(rd[:, 4:4 + nqs], mk(ps[o1], 0, 128, 64, [[65, nqs]])), r=[PR(o1)], w=[R(rd.name, 1)])
                    S.op('dve', lambda e: e.tensor_scalar(rd[:, 4:4 + nqs], rd[:, 4:4 + nqs], nlam[:, l:l + 1], None, ALU.mult),
                         r=[R(rd.name, 1), R('nlam')], w=[R(rd.name, 1)])
                    S.op('dve', lambda e: e.tensor_tensor(t0[:, 0:nqs, :], mk(ps[o0], 0, 128, 0, [[65, nqs], [1, 64]]),
                                                          mk(rd, 0, 128, 0, [[1, nqs], [0, 64]]), ALU.mult),
                         r=[PR(o0), R(rd.name, 0)], w=[R(t0.name)])
                    S.op('dve', lambda e: e.tensor_tensor(t1[:, 0:nqs, :], mk(ps[o1], 0, 128, 0, [[65, nqs], [1, 64]]),
                                                          mk(rd, 0, 128, 4, [[1, nqs], [0, 64]]), ALU.mult),
                         r=[PR(o1), R(rd.name, 1)], w=[R(t1.name)])
                    S.op('dve', lambda e: e.tensor_tensor(t0[:, 0:nqs, :], t0[:, 0:nqs, :], t1[:, 0:nqs, :], ALU.add),
                         r=[R(t0.name), R(t1.name)], w=[R(t0.name)])
                    S.op('dve', lambda e: e.tensor_tensor(tq[:, 0:nqs, :], t0[:, 0:nqs, :], t0[:, 0:nqs, :], ALU.mult),
                         r=[R(t0.name)], w=[R(tq.name)])
                    S.op('dve', lambda e: e.reduce_sum(rd[:, 8:8 + nqs], tq[:, 0:nqs, :], axis=AX.X), r=[R(tq.name)], w=[R(rd.name, 2)])
                    S.op('dve', lambda e: e.tensor_scalar(rd[:, 8:8 + nqs], rd[:, 8:8 + nqs], 1.0 / 64, EPS, ALU.mult, ALU.add),
                         r=[R(rd.name, 2)], w=[R(rd.name, 2)])
                    S.op('pool', lambda e: e.tensor_tensor(rd[:, 12:12 + nqs], rd[:, 8:8 + nqs], mhalf[:, 0:nqs], ALU.pow),
                         r=[R(rd.name, 2), R('mhalf')], w=[R(rd.name, 3)])
                    S.op('dve', lambda e: e.tensor_tensor(t1[:, 0:nqs, :], t0[:, 0:nqs, :], mk(rd, 0, 128, 12, [[1, nqs], [0, 64]]), ALU.mult),
                         r=[R(t0.name), R(rd.name, 3)], w=[R(t1.name)])
                    S.op('dve', lambda e: e.tensor_tensor(o_grp[:, 0:nqs, h * 64:(h + 1) * 64], t1[:, 0:nqs, :],
                                                           mk(gB, 0, 128, l * 64, [[0, nqs], [1, 64]]), ALU.mult),
                         r=[R(t1.name), R('gB')], w=[R(o_grp.name)])

                def diff_pair(h):
                    def lane(m):
                        return (lambda c: kT_sb[m * 32:(m + 1) * 32, h, c * 128:(c + 1) * 128],
                                lambda q0, nq: qT_sb[m * 32:(m + 1) * 32, h, q0:q0 + nq],
                                lambda c: Vaug[:, c, h, :])
                    return ([lane(0), lane(1)], lambda nqs, obanks, o_grp: fin_diff(h, nqs, obanks, o_grp))

                dense_attention(1, [diff_pair(h) for h in range(4)], 32 ** -0.5, ph)
                S.phase_end()
            if stop == ('diff', l):
                break

            with ExitStack() as ph:
                wq = T(ph, "wqn", [128, 8, 768], BF16)
                qT_sb = T(ph, "qT_sbn", [64, 4, NTOK], BF16)
                kT_sb = T(ph, "kT_sbn", [64, 4, NTOK], BF16)
                Vaug = T(ph, "Vaugn", [128, NT, 4, 65], BF16)
                hTs = [T(ph, "hTn%d" % i, [128, 8, 512], BF16) for i in range(2)]
                pTa = [T(ph, "pTa%d" % i, [128, 512], BF16) for i in range(2)]
                pTb = [T(ph, "pTb%d" % i, [128, 512], BF16) for i in range(2)]
                o_grps = [T(ph, "ogn%d" % k, [128, 4, 256], BF16) for k in range(2)]
                brs2 = [T(ph, "brsn%d" % k, [64, 4, 512], BF16) for k in range(2)]
                rden = [T(ph, "rdenn%d" % i, [128, 4], F32) for i in range(2)]
                wload(wq, 'wq', 1024, 1792)
                S.op('pool', lambda e: e.memset(Vaug[:], 1.0), w=[R('V', c) for c in range(NT)])
                load_hT(hTs[0], GROUPS[0])
                pc = 0
                for gi, tiles in enumerate(GROUPS):
                    hb = hTs[gi % 2]
                    ntok = len(tiles) * 128
                    t0 = tiles[0] * 128
                    if gi + 1 < len(GROUPS):
                        load_hT(hTs[(gi + 1) % 2], GROUPS[gi + 1])
                    for qk in range(2):
                        for h in range(4):
                            pb = pc % 4
                            pc += 1
                            c0 = qk * 256 + h * 64
                            for k in range(8):
                                S.op('pe', lambda e: e.matmul(ps[pb][0:64, 0:ntok], lhsT=wq[:, k, c0:c0 + 64], rhs=hb[:, k, 0:ntok],
                                                              start=(k == 0), stop=(k == 7)),
                                     r=[R(hb.name), R('wq')], w=[PR(pb)])
                            dst = (qT_sb if qk == 0 else kT_sb)[0:64, h, t0:t0 + ntok]
                            wres = [R('qT', gi)] if qk == 0 else [R('kT', t) for t in tiles]
                            if pc % 2:
                                S.op('dve', lambda e: e.tensor_copy(dst, ps[pb][0:64, 0:ntok]), r=[PR(pb)], w=wres)
                            else:
                                S.op('act', lambda e: e.activation(dst, ps[pb][0:64, 0:ntok], AF.Copy), r=[PR(pb)], w=wres)
                    for ti, tile in enumerate(tiles):
                        pb = pc % 4
                        pc += 1
                        for k in range(8):
                            S.op('pe', lambda e: e.matmul(ps[pb][:, 0:256], lhsT=hb[:, k, ti * 128:(ti + 1) * 128], rhs=wq[:, k, 512:768],
                                                          start=(k == 0), stop=(k == 7)),
                                 r=[R(hb.name), R('wq')], w=[PR(pb)])
                        S.op('act', lambda e: e.activation(Vaug[:, tile, :, 0:64], ps[pb][:, 0:256].rearrange("p (h d) -> p h d", h=4), AF.Copy),
                             r=[PR(pb)], w=[R('V', tile)])
                qgroups = GROUPS if need_ctx else GROUPS[:8]
                sc = 0
                for gi, tiles in enumerate(qgroups):
                    o_grp = o_grps[gi % 2]
                    for ti, j in enumerate(tiles):
                        if j < 32:
                            kts = [(t, _nat_variant(j, t)) for t in _nat_keytiles(j)] + [(32, None), (33, None)]
                        else:
                            kts = [(32, None), (33, None)]
                        ob = 4 + (ti % 2)
                        hs = {}

                        def nat_a(h):
                            nonlocal sc
                            sa = (sc % 2) * 2
                            pa, pbb = pTa[sc % 2], pTb[sc % 2]
                            sc += 1
                            hs[h] = (pa, pbb)
                            for ii, (t, v) in enumerate(kts):
                                bank = sa + ii // 4
                                col = (ii % 4) * 128
                                S.op('pe', lambda e: e.matmul(ps[bank][:, col:col + 128], lhsT=kT_sb[0:64, h, t * 128:(t + 1) * 128],
                                                              rhs=qT_sb[0:64, h, j * 128:(j + 1) * 128], start=True, stop=(v is None)),
                                     r=[R('kT', t), R('qT', gi)], w=[PR(bank)])
                                if v is not None:
                                    S.op('pe', lambda e: e.matmul(ps[bank][:, col:col + 128], lhsT=biasM[:, v, h, :], rhs=antiB[:],
                                                                  start=False, stop=True),
                                         r=[R('biasM'), R('antiB')], w=[PR(bank)])
                            na = min(4, len(kts))
                            nb = len(kts) - na
                            S.op('act', lambda e: e.activation(pa[:, 0:na * 128], ps[sa][:, 0:na * 128], AF.Exp, scale=0.125),
                                 r=[PR(sa)], w=[R(pa.name)])
                            if nb:
                                S.op('act', lambda e: e.activation(pbb[:, 0:nb * 128], ps[sa + 1][:, 0:nb * 128], AF.Exp, scale=0.125),
                                     r=[PR(sa + 1)], w=[R(pbb.name)])

                        def nat_b(h):
                            pa, pbb = hs.pop(h)
                            for ii, (t, v) in enumerate(kts):
                                src = pa if ii < 4 else pbb
                                col = (ii % 4) * 128
                                S.op('pe', lambda e: e.matmul(ps[ob][:, h * 65:(h + 1) * 65], lhsT=src[:, col:col + 128], rhs=Vaug[:, t, h, :],
                                                              start=(ii == 0), stop=(ii == len(kts) - 1)),
                                     r=[R(src.name), R('V', t)], w=[PR(ob)])

                        nat_a(0)
                        for h in range(4):
                            if h + 1 < 4:
                                nat_a(h + 1)
                            nat_b(h)
                        rd = rden[ti % 2]
                        S.op('dve', lambda e: e.reciprocal(rd[:, 0:4], mk(ps[ob], 0, 128, 64, [[65, 4]])), r=[PR(ob)], w=[R(rd.name)])
                        S.op('dve', lambda e: e.tensor_tensor(o_grp[:, ti, :].rearrange("p (h d) -> p h d", h=4),
                                                              mk(ps[ob], 0, 128, 0, [[65, 4], [1, 64]]),
                                                              mk(rd, 0, 128, 0, [[1, 4], [0, 64]]), ALU.mult),
                             r=[PR(ob), R(rd.name)], w=[R(o_grp.name)])
                    transpose_out(2, o_grp, tiles, brs2[gi % 2], 7)
                S.phase_end()
            lay.close()
            if stop == ('nat', l):
                break

            up_groups = GROUPS if need_ctx else GROUPS[:8]
            with ExitStack() as ph:
                wgate = T(ph, "wgate", [128, 8, 4096], BF16)
                wbr = T(ph, "wbr", [128, 4, 2, D], BF16)
                hTs = [T(ph, "hTm%d" % i, [128, 8, 512], BF16) for i in range(2)]
                brTs = [T(ph, "brTm%d" % i, [128, 4, 2, 512], BF16) for i in range(2)]
                mTs = [T(ph, "mTs%d" % i, [128, 8, 512], BF16) for i in range(2)]
                sgs = [T(ph, "sg%d" % i, [128, 512], F32) for i in range(3)]
                accs = [T(ph, "acc%d" % i, [128, 512], F32) for i in range(2)]
                tmps = [T(ph, "tmpm%d" % i, [128, 512], F32) for i in range(2)]
                for q4 in range(4):
                    S.dma('pool', wgate[:, :, q4 * 1024:(q4 + 1) * 1024],
                          w_in[l][:, 2304 + q4 * 1024:2304 + (q4 + 1) * 1024].rearrange("(k p) n -> p k n", p=128),
                          own=R('wgate', q4), w=[R('wgate', q4)])
                S.dma('pool', wbr[:], w_branch[l].rearrange("b (m p) n -> p b m n", p=128), own=R('wbr'), w=[R('wbr')])

                def load_br(bt, tiles):
                    ntok = len(tiles) * 128
                    t0 = tiles[0] * 128
                    for hh in range(2):
                        S.dma('sp', bt[hh * 64:(hh + 1) * 64, :, :, 0:ntok],
                              brT_d[:, :, :, t0:t0 + ntok].rearrange("b (m hh) d t -> hh d b m t", hh=2)[hh],
                              own=R(bt.name, hh), r=[R('br', i, t) for i in range(4) for t in tiles], w=[R(bt.name, hh)])

                load_hT(hTs[0], up_groups[0])
                load_br(brTs[0], up_groups[0])
                cnt = 0
                for gi, tiles in enumerate(up_groups):
                    ntok = len(tiles) * 128
                    hb, bt, mt = hTs[gi % 2], brTs[gi % 2], mTs[gi % 2]
                    if gi + 1 < len(up_groups):
                        load_hT(hTs[(gi + 1) % 2], up_groups[gi + 1])
                        load_br(brTs[(gi + 1) % 2], up_groups[gi + 1])
                    for dc in range(8):
                        acc = accs[dc % 2]
                        for i in range(4):
                            pg = (cnt % 3)
                            pp = 3 + (cnt % 3)
                            sg = sgs[cnt % 3]
                            cnt += 1
                            c0 = i * 1024 + dc * 128
                            for k in range(8):
                                S.op('pe', lambda e: e.matmul(ps[pg][:, 0:ntok], lhsT=wgate[:, k, c0:c0 + 128], rhs=hb[:, k, 0:ntok],
                                                              start=(k == 0), stop=(k == 7)),
                                     r=[R(hb.name), R('wgate', i)], w=[PR(pg)])
                            for m in range(2):
                                S.op('pe', lambda e: e.matmul(ps[pp][:, 0:ntok], lhsT=wbr[:, i, m, dc * 128:(dc + 1) * 128], rhs=bt[:, i, m, 0:ntok],
                                                              start=(m == 0), stop=(m == 1)),
                                     r=[R(bt.name, 0), R(bt.name, 1), R('wbr')], w=[PR(pp)])
                            S.op('act', lambda e: e.activation(sg[:, 0:ntok], ps[pg][:, 0:ntok], AF.Sigmoid), r=[PR(pg)], w=[R(sg.name)])
                            if i == 0:
                                S.op('dve', lambda e: e.tensor_tensor(acc[:, 0:ntok], sg[:, 0:ntok], ps[pp][:, 0:ntok], ALU.mult),
                                     r=[R(sg.name), PR(pp)], w=[R(acc.name)])
                            else:
                                tm = tmps[i % 2]
                                S.op('dve', lambda e: e.tensor_tensor(tm[:, 0:ntok], sg[:, 0:ntok], ps[pp][:, 0:ntok], ALU.mult),
                                     r=[R(sg.name), PR(pp)], w=[R(tm.name)])
                                if i < 3:
                                    S.op('pool', lambda e: e.tensor_tensor(acc[:, 0:ntok], acc[:, 0:ntok], tm[:, 0:ntok], ALU.add),
                                         r=[R(acc.name), R(tm.name)], w=[R(acc.name)])
                                else:
                                    S.op('pool', lambda e: e.tensor_tensor(mt[:, dc, 0:ntok], acc[:, 0:ntok], tm[:, 0:ntok], ALU.add),
                                         r=[R(acc.name), R(tm.name)], w=[R(mt.name, dc)])
                    t0 = tiles[0] * 128
                    S.dma('sp', mT_d[:, :, t0:t0 + ntok].rearrange("c p t -> p c t"), mt[:, :, 0:ntok], own=R(mt.name, 0),
                          r=[R(mt.name, c) for c in range(8)], w=[R('mT', t) for t in tiles])
                S.phase_end()
            if stop == ('merge', l):
                break

            with ExitStack() as ph:
                wout = T(ph, "wout", [128, 8, D], BF16)
                mTs = [T(ph, "mTo%d" % i, [128, 8, 512], BF16) for i in range(2)]
                xts = [T(ph, "xto%d" % i, [128, 4, D], F32) for i in range(2)]
                gtb = T(ph, "gtb", [128, 2, D], F32)
                tmps = [T(ph, "tmpo%d" % i, [128, 512], F32) for i in range(3)]
                S.dma('pool', wout[:], w_out[l].rearrange("(k p) n -> p k n", p=128), own=R('wout'), w=[R('wout')])
                for kd in range(2):
                    S.dma('sp', gtb[:, kd, :], bass.AP(modd.tensor, modd[l, kd, 2 * D:3 * D].offset, [[0, 128], [1, D]]),
                          own=R('gtb'), r=[R('modd', l)], w=[R('gtb')])

                def load_m(mt, xt, tiles):
                    ntok = len(tiles) * 128
                    t0 = tiles[0] * 128
                    S.dma('sp', mt[:, :, 0:ntok], mT_d[:, :, t0:t0 + ntok].rearrange("c p t -> p c t"), own=R(mt.name),
                          r=[R('mT', t) for t in tiles], w=[R(mt.name)])
                    S.dma('sp', xt[:, 0:len(tiles), :], x_src(l, tiles), own=R(xt.name), r=x_res(l, tiles), w=[R(xt.name)])

                load_m(mTs[0], xts[0], up_groups[0])
                cnt = 0
                for gi, tiles in enumerate(up_groups):
                    n = len(tiles)
                    mt, xt = mTs[gi % 2], xts[gi % 2]
                    kd = kind_of(tiles[0])
                    if gi + 1 < len(up_groups):
                        load_m(mTs[(gi + 1) % 2], xts[(gi + 1) % 2], up_groups[gi + 1])
                    for ts in range(n):
                        for hf in range(2):
                            pb = cnt % 4
                            tm = tmps[cnt % 3]
                            cnt += 1
                            for k in range(8):
                                S.op('pe', lambda e: e.matmul(ps[pb][:, :], lhsT=mt[:, k, ts * 128:(ts + 1) * 128], rhs=wout[:, k, hf * 512:(hf + 1) * 512],
                                                              start=(k == 0), stop=(k == 7)),
                                     r=[R(mt.name), R('wout')], w=[PR(pb)])
                            S.op('dve', lambda e: e.tensor_tensor(tm[:], ps[pb][:, :], gtb[:, kd, hf * 512:(hf + 1) * 512], ALU.mult),
                                 r=[PR(pb), R('gtb')], w=[R(tm.name)])
                            S.op('pool', lambda e: e.tensor_tensor(xt[:, ts, hf * 512:(hf + 1) * 512], xt[:, ts, hf * 512:(hf + 1) * 512], tm[:], ALU.add),
                                 r=[R(tm.name), R(xt.name)], w=[R(xt.name)])
                    S.dma('sp', xs_d[tiles[0] * 128:(tiles[0] + n) * 128, :].rearrange("(t p) d -> p t d", p=128), xt[:, 0:n, :],
                          own=R(xt.name), r=[R(xt.name)], w=[R('xs', t) for t in tiles])
                S.phase_end()
            if stop == ('oproj', l):
                break

            if SPARSE:
                moe_sparse(l, need_ctx)
                if stop == ('moe', l):
                    break
                continue
            last = (l == DEPTH - 1)
            moe_tiles = list(range(34)) if need_ctx else list(range(32))
            nsg = 3
            per = (len(moe_tiles) + nsg - 1) // nsg
            SGS = [moe_tiles[i * per:(i + 1) * per] for i in range(nsg)]
            with ExitStack() as ph:
                tT_sb = T(ph, "tT_sb", [128, 8, per * 128], BF16)
                yacc = T(ph, "yacc", [128, per, D], F32)
                wgu = [T(ph, "wgu%d" % i, [128, 8, 1024], BF16) for i in range(2)]
                wdn = [T(ph, "wdn%d" % i, [128, 4, 1024], BF16) for i in range(2)]
                aT = [T(ph, "aT%d" % i, [128, 4, 512], BF16) for i in range(2)]
                sgt = [T(ph, "sgm%d" % i, [128, 512], BF16) for i in range(3)]
                comb = T(ph, "comb", [128, per, 32], F32)
                xt = T(ph, "xtm", [128, 4, D], F32)
                junk = T(ph, "junkm", [128, D], BF16)
                diag = T(ph, "diagm", [128, 4, 128], F32)
                ssb = T(ph, "ssbm", [128, 12], F32)
                tT32 = T(ph, "tT32", [128, 8, 512], F32)
                wr32 = T(ph, "wr32", [128, 8, 36], F32)
                brt = T(ph, "brt", [128, 36], F32)
                gt2 = T(ph, "gt2", [128, 2, D], F32)
                gfin = T(ph, "gfin", [128, D], F32)
                lg = [T(ph, "lg%d" % i, [128, 36], F32) for i in range(2)]
                rt = [T(ph, "rt%d" % i, [128, 128], F32) for i in range(2)]
                S.dma('sp', wr32[:, :, 0:4], w_rg[l].rearrange("(k p) n -> p k n", p=128), own=R('wr32'), w=[R('wr32')])
                S.dma('sp', wr32[:, :, 4:36], w_re[l].rearrange("(k p) n -> p k n", p=128), own=R('wr32'), w=[R('wr32')])
                S.dma('sp', brt[:, 0:4], bass.AP(b_rg.tensor, b_rg[l].offset, [[0, 128], [1, 4]]), own=R('brt'), w=[R('brt')])
                S.dma('sp', brt[:, 4:36], bass.AP(b_re.tensor, b_re[l].offset, [[0, 128], [1, 32]]), own=R('brt'), w=[R('brt')])
                for kd in range(2):
                    S.dma('sp', gt2[:, kd, :], bass.AP(modd.tensor, modd[l, kd, 5 * D:6 * D].offset, [[0, 128], [1, D]]),
                          own=R('gt2'), r=[R('modd', l)], w=[R('gt2')])
                S.dma('sp', gfin[:], bass.AP(g_final.tensor, g_final.offset, [[0, 128], [1, D]]), own=R('gfin'), w=[R('gfin')])

                def load_exp(e_, slot):
                    S.dma('pool', wgu[slot][:, :, 0:512], w_eg[l, e_].rearrange("(k p) n -> p k n", p=128),
                          own=R('wgu', slot, 0), w=[R('wgu', slot)])
                    S.dma('pool', wgu[slot][:, :, 512:1024], w_eu[l, e_].rearrange("(k p) n -> p k n", p=128),
                          own=R('wgu', slot, 1), w=[R('wgu', slot)])
                    S.dma('pool', wdn[slot][:], w_ed[l, e_].rearrange("(k p) n -> p k n", p=128),
                          own=R('wdn', slot), w=[R('wdn', slot)])

                ecnt = 0
                for sgi, sgt_tiles in enumerate(SGS):
                    chunks = []
                    cur = []
                    for t in sgt_tiles:
                        if cur and (len(cur) == 4 or kind_of(cur[0]) != kind_of(t)):
                            chunks.append(cur)
                            cur = []
                        cur.append(t)
                    if cur:
                        chunks.append(cur)
                    base = sgt_tiles[0]
                    load_exp(0, ecnt % 2)
                    for ch in chunks:
                        n = len(ch)
                        ntok = n * 128
                        off = (ch[0] - base) * 128
                        norm_mod_T(l, 2, ch, xt, junk, diag, ssb,
                                   lambda c: (tT_sb[:, c, off:off + ntok], [R('tT', t) for t in ch]),
                                   lambda c: (tT32[:, c, 0:ntok], [R('tT32', c)]),
                                   [0, 1, 2, 3], x_src(l, ch, True), x_res(l, ch, True))
                        for ti, tile in enumerate(ch):
                            lt = tile - base
                            b = lt % 2
                            L, Rt = lg[b], rt[b]
                            for k in range(8):
                                S.op('pe', lambda e: e.matmul(ps[4 + b][:, 0:36], lhsT=tT32[:, k, ti * 128:(ti + 1) * 128], rhs=wr32[:, k, :],
                                                              start=(k == 0), stop=(k == 7)),
                                     r=[R('tT32', k), R('wr32')], w=[PR(4 + b)])
                            S.op('dve', lambda e: e.tensor_tensor(L[:], ps[4 + b][:, 0:36], brt[:], ALU.add), r=[PR(4 + b), R('brt')], w=[R(L.name)])
                            S.op('dve', lambda e: e.reduce_max(Rt[:, 0:1], L[:, 0:4], axis=AX.X), r=[R(L.name)], w=[R(Rt.name, 0)])
                            S.op('dve', lambda e: e.tensor_scalar(Rt[:, 1:2], Rt[:, 0:1], -1.0, None, ALU.mult), r=[R(Rt.name, 0)], w=[R(Rt.name, 1)])
                            S.op('act', lambda e: e.activation(Rt[:, 12:16], L[:, 0:4], AF.Exp, bias=Rt[:, 1:2], scale=1.0, accum_out=Rt[:, 2:3]),
                                 r=[R(L.name), R(Rt.name, 1)], w=[R(Rt.name, 2)])
                            S.op('dve', lambda e: e.reciprocal(Rt[:, 3:4], Rt[:, 2:3]), r=[R(Rt.name, 2)], w=[R(Rt.name, 3)])
                            S.op('dve', lambda e: e.tensor_scalar(Rt[:, 4:8], L[:, 0:4], Rt[:, 0:1], None, ALU.is_equal),
                                 r=[R(L.name), R(Rt.name, 0)], w=[R(Rt.name, 4)])
                            S.op('dve', lambda e: e.tensor_scalar(Rt[:, 8:12], Rt[:, 4:8], -1.0, 1e30, ALU.add, ALU.mult),
                                 r=[R(Rt.name, 4)], w=[R(Rt.name, 5)])
                            S.op('dve', lambda e: e.tensor_tensor(Rt[:, 16:48].rearrange("p (g k) -> p g k", g=4),
                                                                  L[:, 4:36].rearrange("p (g k) -> p g k", g=4),
                                                                  mk(Rt, 0, 128, 8, [[1, 4], [0, 8]]), ALU.add),
                                 r=[R(L.name), R(Rt.name, 5)], w=[R(Rt.name, 6)])
                            S.op('dve', lambda e: e.reduce_max(Rt[:, 48:49], Rt[:, 16:48], axis=AX.X), r=[R(Rt.name, 6)], w=[R(Rt.name, 7)])
                            S.op('dve', lambda e: e.tensor_scalar(Rt[:, 56:88], Rt[:, 16:48], Rt[:, 48:49], None, ALU.is_equal),
                                 r=[R(Rt.name, 6), R(Rt.name, 7)], w=[R(Rt.name, 8)])
                            S.op('dve', lambda e: e.scalar_tensor_tensor(Rt[:, 88:120], Rt[:, 56:88], -1e30, Rt[:, 16:48], ALU.mult, ALU.add),
                                 r=[R(Rt.name, 8), R(Rt.name, 6)], w=[R(Rt.name, 9)])
                            S.op('dve', lambda e: e.reduce_max(Rt[:, 49:50], Rt[:, 88:120], axis=AX.X), r=[R(Rt.name, 9)], w=[R(Rt.name, 10)])
                            S.op('dve', lambda e: e.tensor_scalar(Rt[:, 88:120], Rt[:, 88:120], Rt[:, 49:50], None, ALU.is_equal),
                                 r=[R(Rt.name, 9), R(Rt.name, 10)], w=[R(Rt.name, 11)])
                            S.op('dve', lambda e: e.tensor_scalar(Rt[:, 50:51], Rt[:, 48:49], -1.0, None, ALU.mult), r=[R(Rt.name, 7)], w=[R(Rt.name, 12)])
                            S.op('act', lambda e: e.activation(Rt[:, 51:52], Rt[:, 49:50], AF.Exp, bias=Rt[:, 50:51], scale=1.0),
                                 r=[R(Rt.name, 10), R(Rt.name, 12)], w=[R(Rt.name, 13)])
                            S.op('dve', lambda e: e.tensor_scalar(Rt[:, 52:53], Rt[:, 51:52], 1.0, None, ALU.add), r=[R(Rt.name, 13)], w=[R(Rt.name, 14)])
                            S.op('dve', lambda e: e.reciprocal(Rt[:, 53:54], Rt[:, 52:53]), r=[R(Rt.name, 14)], w=[R(Rt.name, 15)])
                            S.op('dve', lambda e: e.tensor_tensor(Rt[:, 53:54], Rt[:, 53:54], Rt[:, 3:4], ALU.mult),
                                 r=[R(Rt.name, 15), R(Rt.name, 3)], w=[R(Rt.name, 15)])
                            S.op('dve', lambda e: e.tensor_tensor(Rt[:, 54:55], Rt[:, 53:54], Rt[:, 51:52], ALU.mult),
                                 r=[R(Rt.name, 15), R(Rt.name, 13)], w=[R(Rt.name, 16)])
                            S.op('dve', lambda e: e.tensor_scalar(Rt[:, 56:88], Rt[:, 56:88], Rt[:, 53:54], None, ALU.mult),
                                 r=[R(Rt.name, 8), R(Rt.name, 15)], w=[R(Rt.name, 8)])
                            S.op('dve', lambda e: e.scalar_tensor_tensor(comb[:, lt, :], Rt[:, 88:120], Rt[:, 54:55], Rt[:, 56:88], ALU.mult, ALU.add),
                                 r=[R(Rt.name, 11), R(Rt.name, 16), R(Rt.name, 8)], w=[R('comb', lt)])
                    nsgt = len(sgt_tiles)
                    S.op('pool', lambda e: e.memset(yacc[:, 0:nsgt, :], 0.0), w=[R('yacc', t) for t in range(nsgt)])
                    pcnt = 0
                    for e_ in range(32):
                        slot = ecnt % 2
                        ecnt += 1
                        if e_ + 1 < 32:
                            load_exp(e_ + 1, ecnt % 2)
                        for ch in chunks:
                            n = len(ch)
                            ntok = n * 128
                            off = (ch[0] - base) * 128
                            a_ = aT[pcnt % 2]
                            for fc in range(4):
                                pg = (pcnt * 4 + fc) % 2
                                pu = 2 + (pcnt * 4 + fc) % 2
                                sgb = sgt[(pcnt * 4 + fc) % 3]
                                for k in range(8):
                                    S.op('pe', lambda e: e.matmul(ps[pg][:, 0:ntok], lhsT=wgu[slot][:, k, fc * 128:(fc + 1) * 128],
                                                                  rhs=tT_sb[:, k, off:off + ntok], start=(k == 0), stop=(k == 7)),
                                         r=[R('wgu', slot)] + [R('tT', t) for t in ch], w=[PR(pg)])
                                for k in range(8):
                                    S.op('pe', lambda e: e.matmul(ps[pu][:, 0:ntok], lhsT=wgu[slot][:, k, 512 + fc * 128:512 + (fc + 1) * 128],
                                                                  rhs=tT_sb[:, k, off:off + ntok], start=(k == 0), stop=(k == 7)),
                                         r=[R('wgu', slot)] + [R('tT', t) for t in ch], w=[PR(pu)])
                                S.op('act', lambda e: e.activation(sgb[:, 0:ntok], ps[pg][:, 0:ntok], AF.Silu), r=[PR(pg)], w=[R(sgb.name)])
                                S.op('dve', lambda e: e.tensor_tensor(a_[:, fc, 0:ntok], sgb[:, 0:ntok], ps[pu][:, 0:ntok], ALU.mult),
                                     r=[R(sgb.name), PR(pu)], w=[R(a_.name, fc)])
                            for ti, tile in enumerate(ch):
                                lt = tile - base
                                for hf in range(2):
                                    py = 4 + (pcnt * 8 + ti * 2 + hf) % 4
                                    for fc in range(4):
                                        S.op('pe', lambda e: e.matmul(ps[py][:, :], lhsT=a_[:, fc, ti * 128:(ti + 1) * 128],
                                                                      rhs=wdn[slot][:, fc, hf * 512:(hf + 1) * 512], start=(fc == 0), stop=(fc == 3)),
                                             r=[R(a_.name, fc), R('wdn', slot)], w=[PR(py)])
                                    S.op('dve', lambda e: e.scalar_tensor_tensor(yacc[:, lt, hf * 512:(hf + 1) * 512], ps[py][:, :],
                                                                                 comb[:, lt, e_:e_ + 1], yacc[:, lt, hf * 512:(hf + 1) * 512],
                                                                                 ALU.mult, ALU.add),
                                         r=[PR(py), R('comb', lt), R('yacc', lt)], w=[R('yacc', lt)])
                            pcnt += 1
                    for ch in chunks:
                        n = len(ch)
                        kd = kind_of(ch[0])
                        S.dma('sp', xt[:, 0:n, :], x_src(l, ch, True), own=R(xt.name), r=x_res(l, ch, True), w=[R(xt.name)])
                        for ti, tile in enumerate(ch):
                            lt = tile - base
                            S.op('pool', lambda e: e.tensor_tensor(yacc[:, lt, :], yacc[:, lt, :], gt2[:, kd, :], ALU.mult),
                                 r=[R('yacc', lt), R('gt2')], w=[R('yacc', lt)])
                            S.op('pool', lambda e: e.tensor_tensor(xt[:, ti, :], xt[:, ti, :], yacc[:, lt, :], ALU.add),
                                 r=[R('yacc', lt), R(xt.name)], w=[R(xt.name)])
                        if not last:
                            S.dma('sp', xs_d[ch[0] * 128:(ch[0] + n) * 128, :].rearrange("(t p) d -> p t d", p=128), xt[:, 0:n, :],
                                  own=R(xt.name), r=[R(xt.name)], w=[R('xs', t) for t in ch])
                        else:
                            for ti in range(n):
                                S.op('act', lambda e: e.activation(junk[:], xt[:, ti, :], AF.Square, accum_out=ssb[:, ti:ti + 1]),
                                     r=[R(xt.name)], w=[R(junk.name), R(ssb.name, ti)])
                            S.op('dve', lambda e: e.tensor_scalar(ssb[:, 4:4 + n], ssb[:, 0:n], 1.0 / D, EPS, ALU.mult, ALU.add),
                                 r=[R(ssb.name, t) for t in range(n)], w=[R(ssb.name, 'm')])
                            S.op('pool', lambda e: e.tensor_tensor(ssb[:, 8:8 + n], ssb[:, 4:4 + n], mhalf[:, 0:n], ALU.pow),
                                 r=[R(ssb.name, 'm'), R('mhalf')], w=[R(ssb.name, 'r')])
                            for ti in range(n):
                                S.op('dve', lambda e: e.scalar_tensor_tensor(xt[:, ti, :], xt[:, ti, :], ssb[:, 8 + ti:9 + ti], gfin[:], ALU.mult, ALU.mult),
                                     r=[R(xt.name), R(ssb.name, 'r'), R('gfin')], w=[R(xt.name)])
                            S.dma('sp', out_d[ch[0] * 128:(ch[0] + n) * 128, :].rearrange("(t p) d -> p t d", p=128), xt[:, 0:n, :],
                                  own=R(xt.name), r=[R(xt.name)], w=[R('out', t) for t in ch])
                S.phase_end()
            if stop == ('moe', l):
                break
        S.barrier()
        build.stats = (S.nops, S.nwaits, S.nds)
    return nc


_CACHE = {}


def kernel(**inputs):
    consts = _host_consts()
    if 'nc' not in _CACHE:
        _CACHE['nc'] = build()
    nc = _CACHE['nc']
    f32 = lambda a: np.ascontiguousarray(np.asarray(a, dtype=np.float32))
    shared = {}
    for k in ('c_ctx', 'w_mod', 'b_mod', 'g_mix', 'g_ffn', 'w_in', 'pool_w', 'pool_scale', 'diff_norm_g',
              'gqa_q_norm', 'gqa_k_norm', 'w_branch', 'w_out', 'w_router_group', 'b_router_group',
              'w_router_expert', 'b_router_expert', 'w_exp_gate', 'w_exp_up', 'w_exp_down', 'g_final'):
        shared[k] = f32(inputs[k])
    shared['diff_lambda'] = f32(inputs['diff_lambda']).reshape(2, 128)
    shared['nat_rpb'] = f32(inputs['nat_rpb']).reshape(2, 60, 31)
    for k, v in consts.items():
        shared[k] = v
    x = f32(inputs['x'])
    ctx = f32(inputs['ctx'])
    c = f32(inputs['c'])
    in_maps = []
    for b in range(8):
        m = dict(shared)
        m['x'] = x[b]
        m['ctx'] = ctx[b]
        m['c'] = c[b]
        in_maps.append(m)
    res = run_bass_kernel_spmd(nc, in_maps, core_ids=list(range(8)))
    return np.stack([np.asarray(r['out'], dtype=np.float32) for r in res.results], axis=0)
```

```python
import math
from contextlib import ExitStack
import numpy as np
import concourse.bass as bass
import concourse.mybir as mybir
from concourse.bass_utils import run_bass_kernel_spmd

F32 = mybir.dt.float32
BF16 = mybir.dt.bfloat16
AF = mybir.ActivationFunctionType
ALU = mybir.AluOpType
AX = mybir.AxisListType

D = 1024
NLAT = 4096
NCTX = 256
NTOK = NLAT + NCTX
NT = NTOK // 128
DEPTH = 2
EPS = 1e-6
NEG = -1e30
SPARSE = True
I32 = mybir.dt.int32
DBG_BARRIER = False


class Sched:
    ENG = ('pe', 'act', 'dve', 'pool', 'sp')

    def __init__(self, nc, es):
        self.nc = nc
        self.es = es
        self.eh = {'pe': nc.tensor, 'act': nc.scalar, 'dve': nc.vector, 'pool': nc.gpsimd, 'sp': nc.sync}
        self.sem = {e: es.enter_context(nc.semaphore("sem_" + e)) for e in self.ENG}
        self.cnt = {e: 0 for e in self.ENG}
        self.seen = {e: {} for e in self.ENG}
        self.lastw = {}
        self.readers = {}
        self.dsem = {}
        self.free_ds = []
        self.nds = 0
        self.keep = set()
        self.keep_res = set()
        self.inputs = set()
        self.nops = 0
        self.nwaits = 0

    def _resolve(self, eng, deps):
        need = {}
        for tok, kind in deps:
            if tok is None:
                continue
            if tok[0] == 'E':
                f, idx = tok[1], tok[2]
                if f == eng and kind != 'raw':
                    continue
                key = ('E', f)
                h = self.sem[f]
            else:
                key = ('D', tok[1])
                idx = tok[2]
                h = tok[3]
            if self.seen[eng].get(key, 0) >= idx:
                continue
            if key not in need or need[key][1] < idx:
                need[key] = (h, idx)
        e = self.eh[eng]
        for key, (h, idx) in need.items():
            e.wait_ge(h, idx)
            self.seen[eng][key] = idx
            self.nwaits += 1

    def _deps(self, r, w):
        deps = []
        for x in r:
            if x in self.lastw:
                deps.append((self.lastw[x], 'raw'))
            elif x[0] not in self.inputs:
                raise KeyError("read of never-written resource %r" % (x,))
        for x in w:
            if x in self.lastw:
                deps.append((self.lastw[x], 'waw'))
            for t in self.readers.get(x, ()):
                deps.append((t, 'war'))
        return deps

    def _commit(self, tok, r, w):
        for x in w:
            self.lastw[x] = tok
            self.readers[x] = []
        for x in r:
            self.readers.setdefault(x, []).append(tok)

    def op(self, eng, fn, r=(), w=()):
        r = list(r)
        w = list(w)
        w += [x for x in r if x[0] == 'ps' and x not in w]
        self._resolve(eng, self._deps(r, w))
        ins = fn(self.eh[eng])
        self.cnt[eng] += 1
        ins.then_inc(self.sem[eng], 1)
        self._commit(('E', eng, self.cnt[eng]), r, w)
        self.nops += 1

    def dma(self, q, out, in_, own, r=(), w=(), chain=True, indirect=None, **kw):
        r = list(r)
        w = list(w)
        if own not in self.dsem:
            fl = [d for d in self.free_ds if d[4] == q]
            if fl:
                self.free_ds.remove(fl[0])
                self.dsem[own] = fl[0]
            else:
                h = self.es.enter_context(self.nc.semaphore("dsem%d" % self.nds))
                self.nds += 1
                self.dsem[own] = [h, 0, None, self.nds, q]
        ds = self.dsem[own]
        deps = self._deps(r, w)
        if chain:
            deps.append((ds[2], 'raw'))
        self._resolve(q, deps)
        if indirect is None:
            self.eh[q].dma_start(out=out, in_=in_, **kw).then_inc(ds[0], 16)
        else:
            kind, idx_ap = indirect
            off = bass.IndirectOffsetOnAxis(ap=idx_ap, axis=0)
            self.nc.gpsimd.indirect_dma_start(out=out, out_offset=(off if kind == 'scatter' else None),
                                              in_=in_, in_offset=(off if kind == 'gather' else None), **kw).then_inc(ds[0], 16)
        ds[1] += 16
        tok = ('D', ds[3], ds[1], ds[0])
        ds[2] = tok
        self._commit(tok, r, w)
        self.nops += 1

    def barrier(self, engines=None):
        engines = engines or self.ENG
        for e in engines:
            deps = [(('E', f, self.cnt[f]), 'raw') for f in self.ENG if f != e and self.cnt[f] > 0]
            for k, ds in list(self.dsem.items()) + [(None, d) for d in self.free_ds]:
                if ds[2] is not None and k not in self.keep:
                    deps.append((ds[2], 'raw'))
            self._resolve(e, deps)

    def phase_end(self):
        self.barrier()
        for k in list(self.dsem.keys()):
            if k not in self.keep:
                self.free_ds.append(self.dsem.pop(k))
        for k in self.lastw:
            if k[0] not in self.keep_res:
                self.lastw[k] = None
        self.readers.clear()


def R(*a):
    return a


def _band_mats():
    out = np.zeros((128, 20, 128), np.float32)
    for g, w in enumerate((2, 4, 8, 16)):
        def mat(J, dl, n):
            m = np.zeros((128, 128), np.float32)
            for q in range(128):
                t = J * 128 + q
                lo = max(t - w // 2, 0)
                hi = min(t + w - w // 2, n)
                for tp in range(lo, hi):
                    p = tp - (J + dl) * 128
                    if 0 <= p < 128:
                        m[p, q] += 1.0 / (hi - lo)
                p = t - (J + dl) * 128
                if 0 <= p < 128:
                    m[p, q] -= 1.0
            return m
        out[:, g * 5 + 0] = mat(5, -1, 1280)
        out[:, g * 5 + 1] = mat(5, 0, 1280)
        out[:, g * 5 + 2] = mat(5, 1, 1280)
        out[:, g * 5 + 3] = mat(0, 0, 1280)
        out[:, g * 5 + 4] = mat(9, 0, 1280)
    return out


def _rope_tabs(half):
    t = np.arange(NLAT)
    inv = (10000.0 ** (-np.arange(half, dtype=np.float32) / half)).astype(np.float32)
    cos = np.zeros((128, 32, 2, half), np.float32)
    sin = np.zeros((128, 32, 2, half), np.float32)
    for a, pos in enumerate((t // 64, t % 64)):
        ang = pos.astype(np.float32)[:, None] * inv[None, :]
        c = np.cos(ang).astype(np.float32).reshape(32, 128, half)
        s = np.sin(ang).astype(np.float32).reshape(32, 128, half)
        cos[:, :, a, :] = c.transpose(1, 0, 2)
        sin[:, :, a, :] = s.transpose(1, 0, 2)
    return cos, sin


def _nat_variant(j, t):
    if 2 <= j <= 29:
        return (t - j) + 2
    if j == 0:
        return 5 + t
    if j == 1:
        return 9 + t
    if j == 30:
        return 13 + (t - 28)
    return 17 + (t - 28)


def _nat_keytiles(j):
    rows = [2 * j, 2 * j + 1]
    lo = min(min(max(r - 4, 0), 56) for r in rows)
    hi = max(min(max(r - 4, 0), 56) + 7 for r in rows)
    return list(range(lo // 2, hi // 2 + 1))


def _nat_plan():
    plan = {}
    mask = np.full((128, 21, 128), NEG, np.float32)
    qc = np.arange(64)
    cs = np.clip(qc - 8, 0, 48)
    kc = np.arange(64)
    colvalid = (kc[None, :] >= cs[:, None]) & (kc[None, :] < cs[:, None] + 16)
    for j in range(32):
        for t in _nat_keytiles(j):
            v = _nat_variant(j, t)
            blocks = {}
            for a in range(2):
                for b in range(2):
                    qr = 2 * j + b
                    kr = 2 * t + a
                    rs = min(max(qr - 4, 0), 56)
                    ok = rs <= kr < rs + 8
                    blocks[(a, b)] = (kr - qr + 7) if ok else None
                    if ok:
                        m = np.where(colvalid, 0.0, NEG).astype(np.float32)
                        mask[b * 64:(b + 1) * 64, v, a * 64:(a + 1) * 64] = m[::-1, :]
            if v in plan:
                assert plan[v] == blocks
            plan[v] = blocks
    return plan, mask


def _host_consts():
    c = {}
    c['identf'] = np.eye(128, dtype=np.float32)
    anti = np.zeros((128, 128), np.float32)
    for b in range(2):
        for q in range(64):
            anti[b * 64 + 63 - q, b * 64 + q] = 1.0
    c['antii'] = anti
    c['band'] = _band_mats()
    c['cosg'], c['sing'] = _rope_tabs(16)
    c['cosd'], c['sind'] = _rope_tabs(8)
    _, c['natmask'] = _nat_plan()
    c['ltri'] = np.triu(np.ones((128, 128), np.float32), 1)
    c['ustrict'] = np.triu(np.ones((32, 32), np.float32), 1)
    c['thr'] = np.tile((128.0 * np.arange(100, dtype=np.float32))[None, :], (128, 1))
    c['iota'] = np.arange(128, dtype=np.float32).reshape(128, 1)
    return c


def build(stop=None, dbg=False):
    nc = bass.Bass("TRN2", target_bir_lowering=False)

    def din(name, shape, dt=F32):
        return nc.dram_tensor(name, list(shape), dt, kind="ExternalInput").ap()

    def dscr(name, shape, dt=F32):
        return nc.dram_tensor(name, list(shape), dt, kind=("ExternalOutput" if dbg else "Internal")).ap()

    x_in = din("x", [NLAT, D])
    ctx_in = din("ctx", [NCTX, D])
    c_in = din("c", [D])
    cctx_in = din("c_ctx", [D])
    w_mod = din("w_mod", [2, D, 6 * D])
    b_mod = din("b_mod", [2, 6 * D])
    g_mix = din("g_mix", [2, D])
    g_ffn = din("g_ffn", [2, D])
    w_in = din("w_in", [2, D, 6400])
    pool_w = din("pool_w", [2, 4, 64, 64])
    pool_scale = din("pool_scale", [2, 256])
    diff_lambda = din("diff_lambda", [2, 128])
    diff_norm_g = din("diff_norm_g", [2, 64])
    nat_rpb = din("nat_rpb", [2, 60, 31])
    gqa_q_norm = din("gqa_q_norm", [2, 64])
    gqa_k_norm = din("gqa_k_norm", [2, 64])
    w_branch = din("w_branch", [2, 4, 256, D])
    w_out = din("w_out", [2, D, D])
    w_rg = din("w_router_group", [2, D, 4])
    b_rg = din("b_router_group", [2, 4])
    w_re = din("w_router_expert", [2, D, 32])
    b_re = din("b_router_expert", [2, 32])
    w_eg = din("w_exp_gate", [2, 32, D, 512])
    w_eu = din("w_exp_up", [2, 32, D, 512])
    w_ed = din("w_exp_down", [2, 32, 512, D])
    g_final = din("g_final", [D])
    identf_in = din("identf", [128, 128])
    antii_in = din("antii", [128, 128])
    band_in = din("band", [128, 20, 128])
    cosg_in = din("cosg", [128, 32, 2, 16])
    sing_in = din("sing", [128, 32, 2, 16])
    cosd_in = din("cosd", [128, 32, 2, 8])
    sind_in = din("sind", [128, 32, 2, 8])
    natmask_in = din("natmask", [128, 21, 128])
    ltri_in = din("ltri", [128, 128])
    ustrict_in = din("ustrict", [32, 32])
    thr_in = din("thr", [128, 100])
    iota_in = din("iota", [128, 1])

    out_d = nc.dram_tensor("out", [NLAT, D], F32, kind="ExternalOutput").ap()
    xs_d = dscr("xs", [NTOK, D])
    hT_d = dscr("hT", [8, 128, NTOK], BF16)
    brT_d = dscr("brT", [4, 4, 64, NTOK], BF16)
    mT_d = dscr("mT", [8, 128, NTOK], BF16)
    modd = dscr("modd", [2, 2, 6 * D])
    rpbpad = dscr("rpbpad", [2, 60, 128])
    wbf_d = [nc.dram_tensor("wbf%d" % i, [32 * 128, 12288], BF16, kind="Internal").ap() for i in range(2)]
    xslot_d = nc.dram_tensor("xslot", [100 * 128, D], BF16, kind="Internal").ap()
    yslot_d = nc.dram_tensor("yslot", [100 * 128, D], F32, kind="Internal").ap()

    nat_plan, _ = _nat_plan()

    def kind_of(tile):
        return 0 if tile < 32 else 1

    GROUPS = [list(range(g * 4, g * 4 + 4)) for g in range(8)] + [[32, 33]]

    def x_src(l, tiles, after_mix=False):
        t0 = tiles[0]
        n = len(tiles)
        if l == 0 and not after_mix:
            if t0 < 32:
                return x_in[t0 * 128:(t0 + n) * 128, :].rearrange("(t p) d -> p t d", p=128)
            return ctx_in[(t0 - 32) * 128:(t0 - 32 + n) * 128, :].rearrange("(t p) d -> p t d", p=128)
        return xs_d[t0 * 128:(t0 + n) * 128, :].rearrange("(t p) d -> p t d", p=128)

    def x_res(l, tiles, after_mix=False):
        if l == 0 and not after_mix:
            return [R('xin', t) for t in tiles]
        return [R('xs', t) for t in tiles]

    with ExitStack() as es:
        S = Sched(nc, es)
        S.inputs.add('xin')

        _tn = [0]

        def T(ctx, name, shape, dt):
            _tn[0] += 1
            return ctx.enter_context(nc.sbuf_tensor("%s_%d" % (name, _tn[0]), list(shape), dt))

        def mk(t, p0, pn, off, dims):
            row = 1
            for s in t.shape[1:]:
                row *= int(s)
            return bass.AP(t, p0 * row + off, [[row, pn]] + [list(d) for d in dims])

        ps = [es.enter_context(nc.psum_tensor("ps%d" % i, [128, 512], F32)) for i in range(8)]
        psb = [p.bitcast(BF16) for p in ps]

        def PR(i):
            return R('ps', i)

        identF = T(es, "identF", [128, 128], F32)
        identB = T(es, "identB", [128, 128], BF16)
        antiB = T(es, "antiB", [128, 128], BF16)
        colsP = T(es, "colsP", [128, 64], F32)
        cols64 = T(es, "cols64", [64, 8], F32)
        sT = T(es, "sT", [128, 8, 2], F32)
        modP = T(es, "modP", [128, 2, 2, 4, 8], F32)
        nlam = T(es, "nlam", [128, 2], F32)
        gainG = T(es, "gainG", [128, 2, 8, 64], F32)
        gB = T(es, "gB", [128, 2, 64], F32)
        epsT = T(es, "epsT", [128, 1], F32)
        mhalf = T(es, "mhalf", [128, 16], F32)

        S.dma('sp', identF[:], identf_in, own=R('identF'), w=[R('identF')])
        S.dma('pool', identB[:], identf_in, own=R('identB'), w=[R('identB')])
        S.dma('pool', antiB[:], antii_in, own=R('antiB'), w=[R('antiB')])

        with ExitStack() as ph:
            rowsA = T(ph, "rowsA", [52, 128], F32)
            rows64 = T(ph, "rows64", [8, 64], F32)
            S.dma('sp', rowsA[0:8, :], c_in.rearrange("(r d) -> r d", d=128), own=R('rowsA'), w=[R('rowsA')])
            S.dma('sp', rowsA[8:16, :], cctx_in.rearrange("(r d) -> r d", d=128), own=R('rowsA'), w=[R('rowsA')])
            for l in range(2):
                b0 = 16 + l * 18
                S.dma('sp', rowsA[b0:b0 + 8, :], g_mix[l].rearrange("(r d) -> r d", d=128), own=R('rowsA'), w=[R('rowsA')])
                S.dma('sp', rowsA[b0 + 8:b0 + 16, :], g_ffn[l].rearrange("(r d) -> r d", d=128), own=R('rowsA'), w=[R('rowsA')])
                S.dma('sp', rowsA[b0 + 16:b0 + 18, :], pool_scale[l].rearrange("(r d) -> r d", d=128), own=R('rowsA'), w=[R('rowsA')])
                S.dma('sp', rows64[l * 4:l * 4 + 4, :], pool_scale[l].rearrange("(r d) -> r d", d=64), own=R('rows64'), w=[R('rows64')])
            S.op('pe', lambda e: e.transpose(ps[0][:, 0:52], rowsA[0:52, :], identF[0:52, 0:52]),
                 r=[R('rowsA'), R('identF')], w=[PR(0)])
            S.op('dve', lambda e: e.tensor_copy(colsP[:, 0:52], ps[0][:, 0:52]), r=[PR(0)], w=[R('colsP')])
            S.op('pe', lambda e: e.transpose(ps[1][0:64, 0:8], rows64[0:8, :], identF[0:8, 0:8]),
                 r=[R('rows64'), R('identF')], w=[PR(1)])
            S.op('dve', lambda e: e.tensor_copy(cols64[:, :], ps[1][0:64, 0:8]), r=[PR(1)], w=[R('cols64')])
            S.op('dve', lambda e: e.memset(epsT[:], EPS), w=[R('epsT')])
            S.op('pool', lambda e: e.memset(mhalf[:], -0.5), w=[R('mhalf')])
            S.op('act', lambda e: e.activation(mk(sT, 0, 128, 0, [[1, 2], [2, 8]]),
                                               colsP[:, 0:16].rearrange("p (a k) -> p a k", a=2), AF.Silu),
                 r=[R('colsP')], w=[R('sT')])

            wm = [T(ph, "wm%d" % i, [128, 8, 512], F32) for i in range(2)]
            bm = T(ph, "bm", [2, 6 * D], F32)
            modsb = T(ph, "modsb", [2, 6 * D], F32)
            raw = T(ph, "raw", [128, 48, 2], F32)
            dl = T(ph, "dl", [128, 128], F32)
            pr = T(ph, "pr", [128, 2, 32], F32)
            sm = T(ph, "sm", [128, 2], F32)
            for l in range(2):
                for kk in range(2):
                    S.dma('sp', bm[kk:kk + 1, :], b_mod[l:l + 1, :], own=R('bm'), w=[R('bm')])
                for n in range(12):
                    wb = wm[n % 2]
                    S.dma('sp', wb[:], w_mod[l][:, n * 512:(n + 1) * 512].rearrange("(k p) n -> p k n", p=128),
                          own=R('wm', n % 2), w=[R('wm', n % 2)])
                    pb = 2 + (n % 2)
                    for k in range(8):
                        S.op('pe', lambda e: e.matmul(ps[pb][0:2, :], lhsT=sT[:, k, :], rhs=wb[:, k, :],
                                                      start=(k == 0), stop=(k == 7)),
                             r=[R('sT'), R('wm', n % 2)], w=[PR(pb)])
                    S.op('dve', lambda e: e.tensor_tensor(modsb[0:2, n * 512:(n + 1) * 512], ps[pb][0:2, :],
                                                          bm[0:2, n * 512:(n + 1) * 512], ALU.add),
                         r=[PR(pb), R('bm')], w=[R('modsb')])
                S.dma('sp', modd[l], modsb[:], own=R('modsb'), r=[R('modsb')], w=[R('modd', l)])
                for j in range(48):
                    S.op('pe', lambda e: e.transpose(ps[4][:, j * 2:(j + 1) * 2], modsb[0:2, j * 128:(j + 1) * 128],
                                                     identF[0:2, 0:2]),
                         r=[R('modsb'), R('identF')], w=[PR(4)])
                S.op('dve', lambda e: e.tensor_copy(raw[:].rearrange("p j k -> p (j k)"), ps[4][:, 0:96]),
                     r=[PR(4)], w=[R('raw')])
                b0 = 16 + l * 18
                for kd in range(2):
                    S.op('dve', lambda e: e.scalar_tensor_tensor(modP[:, l, kd, 0, :], raw[:, 8:16, kd], 1.0,
                                                                 colsP[:, b0:b0 + 8], ALU.add, ALU.mult),
                         r=[R('raw'), R('colsP')], w=[R('modP')])
                    S.op('dve', lambda e: e.tensor_copy(modP[:, l, kd, 1, :], raw[:, 0:8, kd]), r=[R('raw')], w=[R('modP')])
                    S.op('dve', lambda e: e.scalar_tensor_tensor(modP[:, l, kd, 2, :], raw[:, 32:40, kd], 1.0,
                                                                 colsP[:, b0 + 8:b0 + 16], ALU.add, ALU.mult),
                         r=[R('raw'), R('colsP')], w=[R('modP')])
                    S.op('dve', lambda e: e.tensor_copy(modP[:, l, kd, 3, :], raw[:, 24:32, kd]), r=[R('raw')], w=[R('modP')])
                lam_init = 0.8 - 0.6 * math.exp(-0.3 * l)
                S.dma('sp', dl[:], bass.AP(diff_lambda.tensor, diff_lambda[l].offset, [[0, 128], [1, 128]]),
                      own=R('dl'), w=[R('dl')])
                S.op('dve', lambda e: e.tensor_tensor(pr[:], mk(dl, 0, 128, 0, [[64, 2], [1, 32]]),
                                                      mk(dl, 0, 128, 32, [[64, 2], [1, 32]]), ALU.mult),
                     r=[R('dl')], w=[R('pr')])
                S.op('dve', lambda e: e.reduce_sum(sm[:], pr[:], axis=AX.X), r=[R('pr')], w=[R('sm')])
                S.op('act', lambda e: e.activation(sm[:], sm[:], AF.Exp), r=[R('sm')], w=[R('sm')])
                S.op('dve', lambda e: e.tensor_tensor(nlam[:, l:l + 1], sm[:, 1:2], sm[:, 0:1], ALU.subtract),
                     r=[R('sm')], w=[R('nlam')])
                S.op('dve', lambda e: e.tensor_scalar(nlam[:, l:l + 1], nlam[:, l:l + 1], -lam_init, None, ALU.add),
                     r=[R('nlam')], w=[R('nlam')])
                S.dma('sp', gainG[:, l, 0:4, :], bass.AP(gqa_q_norm.tensor, gqa_q_norm[l].offset, [[0, 128], [0, 4], [1, 64]]),
                      own=R('gainG'), w=[R('gainG')])
                S.dma('sp', gainG[:, l, 4:8, :], bass.AP(gqa_k_norm.tensor, gqa_k_norm[l].offset, [[0, 128], [0, 4], [1, 64]]),
                      own=R('gainG'), w=[R('gainG')])
                S.dma('sp', gB[:, l, :], bass.AP(diff_norm_g.tensor, diff_norm_g[l].offset, [[0, 128], [1, 64]]),
                      own=R('gB'), w=[R('gB')])
                S.op('dve', lambda e: e.tensor_scalar(gB[:, l, :], gB[:, l, :], 1.0 - lam_init, None, ALU.mult),
                     r=[R('gB')], w=[R('gB')])
            S.phase_end()

        def norm_mod_T(l, which, tiles, xt, junk, diag, ssb, outB, out32, pbanks, srcap, srcres):
            norm_p1(tiles, xt, junk, diag, ssb, srcap, srcres)
            norm_p2(l, which, tiles, xt, diag, outB, out32, pbanks)

        def norm_p1(tiles, xt, junk, diag, ssb, srcap, srcres):
            n = len(tiles)
            S.dma('sp', xt[:, 0:n, :], srcap, own=R(xt.name), r=srcres, w=[R(xt.name)])
            for t in range(n):
                S.op('act', lambda e: e.activation(junk[:], xt[:, t, :], AF.Square, accum_out=ssb[:, t:t + 1]),
                     r=[R(xt.name)], w=[R(junk.name), R(ssb.name, t)])
            S.op('dve', lambda e: e.tensor_scalar(ssb[:, 4:4 + n], ssb[:, 0:n], 1.0 / D, EPS, ALU.mult, ALU.add),
                 r=[R(ssb.name, t) for t in range(n)], w=[R(ssb.name, 'm')])
            S.op('pool', lambda e: e.tensor_tensor(ssb[:, 8:8 + n], ssb[:, 4:4 + n], mhalf[:, 0:n], ALU.pow),
                 r=[R(ssb.name, 'm'), R('mhalf')], w=[R(ssb.name, 'r')])
            for t in range(n):
                S.op('pool' if t % 2 else 'dve',
                     lambda e: e.tensor_scalar(diag[:, t, :], identF[:], ssb[:, 8 + t:9 + t], None, ALU.mult),
                     r=[R(ssb.name, 'r'), R('identF')], w=[R(diag.name, t)])

        def norm_p2(l, which, tiles, xt, diag, outB, out32, pbanks):
            n = len(tiles)
            ntok = n * 128
            kd = kind_of(tiles[0])
            a_i = 0 if which == 1 else 2
            for c in range(8):
                pb = pbanks[c % len(pbanks)]
                for t in range(n):
                    S.op('pe', lambda e: e.matmul(ps[pb][:, t * 128:(t + 1) * 128], lhsT=xt[:, t, c * 128:(c + 1) * 128],
                                                  rhs=diag[:, t, :], start=True, stop=True),
                         r=[R(xt.name), R(diag.name, t)], w=[PR(pb)])
                A = modP[:, l, kd, a_i, c:c + 1]
                B = modP[:, l, kd, a_i + 1, c:c + 1]
                if outB is None:
                    o32, o32res = out32(c)
                    S.op('act' if c % 2 else 'dve',
                         (lambda e: e.activation(o32, ps[pb][:, 0:ntok], AF.Identity, bias=B, scale=A)) if c % 2 else
                         (lambda e: e.tensor_scalar(o32, ps[pb][:, 0:ntok], A, B, ALU.mult, ALU.add)),
                         r=[PR(pb), R('modP')], w=o32res)
                    continue
                ob, obres = outB(c)
                if out32 is None:
                    if c % 2 == 0:
                        S.op('dve', lambda e: e.tensor_scalar(ob, ps[pb][:, 0:ntok], A, B, ALU.mult, ALU.add),
                             r=[PR(pb), R('modP')], w=obres)
                    else:
                        S.op('act', lambda e: e.activation(ob, ps[pb][:, 0:ntok], AF.Identity, bias=B, scale=A),
                             r=[PR(pb), R('modP')], w=obres)
                else:
                    o32, o32res = out32(c)
                    S.op('dve', lambda e: e.tensor_scalar(ob, ps[pb][:, 0:ntok], A, B, ALU.mult, ALU.add),
                         r=[PR(pb), R('modP')], w=obres)
                    S.op('act', lambda e: e.activation(o32, ps[pb][:, 0:ntok], AF.Identity, bias=B, scale=A),
                         r=[PR(pb), R('modP')], w=o32res)

        def hT_view(tok0, ntok):
            return hT_d[:, :, tok0:tok0 + ntok].rearrange("c p t -> p c t")


        S.keep.add(R('wbfsem'))
        S.keep_res.add('wbf')

        def issue_casts(l):
            for e_ in range(32):
                rows = wbf_d[l][e_ * 128:(e_ + 1) * 128, :]
                S.dma('pool', rows[:, 0:4096].rearrange("p (k n) -> p k n", k=8),
                      w_eg[l, e_].rearrange("(k p) n -> p k n", p=128), own=R('wbfsem'), chain=False, w=[R('wbf', l, e_, 0)])
                S.dma('pool', rows[:, 4096:8192].rearrange("p (k n) -> p k n", k=8),
                      w_eu[l, e_].rearrange("(k p) n -> p k n", p=128), own=R('wbfsem'), chain=False, w=[R('wbf', l, e_, 1)])
                S.dma('pool', rows[:, 8192:12288].rearrange("p (k n) -> p k n", k=4),
                      w_ed[l, e_].rearrange("(k p) n -> p k n", p=128), own=R('wbfsem'), chain=False, w=[R('wbf', l, e_, 2)])

        def moe_sparse(l, need_ctx):
            last = (l == DEPTH - 1)
            moe_tiles = list(range(34)) if need_ctx else list(range(32))
            NTt = len(moe_tiles)
            NS = 100 if need_ctx else 96
            wbf_res = [R('wbf', l, e_, m) for e_ in range(32) for m in range(3)]
            with ExitStack() as ph:
                OHs = T(ph, "OHs", [128, NTt, 2, 32], F32)
                W12 = T(ph, "W12", [128, NTt, 2], F32)
                Pall = T(ph, "Pall", [128, NTt, 32], F32)
                posF = T(ph, "posF", [128, NTt, 2], F32)
                posI = T(ph, "posI", [128, NTt, 2], I32)
                idxW = T(ph, "idxW", [128, NS], I32)
                gt2 = T(ph, "gt2", [128, 2, D], F32)
                for kd in range(2):
                    S.dma('sp', gt2[:, kd, :], bass.AP(modd.tensor, modd[l, kd, 5 * D:6 * D].offset, [[0, 128], [1, D]]),
                          own=R('gt2'), r=[R('modd', l)], w=[R('gt2')])
                with ExitStack() as pa:
                    xt = T(pa, "xtm", [128, 4, D], F32)
                    junk = T(pa, "junkm", [128, D], BF16)
                    diag = T(pa, "diagm", [128, 4, 128], F32)
                    ssb = T(pa, "ssbm", [128, 12], F32)
                    tT32 = T(pa, "tT32", [128, 8, 512], F32)
                    wr32 = T(pa, "wr32", [128, 8, 36], F32)
                    brt = T(pa, "brt", [128, 36], F32)
                    A2b = T(pa, "A2b", [128, 2, D], F32)
                    B2b = T(pa, "B2b", [128, 2, D], F32)
                    ttok = T(pa, "ttok", [128, NTt, D], BF16)
                    tmpf = [T(pa, "tmpf%d" % i, [128, D], F32) for i in range(2)]
                    lg = [T(pa, "lg%d" % i, [128, 36], F32) for i in range(4)]
                    rt = [T(pa, "rt%d" % i, [128, 128], F32) for i in range(4)]
                    Cb = [T(pa, "Cb%d" % i, [128, 32], BF16) for i in range(4)]
                    Ccum = [T(pa, "Ccum%d" % i, [128, 32], BF16) for i in range(2)]
                    ltri = T(pa, "ltri", [128, 128], BF16)
                    onesB = T(pa, "onesB", [128, 128], BF16)
                    ustr = T(pa, "ustr", [32, 32], F32)
                    thr = T(pa, "thr", [128, 100], F32)
                    iota = T(pa, "iota", [128, 1], F32)
                    zt = T(pa, "zt", [128, D], BF16)
                    ncol = T(pa, "ncol", [32, 4], F32)
                    cmp32 = T(pa, "cmp32", [32, 100], F32)
                    npbc = T(pa, "npbc", [32, 128], F32)
                    offs = T(pa, "offs", [128, 32], F32)
                    cmpw = T(pa, "cmpw", [128, NS, 32], F32)
                    cntw = T(pa, "cntw", [128, NS], F32)
                    prod = T(pa, "prod", [128, NTt, 2, 32], F32)
                    onesF = T(pa, "onesF", [32, 1], F32)
                    totc = T(pa, "totc", [128, 1], F32)
                    unused = T(pa, "unused", [128, NS], F32)
                    S.op('dve', lambda e: e.memset(onesF[:], 1.0), w=[R('onesF')])
                    S.dma('sp', wr32[:, :, 0:4], w_rg[l].rearrange("(k p) n -> p k n", p=128), own=R('wr32'), w=[R('wr32')])
                    S.dma('sp', wr32[:, :, 4:36], w_re[l].rearrange("(k p) n -> p k n", p=128), own=R('wr32'), w=[R('wr32')])
                    S.dma('sp', brt[:, 0:4], bass.AP(b_rg.tensor, b_rg[l].offset, [[0, 128], [1, 4]]), own=R('brt'), w=[R('brt')])
                    S.dma('sp', brt[:, 4:36], bass.AP(b_re.tensor, b_re[l].offset, [[0, 128], [1, 32]]), own=R('brt'), w=[R('brt')])
                    S.dma('pool', ltri[:], ltri_in, own=R('ltri'), w=[R('ltri')])
                    S.dma('sp', ustr[:], ustrict_in, own=R('ustr'), w=[R('ustr')])
                    S.dma('sp', thr[:], thr_in, own=R('thr'), w=[R('thr')])
                    S.dma('sp', iota[:], iota_in, own=R('iota'), w=[R('iota')])
                    S.op('pool', lambda e: e.memset(onesB[:], 1.0), w=[R('onesB')])
                    S.op('pool', lambda e: e.memset(zt[:], 0.0), w=[R('zt')])
                    S.op('pool', lambda e: e.memset(Ccum[0][:], 0.0), w=[R(Ccum[0].name)])
                    S.dma('sp', xslot_d[0:NS * 128, :].rearrange("(i p) d -> p i d", p=128),
                          mk(zt, 0, 128, 0, [[0, NS], [1, D]]), own=R('zt'), r=[R('zt')], w=[R('xslot0')])
                    for kd in range(2):
                        S.dma('sp', A2b[:, kd, :], bass.AP(modd.tensor, modd[l, kd, 4 * D:5 * D].offset, [[0, 128], [1, D]]),
                              own=R('A2b'), r=[R('modd', l)], w=[R('A2b')])
                        S.dma('sp', B2b[:, kd, :], bass.AP(modd.tensor, modd[l, kd, 3 * D:4 * D].offset, [[0, 128], [1, D]]),
                              own=R('B2b'), r=[R('modd', l)], w=[R('B2b')])
                    S.dma('sp', tmpf[0][:], bass.AP(g_ffn.tensor, g_ffn[l].offset, [[0, 128], [1, D]]), own=R(tmpf[0].name), w=[R(tmpf[0].name)])
                    for kd in range(2):
                        S.op('dve', lambda e: e.scalar_tensor_tensor(A2b[:, kd, :], A2b[:, kd, :], 1.0, tmpf[0][:], ALU.add, ALU.mult),
                             r=[R('A2b'), R(tmpf[0].name)], w=[R('A2b')])
                    chunks = []
                    cur = []
                    for t in moe_tiles:
                        if cur and (len(cur) == 4 or kind_of(cur[0]) != kind_of(t)):
                            chunks.append(cur)
                            cur = []
                        cur.append(t)
                    chunks.append(cur)
                    for ch in chunks:
                        n = len(ch)
                        ntok = n * 128
                        kd = kind_of(ch[0])
                        norm_mod_T(l, 2, ch, xt, junk, diag, ssb, None,
                                   lambda c: (tT32[:, c, 0:ntok], [R('tT32', c)]),
                                   [0, 1, 2, 3], x_src(l, ch, True), x_res(l, ch, True))
                        tl = list(enumerate(ch))
                        for ti, lt in tl:
                            b = ti
                            S.op('dve', lambda e: e.scalar_tensor_tensor(tmpf[b % 2][:], xt[:, ti, :], ssb[:, 8 + ti:9 + ti], A2b[:, kd, :], ALU.mult, ALU.mult),
                                 r=[R(xt.name), R(ssb.name, 'r'), R('A2b')], w=[R(tmpf[b % 2].name)])
                            S.op('pool', lambda e: e.tensor_tensor(ttok[:, lt, :], tmpf[b % 2][:], B2b[:, kd, :], ALU.add),
                                 r=[R(tmpf[b % 2].name), R('B2b')], w=[R('ttok', lt)])
                        for ti, lt in tl:
                            for k in range(8):
                                S.op('pe', lambda e: e.matmul(ps[4][:, ti * 64:ti * 64 + 36], lhsT=tT32[:, k, ti * 128:(ti + 1) * 128], rhs=wr32[:, k, :],
                                                              start=(k == 0), stop=(k == 7)),
                                     r=[R('tT32', k), R('wr32')], w=[PR(4)])
                        for ti, lt in tl:
                            L, Rt = lg[ti], rt[ti]
                            S.op('dve', lambda e: e.tensor_tensor(L[:], ps[4][:, ti * 64:ti * 64 + 36], brt[:], ALU.add), r=[PR(4), R('brt')], w=[R(L.name)])
                            S.op('dve', lambda e: e.reduce_max(Rt[:, 0:1], L[:, 0:4], axis=AX.X), r=[R(L.name)], w=[R(Rt.name, 0)])
                            S.op('dve', lambda e: e.tensor_scalar(Rt[:, 1:2], Rt[:, 0:1], -1.0, None, ALU.mult), r=[R(Rt.name, 0)], w=[R(Rt.name, 1)])
                        for ti, lt in tl:
                            L, Rt = lg[ti], rt[ti]
                            S.op('act', lambda e: e.activation(Rt[:, 12:16], L[:, 0:4], AF.Exp, bias=Rt[:, 1:2], scale=1.0, accum_out=Rt[:, 2:3]),
                                 r=[R(L.name), R(Rt.name, 1)], w=[R(Rt.name, 2)])
                        for ti, lt in tl:
                            L, Rt = lg[ti], rt[ti]
                            oh1 = OHs[:, lt, 0, :]
                            oh2 = OHs[:, lt, 1, :]
                            S.op('dve', lambda e: e.reciprocal(Rt[:, 3:4], Rt[:, 2:3]), r=[R(Rt.name, 2)], w=[R(Rt.name, 3)])
                            S.op('dve', lambda e: e.tensor_scalar(Rt[:, 4:8], L[:, 0:4], Rt[:, 0:1], None, ALU.is_equal),
                                 r=[R(L.name), R(Rt.name, 0)], w=[R(Rt.name, 4)])
                            S.op('dve', lambda e: e.tensor_scalar(Rt[:, 8:12], Rt[:, 4:8], -1.0, 1e30, ALU.add, ALU.mult),
                                 r=[R(Rt.name, 4)], w=[R(Rt.name, 5)])
                            S.op('dve', lambda e: e.tensor_tensor(Rt[:, 16:48].rearrange("p (g k) -> p g k", g=4),
                                                                  L[:, 4:36].rearrange("p (g k) -> p g k", g=4),
                                                                  mk(Rt, 0, 128, 8, [[1, 4], [0, 8]]), ALU.add),
                                 r=[R(L.name), R(Rt.name, 5)], w=[R(Rt.name, 6)])
                            S.op('dve', lambda e: e.reduce_max(Rt[:, 48:49], Rt[:, 16:48], axis=AX.X), r=[R(Rt.name, 6)], w=[R(Rt.name, 7)])
                            S.op('dve', lambda e: e.tensor_scalar(oh1, Rt[:, 16:48], Rt[:, 48:49], None, ALU.is_equal),
                                 r=[R(Rt.name, 6), R(Rt.name, 7)], w=[R('oh', lt, 0)])
                            S.op('dve', lambda e: e.scalar_tensor_tensor(Rt[:, 88:120], oh1, -1e30, Rt[:, 16:48], ALU.mult, ALU.add),
                                 r=[R('oh', lt, 0), R(Rt.name, 6)], w=[R(Rt.name, 9)])
                            S.op('dve', lambda e: e.reduce_max(Rt[:, 49:50], Rt[:, 88:120], axis=AX.X), r=[R(Rt.name, 9)], w=[R(Rt.name, 10)])
                            S.op('dve', lambda e: e.tensor_scalar(oh2, Rt[:, 88:120], Rt[:, 49:50], None, ALU.is_equal),
                                 r=[R(Rt.name, 9), R(Rt.name, 10)], w=[R('oh', lt, 1)])
                            S.op('dve', lambda e: e.tensor_scalar(Rt[:, 50:51], Rt[:, 48:49], -1.0, None, ALU.mult), r=[R(Rt.name, 7)], w=[R(Rt.name, 12)])
                        for ti, lt in tl:
                            Rt = rt[ti]
                            S.op('act', lambda e: e.activation(Rt[:, 51:52], Rt[:, 49:50], AF.Exp, bias=Rt[:, 50:51], scale=1.0),
                                 r=[R(Rt.name, 10), R(Rt.name, 12)], w=[R(Rt.name, 13)])
                        for ti, lt in tl:
                            Rt = rt[ti]
                            oh1 = OHs[:, lt, 0, :]
                            oh2 = OHs[:, lt, 1, :]
                            S.op('dve', lambda e: e.tensor_scalar(Rt[:, 52:53], Rt[:, 51:52], 1.0, None, ALU.add), r=[R(Rt.name, 13)], w=[R(Rt.name, 14)])
                            S.op('dve', lambda e: e.reciprocal(Rt[:, 53:54], Rt[:, 52:53]), r=[R(Rt.name, 14)], w=[R(Rt.name, 15)])
                            S.op('dve', lambda e: e.tensor_tensor(W12[:, lt, 0:1], Rt[:, 53:54], Rt[:, 3:4], ALU.mult),
                                 r=[R(Rt.name, 15), R(Rt.name, 3)], w=[R('w12', lt, 0)])
                            S.op('dve', lambda e: e.tensor_tensor(W12[:, lt, 1:2], W12[:, lt, 0:1], Rt[:, 51:52], ALU.mult),
                                 r=[R('w12', lt, 0), R(Rt.name, 13)], w=[R('w12', lt, 1)])
                            S.op('dve', lambda e: e.tensor_tensor(Cb[ti][:], oh1, oh2, ALU.add), r=[R('oh', lt, 0), R('oh', lt, 1)], w=[R(Cb[ti].name)])
                        for ti, lt in tl:
                            cb = Cb[ti]
                            pbk = 6 + (ti % 2)
                            cprev, cnext = Ccum[lt % 2], Ccum[(lt + 1) % 2]
                            S.op('pe', lambda e: e.matmul(ps[pbk][:, 0:32], lhsT=ltri[:], rhs=cb[:], start=True, stop=False),
                                 r=[R('ltri'), R(cb.name)], w=[PR(pbk)])
                            S.op('pe', lambda e: e.matmul(ps[pbk][:, 0:32], lhsT=onesB[:], rhs=cprev[:], start=False, stop=True),
                                 r=[R('onesB'), R(cprev.name)], w=[PR(pbk)])
                            S.op('act', lambda e: e.activation(Pall[:, lt, :], ps[pbk][:, 0:32], AF.Copy), r=[PR(pbk)], w=[R('Pall', lt)])
                            S.op('pool', lambda e: e.tensor_tensor(cnext[:], cprev[:], cb[:], ALU.add),
                                 r=[R(cprev.name), R(cb.name)], w=[R(cnext.name)])

                    cfin = Ccum[NTt % 2]
                    S.op('pe', lambda e: e.matmul(ps[7][0:32, 0:1], lhsT=cfin[:], rhs=onesB[:, 0:1], start=True, stop=True),
                         r=[R(cfin.name), R('onesB')], w=[PR(7)])
                    S.op('dve', lambda e: e.tensor_copy(ncol[:, 0:1], ps[7][0:32, 0:1]), r=[PR(7)], w=[R('ncol', 0)])
                    S.op('dve', lambda e: e.tensor_scalar(cmp32[:], thr[0:32, :], ncol[:, 0:1], None, ALU.is_lt),
                         r=[R('thr'), R('ncol', 0)], w=[R('cmp32')])
                    S.op('dve', lambda e: e.reduce_sum(ncol[:, 1:2], cmp32[:], axis=AX.X), r=[R('cmp32')], w=[R('ncol', 1)])
                    S.op('dve', lambda e: e.tensor_scalar(ncol[:, 2:3], ncol[:, 1:2], 128.0, None, ALU.mult), r=[R('ncol', 1)], w=[R('ncol', 2)])
                    S.op('dve', lambda e: e.tensor_copy(npbc[:], mk(ncol, 0, 32, 2, [[0, 128]])), r=[R('ncol', 2)], w=[R('npbc')])
                    S.op('pe', lambda e: e.matmul(ps[7][:, 32:64], lhsT=npbc[:], rhs=ustr[:], start=True, stop=True),
                         r=[R('npbc'), R('ustr')], w=[PR(7)])
                    S.op('dve', lambda e: e.tensor_copy(offs[:], ps[7][:, 32:64]), r=[PR(7)], w=[R('offs')])
                    S.op('dve', lambda e: e.tensor_tensor(cmpw[:], mk(offs, 0, 128, 0, [[0, NS], [1, 32]]),
                                                          mk(thr, 0, 128, 0, [[1, NS], [0, 32]]), ALU.is_le),
                         r=[R('offs'), R('thr')], w=[R('cmpw')])
                    S.op('dve', lambda e: e.reduce_sum(cntw[:], cmpw[:], axis=AX.X), r=[R('cmpw')], w=[R('cntw')])
                    S.op('dve', lambda e: e.tensor_scalar(cntw[:], cntw[:], -1.0, 128.0, ALU.add, ALU.mult), r=[R('cntw')], w=[R('cntw')])
                    S.op('dve', lambda e: e.tensor_scalar(cntw[:], cntw[:], iota[:, 0:1], None, ALU.add), r=[R('cntw'), R('iota')], w=[R('cntw')])
                    S.op('dve', lambda e: e.tensor_copy(idxW[:], cntw[:]), r=[R('cntw')], w=[R('idxW')])
                    S.op('dve', lambda e: e.tensor_tensor(Pall[:], Pall[:], mk(offs, 0, 128, 0, [[0, NTt], [1, 32]]), ALU.add),
                         r=[R('Pall', t) for t in range(NTt)] + [R('offs')], w=[R('Pall2')])
                    S.op('dve', lambda e: e.tensor_tensor(prod[:], OHs[:], mk(Pall, 0, 128, 0, [[32, NTt], [0, 2], [1, 32]]), ALU.mult),
                         r=[R('Pall2')] + [R('oh', t, k) for t in range(NTt) for k in range(2)], w=[R('prod')])
                    S.op('dve', lambda e: e.reduce_sum(posF[:].rearrange("p t k -> p (t k)"), prod[:].rearrange("p t k e -> p (t k) e"), axis=AX.X),
                         r=[R('prod')], w=[R('posF')])
                    S.op('dve', lambda e: e.tensor_copy(posI[:], posF[:]), r=[R('posF')], w=[R('posI')])
                    for lt in range(NTt):
                        for k in range(2):
                            S.dma('pool', xslot_d[:, :], ttok[:, lt, :], own=R('scat', (lt * 2 + k) % 4),
                                  r=[R('ttok', lt), R('posI'), R('xslot0')], w=[R('xslot', lt, k)],
                                  indirect=('scatter', posI[:, lt, k:k + 1]))
                    S.barrier()
                xslot_res = [R('xslot', lt, k) for lt in range(NTt) for k in range(2)]
                with ExitStack() as pb_:
                    wt = [T(pb_, "wt%d" % i, [128, 12288], BF16) for i in range(2)]
                    xs = [T(pb_, "xs%d" % i, [128, D], BF16) for i in range(2)]
                    xT = [T(pb_, "xT%d" % i, [128, 8, 128], BF16) for i in range(2)]
                    sgb = [T(pb_, "sgb%d" % i, [128, 512], BF16) for i in range(2)]
                    aT = [T(pb_, "aTs%d" % i, [128, 4, 128], BF16) for i in range(2)]
                    ysb = [T(pb_, "ysb%d" % i, [128, D], F32) for i in range(2)]

                    def fetch(i):
                        S.dma('pool', wt[i % 2][:, :], wbf_d[l][:, :], own=R('wt', i % 2),
                              r=[R('idxW')] + wbf_res, w=[R('wt', i % 2)], indirect=('gather', idxW[:, i:i + 1]))
                        S.dma('sp', xs[i % 2][:], xslot_d[i * 128:(i + 1) * 128, :], own=R('xs', i % 2), r=xslot_res, w=[R('xs', i % 2)])

                    fetch(0)
                    for i in range(NS):
                        if i + 1 < NS:
                            fetch(i + 1)
                        w_, x_, xT_, sg_, a_, y_ = wt[i % 2], xs[i % 2], xT[i % 2], sgb[i % 2], aT[i % 2], ysb[i % 2]
                        tb = i % 2
                        for k in range(8):
                            S.op('pe', lambda e: e.transpose(psb[tb][:, k * 128:(k + 1) * 128], x_[:, k * 128:(k + 1) * 128], identB[:]),
                                 r=[R('xs', i % 2), R('identB')], w=[PR(tb)])
                        if i % 2:
                            S.op('dve', lambda e: e.tensor_copy(xT_[:].rearrange("p k s -> p (k s)"), psb[tb][:, 0:1024]), r=[PR(tb)], w=[R(xT_.name)])
                        else:
                            S.op('act', lambda e: e.activation(xT_[:].rearrange("p k s -> p (k s)"), psb[tb][:, 0:1024], AF.Copy), r=[PR(tb)], w=[R(xT_.name)])
                        G = 2 + (i % 2)
                        U = 4 + (i % 2)
                        for fc in range(4):
                            for k in range(8):
                                S.op('pe', lambda e: e.matmul(ps[G][:, fc * 128:(fc + 1) * 128], lhsT=w_[:, k * 512 + fc * 128:k * 512 + (fc + 1) * 128],
                                                              rhs=xT_[:, k, :], start=(k == 0), stop=(k == 7)),
                                     r=[R('wt', i % 2), R(xT_.name)], w=[PR(G)])
                        for fc in range(4):
                            for k in range(8):
                                S.op('pe', lambda e: e.matmul(ps[U][:, fc * 128:(fc + 1) * 128],
                                                              lhsT=w_[:, 4096 + k * 512 + fc * 128:4096 + k * 512 + (fc + 1) * 128],
                                                              rhs=xT_[:, k, :], start=(k == 0), stop=(k == 7)),
                                     r=[R('wt', i % 2), R(xT_.name)], w=[PR(U)])
                        S.op('act', lambda e: e.activation(sg_[:], ps[G][:, :], AF.Silu), r=[PR(G)], w=[R(sg_.name)])
                        S.op('dve', lambda e: e.tensor_tensor(a_[:].rearrange("p f s -> p (f s)"), sg_[:], ps[U][:, :], ALU.mult),
                             r=[R(sg_.name), PR(U)], w=[R(a_.name)])
                        for hf in range(2):
                            Y = 6 + hf
                            for fc in range(4):
                                S.op('pe', lambda e: e.matmul(ps[Y][:, :], lhsT=a_[:, fc, :],
                                                              rhs=w_[:, 8192 + fc * 1024 + hf * 512:8192 + fc * 1024 + (hf + 1) * 512],
                                                              start=(fc == 0), stop=(fc == 3)),
                                     r=[R(a_.name), R('wt', i % 2)], w=[PR(Y)])
                            if hf:
                                S.op('dve', lambda e: e.tensor_copy(y_[:, hf * 512:(hf + 1) * 512], ps[Y][:, :]), r=[PR(Y)], w=[R(y_.name, hf)])
                            else:
                                S.op('act', lambda e: e.activation(y_[:, hf * 512:(hf + 1) * 512], ps[Y][:, :], AF.Copy), r=[PR(Y)], w=[R(y_.name, hf)])
                        S.dma('sp', yslot_d[i * 128:(i + 1) * 128, :], y_[:], own=R(y_.name, 0),
                              r=[R(y_.name, 0), R(y_.name, 1)], w=[R('yslot', i)])
                    S.barrier()
                yslot_res = [R('yslot', i) for i in range(NS)]
                with ExitStack() as pc_:
                    g1 = [T(pc_, "g1%d" % i, [128, D], F32) for i in range(2)]
                    g2 = [T(pc_, "g2%d" % i, [128, D], F32) for i in range(2)]
                    xq = [T(pc_, "xq%d" % i, [128, D], F32) for i in range(2)]
                    junk = T(pc_, "junkc", [128, D], BF16)
                    gfin = T(pc_, "gfin", [128, D], F32)
                    ssc = [T(pc_, "ssc%d" % i, [128, 4], F32) for i in range(2)]
                    S.dma('sp', gfin[:], bass.AP(g_final.tensor, g_final.offset, [[0, 128], [1, D]]), own=R('gfin'), w=[R('gfin')])
                    for lt in range(NTt):
                        tile = moe_tiles[lt]
                        b = lt % 2
                        kd = kind_of(tile)
                        a1, a2, xx, sc_ = g1[b], g2[b], xq[b], ssc[b]
                        S.dma('pool', a1[:, :], yslot_d[:, :], own=R(a1.name), r=[R('posI')] + yslot_res, w=[R(a1.name)],
                              indirect=('gather', posI[:, lt, 0:1]))
                        S.dma('pool', a2[:, :], yslot_d[:, :], own=R(a2.name), r=[R('posI')] + yslot_res, w=[R(a2.name)],
                              indirect=('gather', posI[:, lt, 1:2]))
                        S.dma('sp', xx[:], x_src(l, [tile], True)[:, 0, :], own=R(xx.name), r=x_res(l, [tile], True), w=[R(xx.name)])
                        S.op('dve', lambda e: e.tensor_scalar(a1[:], a1[:], W12[:, lt, 0:1], None, ALU.mult),
                             r=[R(a1.name), R('w12', lt, 0)], w=[R(a1.name)])
                        S.op('dve', lambda e: e.scalar_tensor_tensor(a1[:], a2[:], W12[:, lt, 1:2], a1[:], ALU.mult, ALU.add),
                             r=[R(a1.name), R(a2.name), R('w12', lt, 1)], w=[R(a1.name)])
                        S.op('dve', lambda e: e.tensor_tensor(a1[:], a1[:], gt2[:, kd, :], ALU.mult), r=[R(a1.name), R('gt2')], w=[R(a1.name)])
                        S.op('dve', lambda e: e.tensor_tensor(xx[:], xx[:], a1[:], ALU.add), r=[R(a1.name), R(xx.name)], w=[R(xx.name)])
                        if not last:
                            S.dma('sp', xs_d[tile * 128:(tile + 1) * 128, :], xx[:], own=R(xx.name), r=[R(xx.name)], w=[R('xs', tile)])
                        else:
                            S.op('act', lambda e: e.activation(junk[:], xx[:], AF.Square, accum_out=sc_[:, 0:1]),
                                 r=[R(xx.name)], w=[R(junk.name), R(sc_.name, 0)])
                            S.op('dve', lambda e: e.tensor_scalar(sc_[:, 1:2], sc_[:, 0:1], 1.0 / D, EPS, ALU.mult, ALU.add),
                                 r=[R(sc_.name, 0)], w=[R(sc_.name, 1)])
                            S.op('pool', lambda e: e.tensor_tensor(sc_[:, 2:3], sc_[:, 1:2], mhalf[:, 0:1], ALU.pow),
                                 r=[R(sc_.name, 1), R('mhalf')], w=[R(sc_.name, 2)])
                            S.op('dve', lambda e: e.scalar_tensor_tensor(xx[:], xx[:], sc_[:, 2:3], gfin[:], ALU.mult, ALU.mult),
                                 r=[R(xx.name), R(sc_.name, 2), R('gfin')], w=[R(xx.name)])
                            S.dma('sp', out_d[tile * 128:(tile + 1) * 128, :], xx[:], own=R(xx.name), r=[R(xx.name)], w=[R('out', tile)])
                S.phase_end()

        for l in range(DEPTH):
            need_ctx = l < DEPTH - 1
            lay = ExitStack()
            es.callback(lay.close)
            biasM = T(lay, "biasM", [128, 21, 4, 128], BF16)
            wqd_next = T(lay, "wqd_next", [128, 8, 768], BF16)
            with ExitStack() as ph:
                xts = [T(ph, "xt%d" % i, [128, 4, D], F32) for i in range(2)]
                junk = T(ph, "junk", [128, D], BF16)
                diags = [T(ph, "diag%d" % i, [128, 4, 128], F32) for i in range(2)]
                ssbs = [T(ph, "ssb%d" % i, [128, 12], F32) for i in range(2)]
                hTs = [T(ph, "hTs%d" % i, [128, 8, 512], BF16) for i in range(2)]
                norm_p1(GROUPS[0], xts[0], junk, diags[0], ssbs[0], x_src(l, GROUPS[0]), x_res(l, GROUPS[0]))
                for gi, tiles in enumerate(GROUPS):
                    n = len(tiles)
                    ntok = n * 128
                    hb = hTs[gi % 2]
                    if gi + 1 < len(GROUPS):
                        nt_ = GROUPS[gi + 1]
                        norm_p1(nt_, xts[(gi + 1) % 2], junk, diags[(gi + 1) % 2], ssbs[(gi + 1) % 2], x_src(l, nt_), x_res(l, nt_))
                    norm_p2(l, 1, tiles, xts[gi % 2], diags[gi % 2],
                            lambda c: (hb[:, c, 0:ntok], [R(hb.name, c)]), None,
                            [0, 1, 2, 3] if gi % 2 == 0 else [4, 5, 6, 7])
                    S.dma('sp', hT_view(tiles[0] * 128, ntok), hb[:, :, 0:ntok], own=R(hb.name, 0),
                          r=[R(hb.name, c) for c in range(8)], w=[R('hT', t) for t in tiles])
                S.phase_end()
            if stop == ('p1', l):
                break

            def load_hT(hb, tiles):
                ntok = len(tiles) * 128
                S.dma('sp', hb[:, :, 0:ntok], hT_view(tiles[0] * 128, ntok), own=R(hb.name),
                      r=[R('hT', t) for t in tiles], w=[R(hb.name)])

            def store_br(i, brs, tiles):
                ntok = len(tiles) * 128
                t0 = tiles[0] * 128
                S.dma('sp', brT_d[i, :, :, t0:t0 + ntok].rearrange("g p t -> p g t"), brs[0:64, :, 0:ntok],
                      own=R(brs.name), r=[R(brs.name)], w=[R('br', i, t) for t in tiles])

            def wload(dst, res, cols0, cols1):
                S.dma('pool', dst[:], w_in[l][:, cols0:cols1].rearrange("(k p) n -> p k n", p=128),
                      own=R(res), w=[R(res)])

            with ExitStack() as ph:
                wu = T(ph, "wu", [128, 8, 256], BF16)
                pw = T(ph, "pw", [64, 4, 64], BF16)
                band = T(ph, "band", [128, 20, 128], BF16)
                u_sb = T(ph, "u_sb", [128, NT, 256], BF16)
                hTs = [T(ph, "hTp%d" % i, [128, 8, 512], BF16) for i in range(2)]
                brs2 = [T(ph, "brs%d" % i, [64, 4, 512], BF16) for i in range(2)]
                pooled = [T(ph, "pooled%d" % i, [64, 512], BF16) for i in range(2)]
                wload(wu, 'wu', 0, 256)
                S.dma('pool', pw[:], pool_w[l].rearrange("g c d -> c g d"), own=R('pw'), w=[R('pw')])
                S.dma('pool', band[:], band_in, own=R('band'), w=[R('band')])
                if DBG_BARRIER:
                    S.barrier(DBG_BARRIER)
                groups = GROUPS if need_ctx else GROUPS[:8]
                load_hT(hTs[0], groups[0])
                for gi, tiles in enumerate(groups):
                    hb = hTs[gi % 2]
                    if gi + 1 < len(groups):
                        load_hT(hTs[(gi + 1) % 2], groups[gi + 1])
                    for ti, tile in enumerate(tiles):
                        pb = (gi * 4 + ti) % 4
                        for k in range(8):
                            S.op('pe', lambda e: e.matmul(ps[pb][:, 0:256], lhsT=hb[:, k, ti * 128:(ti + 1) * 128],
                                                          rhs=wu[:, k, :], start=(k == 0), stop=(k == 7)),
                                 r=[R(hb.name), R('wu')], w=[PR(pb)])
                        if ti % 2 == 0:
                            S.op('dve', lambda e: e.tensor_copy(u_sb[:, tile, :], ps[pb][:, 0:256]), r=[PR(pb)], w=[R('u', tile)])
                        else:
                            S.op('act', lambda e: e.activation(u_sb[:, tile, :], ps[pb][:, 0:256], AF.Copy), r=[PR(pb)], w=[R('u', tile)])
                cnt = 0
                for gi, tiles in enumerate(groups):
                    first, last = (0, 31) if tiles[0] < 32 else (32, 33)
                    ntok = len(tiles) * 128
                    brs = brs2[gi % 2]
                    for g in range(4):
                        pa = 4 + (cnt % 2)
                        pbk = 6 + (cnt % 2)
                        pl = pooled[cnt % 2]
                        cnt += 1
                        for jj, j in enumerate(tiles):
                            ins = []
                            if j > first:
                                ins.append((j - 1, 0))
                            ins.append((j, 3 if j == first else (4 if j == last else 1)))
                            if j < last:
                                ins.append((j + 1, 2))
                            for ii, (jin, var) in enumerate(ins):
                                S.op('pe', lambda e: e.matmul(ps[pa][0:64, jj * 128:(jj + 1) * 128],
                                                              lhsT=u_sb[:, jin, g * 64:(g + 1) * 64], rhs=band[:, g * 5 + var, :],
                                                              start=(ii == 0), stop=(ii == len(ins) - 1)),
                                     r=[R('u', jin), R('band')], w=[PR(pa)])
                        S.op('act', lambda e: e.activation(pl[0:64, 0:ntok], ps[pa][0:64, 0:ntok], AF.Copy), r=[PR(pa)], w=[R(pl.name)])
                        S.op('pe', lambda e: e.matmul(ps[pbk][0:64, 0:ntok], lhsT=pw[0:64, g, :], rhs=pl[0:64, 0:ntok],
                                                      start=True, stop=True),
                             r=[R('pw'), R(pl.name)], w=[PR(pbk)])
                        S.op('dve', lambda e: e.tensor_scalar(brs[0:64, g, 0:ntok], ps[pbk][0:64, 0:ntok],
                                                              cols64[0:64, l * 4 + g:l * 4 + g + 1], None, ALU.mult),
                             r=[PR(pbk), R('cols64')], w=[R(brs.name)])
                    store_br(0, brs, tiles)
                S.phase_end()
            if stop == ('pool', l):
                break

            def transpose_out(i, o_grp, tiles, brs, pbank):
                n = len(tiles)
                for t0 in range(0, n, 2):
                    nn = min(2, n - t0)
                    for tt in range(nn):
                        for h in range(4):
                            col = (tt * 4 + h) * 128
                            S.op('pe', lambda e: e.transpose(psb[pbank][0:64, col:col + 128],
                                                             o_grp[:, t0 + tt, h * 64:(h + 1) * 64], identB[:]),
                                 r=[R(o_grp.name), R('identB')], w=[PR(pbank)])
                    S.op('dve', lambda e: e.tensor_copy(
                        brs[0:64, :, t0 * 128:(t0 + nn) * 128].rearrange("p h (t q) -> p t h q", t=nn),
                        psb[pbank][0:64, 0:nn * 512].rearrange("p (t h q) -> p t h q", t=nn, h=4)),
                        r=[PR(pbank)], w=[R(brs.name)])
                store_br(i, brs, tiles)

            def dense_attention(i, pair_list, scale, ph, bg=None, bg_n=0):
                pTs = [T(ph, "pT%d" % k, [128, 512], BF16) for k in range(6)]
                o_grps = [T(ph, "ogrp%d" % k, [128, 4, 256], BF16) for k in range(2)]
                brs2 = [T(ph, "brsA%d" % k, [64, 4, 512], BF16) for k in range(2)]
                qgroups = GROUPS if need_ctx else GROUPS[:8]
                sbanks = [0, 1, 2, 3]
                obanks = [4, 5]
                sc = 0
                for gi, tiles in enumerate(qgroups):
                    nqs = len(tiles)
                    nq = nqs * 128
                    q0 = tiles[0] * 128
                    chunks = list(range(NT)) if tiles[0] < 32 else [32, 33]
                    o_grp = o_grps[gi % 2]
                    for lanes, fin in pair_list:
                        for ob in obanks:
                            S.op('dve', lambda e: e.memset(ps[ob][:, 0:nqs * 65], 0.0), w=[PR(ob)])
                        if bg is not None:
                            for _ in range(bg_n):
                                f_ = next(bg, None)
                                if f_ is not None:
                                    f_()
                        slots = {}

                        def qk_pair(ci):
                            nonlocal sc
                            c = chunks[ci]
                            for m in range(2):
                                kf, qf, vf = lanes[m]
                                sb = sbanks[sc % 4]
                                pT = pTs[sc % 6]
                                sc += 1
                                slots[(ci, m)] = (sb, pT)
                                S.op('pe', lambda e: e.matmul(ps[sb][:, 0:nq], lhsT=kf(c), rhs=qf(q0, nq), start=True, stop=True),
                                     r=[R('kT', c), R('qT', gi)], w=[PR(sb)])
                            for m in range(2):
                                sb, pT = slots[(ci, m)]
                                S.op('act', lambda e: e.activation(pT[:, 0:nq], ps[sb][:, 0:nq], AF.Exp, scale=scale),
                                     r=[PR(sb)], w=[R(pT.name)])

                        def pv_pair(ci):
                            c = chunks[ci]
                            for m in range(2):
                                kf, qf, vf = lanes[m]
                                sb, pT = slots.pop((ci, m))
                                for qs in range(nqs):
                                    S.op('pe', lambda e: e.matmul(ps[obanks[m]][:, qs * 65:(qs + 1) * 65],
                                                                  lhsT=pT[:, qs * 128:(qs + 1) * 128], rhs=vf(c),
                                                                  start=False, stop=(ci == len(chunks) - 1), skip_group_check=True),
                                         r=[R(pT.name), R('V', c)], w=[PR(obanks[m])])

                        qk_pair(0)
                        for ci in range(len(chunks)):
                            if ci + 1 < len(chunks):
                                qk_pair(ci + 1)
                            pv_pair(ci)
                        fin(nqs, obanks, o_grp)
                    transpose_out(i, o_grp, tiles, brs2[gi % 2], 7)

            with ExitStack() as ph:
                wq = T(ph, "wq", [128, 8, 512], BF16)
                qT_sb = T(ph, "qT_sb", [128, 2, NTOK], BF16)
                kT_sb = T(ph, "kT_sb", [128, 2, NTOK], BF16)
                Vaug = T(ph, "Vaug", [128, NT, 2, 65], BF16)
                cosT = T(ph, "cosT", [128, 32, 2, 16], F32)
                sinT = T(ph, "sinT", [128, 32, 2, 16], F32)
                hTs = [T(ph, "hTg%d" % i, [128, 8, 512], BF16) for i in range(2)]
                sqt = [T(ph, "sqt%d" % i, [128, 6, 64], F32) for i in range(4)]
                qn1 = [T(ph, "qn1%d" % i, [128, 8, 64], F32) for i in range(4)]
                qn2 = [T(ph, "qn2%d" % i, [128, 8, 64], F32) for i in range(4)]
                rm = [T(ph, "rm%d" % i, [128, 4, 8, 32], F32) for i in range(4)]
                qr = [T(ph, "qr%d" % i, [128, 8, 64], BF16) for i in range(4)]
                st6 = [T(ph, "st6%d" % i, [128, 18], F32) for i in range(4)]
                rden = [T(ph, "rden%d" % i, [128, 4], F32) for i in range(2)]
                wload(wq, 'wq', 1792, 2304)
                S.dma('sp', cosT[:], cosg_in, own=R('cosT'), w=[R('cosT')])
                S.dma('sp', sinT[:], sing_in, own=R('sinT'), w=[R('sinT')])
                S.op('pool', lambda e: e.memset(Vaug[:], 1.0), w=[R('V', c) for c in range(NT)])
                load_hT(hTs[0], GROUPS[0])
                for gi, tiles in enumerate(GROUPS):
                    hb = hTs[gi % 2]
                    if gi + 1 < len(GROUPS):
                        load_hT(hTs[(gi + 1) % 2], GROUPS[gi + 1])
                    for ti, tile in enumerate(tiles):
                        for k in range(8):
                            S.op('pe', lambda e: e.matmul(ps[ti][:, :], lhsT=hb[:, k, ti * 128:(ti + 1) * 128], rhs=wq[:, k, :],
                                                          start=(k == 0), stop=(k == 7)),
                                 r=[R(hb.name), R('wq')], w=[PR(ti)])
                    for ti, tile in enumerate(tiles):
                        S.op('act', lambda e: e.activation(Vaug[:, tile, :, 0:64], ps[ti][:, 384:512].rearrange("p (h d) -> p h d", h=2), AF.Copy),
                             r=[PR(ti)], w=[R('V', tile)])
                        S.op('act', lambda e: e.activation(sqt[ti][:], ps[ti][:, 0:384].rearrange("p (h d) -> p h d", h=6), AF.Square),
                             r=[PR(ti)], w=[R(sqt[ti].name)])
                    for ti, tile in enumerate(tiles):
                        S.op('dve', lambda e: e.reduce_sum(st6[ti][:, 0:6], sqt[ti][:], axis=AX.X), r=[R(sqt[ti].name)], w=[R(st6[ti].name, 0)])
                        S.op('dve', lambda e: e.tensor_scalar(st6[ti][:, 6:12], st6[ti][:, 0:6], 1.0 / 64, EPS, ALU.mult, ALU.add),
                             r=[R(st6[ti].name, 0)], w=[R(st6[ti].name, 1)])
                    for ti, tile in enumerate(tiles):
                        S.op('pool', lambda e: e.tensor_tensor(st6[ti][:, 12:18], st6[ti][:, 6:12], mhalf[:, 0:6], ALU.pow),
                             r=[R(st6[ti].name, 1), R('mhalf')], w=[R(st6[ti].name, 2)])
                    for ti, tile in enumerate(tiles):
                        dst = qn2[ti] if tile < 32 else qr[ti]
                        dres = [R(qn2[ti].name)] if tile < 32 else [R(qr[ti].name, 0), R(qr[ti].name, 1)]
                        for hh in range(8):
                            src_h = hh if hh < 4 else 4 + (hh - 4) // 2
                            S.op('dve', lambda e: e.scalar_tensor_tensor(dst[:, hh, :], ps[ti][:, src_h * 64:(src_h + 1) * 64],
                                                                         st6[ti][:, 12 + src_h:13 + src_h], gainG[:, l, hh, :],
                                                                         ALU.mult, ALU.mult),
                                 r=[PR(ti), R(st6[ti].name, 2), R('gainG')], w=dres)
                    for half in range(2):
                        for ti, tile in enumerate(tiles):
                            if tile >= 32:
                                continue
                            X = qn2[ti]
                            x1 = mk(X, 0, 128, 0, [[64, 8], [32, 2], [1, 16]])
                            x2 = mk(X, 0, 128, 16, [[64, 8], [32, 2], [1, 16]])
                            cs = mk(cosT, 0, 128, tile * 32, [[0, 8], [16, 2], [1, 16]])
                            sn = mk(sinT, 0, 128, tile * 32, [[0, 8], [16, 2], [1, 16]])
                            m_ = [mk(rm[ti], 0, 128, q * 256, [[32, 8], [16, 2], [1, 16]]) for q in range(4)]
                            o1 = mk(qr[ti], 0, 128, 0, [[64, 8], [32, 2], [1, 16]])
                            o2 = mk(qr[ti], 0, 128, 16, [[64, 8], [32, 2], [1, 16]])
                            if half == 0:
                                S.op('dve', lambda e: e.tensor_tensor(m_[0], x1, cs, ALU.mult), r=[R(X.name), R('cosT')], w=[R(rm[ti].name, 0)])
                                S.op('dve', lambda e: e.tensor_tensor(m_[1], x2, sn, ALU.mult), r=[R(X.name), R('sinT')], w=[R(rm[ti].name, 1)])
                                S.op('pool', lambda e: e.tensor_tensor(m_[2], x1, sn, ALU.mult), r=[R(X.name), R('sinT')], w=[R(rm[ti].name, 2)])
                                S.op('pool', lambda e: e.tensor_tensor(m_[3], x2, cs, ALU.mult), r=[R(X.name), R('cosT')], w=[R(rm[ti].name, 3)])
                            else:
                                S.op('dve', lambda e: e.tensor_tensor(o1, m_[0], m_[1], ALU.subtract),
                                     r=[R(rm[ti].name, 0), R(rm[ti].name, 1)], w=[R(qr[ti].name, 0)])
                                S.op('pool', lambda e: e.tensor_tensor(o2, m_[2], m_[3], ALU.add),
                                     r=[R(rm[ti].name, 2), R(rm[ti].name, 3)], w=[R(qr[ti].name, 1)])
                    for ti, tile in enumerate(tiles):
                        tb = 4 + ti
                        for jj in range(4):
                            S.op('pe', lambda e: e.transpose(psb[tb][:, jj * 128:(jj + 1) * 128],
                                                             qr[ti][:, 2 * jj:2 * jj + 2, :].rearrange("p h d -> p (h d)"), identB[:]),
                                 r=[R(qr[ti].name, 0), R(qr[ti].name, 1), R('identB')], w=[PR(tb)])
                    for ti, tile in enumerate(tiles):
                        tb = 4 + ti
                        S.op('act', lambda e: e.activation(qT_sb[:, :, tile * 128:(tile + 1) * 128],
                                                           psb[tb][:, 0:256].rearrange("p (h q) -> p h q", h=2), AF.Copy),
                             r=[PR(tb)], w=[R('qT', tile // 4)])
                        S.op('dve', lambda e: e.tensor_copy(kT_sb[:, :, tile * 128:(tile + 1) * 128],
                                                            psb[tb][:, 256:512].rearrange("p (h q) -> p h q", h=2)),
                             r=[PR(tb)], w=[R('kT', tile)])

                def fin_gqa(h, nqs, ob, o_grp):
                    rd = rden[h % 2]
                    S.op('dve', lambda e: e.reciprocal(rd[:, 0:nqs], mk(ps[ob], 0, 128, 64, [[65, nqs]])), r=[PR(ob)], w=[R(rd.name)])
                    S.op('dve', lambda e: e.tensor_tensor(o_grp[:, 0:nqs, h * 64:(h + 1) * 64],
                                                          mk(ps[ob], 0, 128, 0, [[65, nqs], [1, 64]]),
                                                          mk(rd, 0, 128, 0, [[1, nqs], [0, 64]]), ALU.mult),
                         r=[PR(ob), R(rd.name)], w=[R(o_grp.name)])

                rp = T(ph, "rp", [60, 128], F32)
                Tall = T(ph, "Tall", [128, 60, 64], F32)
                maskc = T(ph, "maskc", [128, 21, 128], F32)
                S.op('pool', lambda e: e.memset(rp[:], 0.0), w=[R('rp')])
                S.dma('sp', rp[:, 48:79], nat_rpb[l], own=R('rp'), w=[R('rp')])
                S.dma('sp', rpbpad[l], rp[:], own=R('rp'), r=[R('rp')], w=[R('rpbpad')])
                S.dma('sp', maskc[:], natmask_in, own=R('maskc'), w=[R('maskc')])
                for b in range(2):
                    S.dma('sp', Tall[b * 64:(b + 1) * 64, :, :],
                          bass.AP(rpbpad.tensor, rpbpad[l].offset, [[1, 64], [128, 60], [1, 64]]),
                          own=R('Tall', b), r=[R('rpbpad')], w=[R('Tall', b)])
                wload(wqd_next, 'wqd', 256, 1024)
                if SPARSE:
                    S.barrier(['pool'])
                    issue_casts(l)
                bg_ops = []
                for h in range(4):
                    bg_ops.append(lambda h=h: S.op('dve', lambda e: e.tensor_copy(biasM[:, :, h, :], maskc[:]), r=[R('maskc')], w=[R('biasM')]))
                for v in range(21):
                    for (a, b), dr in nat_plan[v].items():
                        if dr is None:
                            continue
                        for h in range(4):
                            bg_ops.append(lambda v=v, a=a, b=b, dr=dr, h=h: S.op(
                                'dve', lambda e: e.scalar_tensor_tensor(biasM[b * 64:(b + 1) * 64, v, h, a * 64:(a + 1) * 64],
                                                                        Tall[b * 64:(b + 1) * 64, h * 15 + dr, :], 8.0,
                                                                        maskc[b * 64:(b + 1) * 64, v, a * 64:(a + 1) * 64], ALU.mult, ALU.add),
                                r=[R('Tall', b), R('maskc'), R('biasM')], w=[R('biasM')]))
                bg_iter = iter(bg_ops)

                def gqa_pair(j):
                    def lane(r):
                        return (lambda c: kT_sb[r * 64:(r + 1) * 64, j, c * 128:(c + 1) * 128],
                                lambda q0, nq: qT_sb[r * 64:(r + 1) * 64, j, q0:q0 + nq],
                                lambda c: Vaug[:, c, j, :])

                    def fin(nqs, obanks, o_grp):
                        for r in range(2):
                            fin_gqa(2 * j + r, nqs, obanks[r], o_grp)
                    return ([lane(0), lane(1)], fin)

                dense_attention(3, [gqa_pair(0), gqa_pair(1)], 0.125, ph, bg=bg_iter, bg_n=24)
                for f_ in bg_iter:
                    f_()
                S.phase_end()
            if stop == ('gqa', l):
                break

            with ExitStack() as ph:
                wq = wqd_next
                qT_sb = T(ph, "qT_sbd", [64, 4, NTOK], BF16)
                kT_sb = T(ph, "kT_sbd", [64, 4, NTOK], BF16)
                Vaug = T(ph, "Vaugd", [128, NT, 4, 65], BF16)
                cosT = T(ph, "cosTd", [128, 32, 2, 8], F32)
                sinT = T(ph, "sinTd", [128, 32, 2, 8], F32)
                hTs = [T(ph, "hTd%d" % i, [128, 8, 512], BF16) for i in range(2)]
                qf = [T(ph, "qf%d" % i, [128, 16, 32], F32) for i in range(2)]
                rm = [T(ph, "rmd%d" % i, [128, 4, 16, 16], F32) for i in range(2)]
                qr = [T(ph, "qrd%d" % i, [128, 16, 32], BF16) for i in range(2)]
                rden = [T(ph, "rdend%d" % i, [128, 16], F32) for i in range(2)]
                t0s = [T(ph, "t0s%d" % i, [128, 4, 64], F32) for i in range(2)]
                t1s = [T(ph, "t1s%d" % i, [128, 4, 64], F32) for i in range(2)]
                tsq = [T(ph, "tsq%d" % i, [128, 4, 64], F32) for i in range(2)]
                S.dma('sp', cosT[:], cosd_in, own=R('cosT'), w=[R('cosT')])
                S.dma('sp', sinT[:], sind_in, own=R('sinT'), w=[R('sinT')])
                S.op('pool', lambda e: e.memset(Vaug[:], 1.0), w=[R('V', c) for c in range(NT)])
                load_hT(hTs[0], GROUPS[0])
                bcnt = 0
                for gi, tiles in enumerate(GROUPS):
                    hb = hTs[gi % 2]
                    if gi + 1 < len(GROUPS):
                        load_hT(hTs[(gi + 1) % 2], GROUPS[gi + 1])
                    for t0_ in range(0, len(tiles), 2):
                        batch = [(t0_ + u, tiles[t0_ + u]) for u in range(min(2, len(tiles) - t0_))]
                        tbs = [4, 5] if bcnt % 2 == 0 else [6, 7]
                        bcnt += 1
                        for u, (ti, tile) in enumerate(batch):
                            pb = 2 * u
                            for k in range(8):
                                S.op('pe', lambda e: e.matmul(ps[pb][:, :], lhsT=hb[:, k, ti * 128:(ti + 1) * 128], rhs=wq[:, k, 0:512],
                                                              start=(k == 0), stop=(k == 7)),
                                     r=[R(hb.name), R('wqd')], w=[PR(pb)])
                            for k in range(8):
                                S.op('pe', lambda e: e.matmul(ps[pb + 1][:, 0:256], lhsT=hb[:, k, ti * 128:(ti + 1) * 128], rhs=wq[:, k, 512:768],
                                                              start=(k == 0), stop=(k == 7)),
                                     r=[R(hb.name), R('wqd')], w=[PR(pb + 1)])
                        for u, (ti, tile) in enumerate(batch):
                            pb = 2 * u
                            S.op('act', lambda e: e.activation(Vaug[:, tile, :, 0:64], ps[pb + 1][:, 0:256].rearrange("p (h d) -> p h d", h=4), AF.Copy),
                                 r=[PR(pb + 1)], w=[R('V', tile)])
                            if tile < 32:
                                S.op('act', lambda e: e.activation(qf[u][:].rearrange("p a b -> p (a b)"), ps[pb][:, :], AF.Copy),
                                     r=[PR(pb)], w=[R(qf[u].name)])
                            else:
                                S.op('act', lambda e: e.activation(qr[u][:].rearrange("p a b -> p (a b)"), ps[pb][:, :], AF.Copy),
                                     r=[PR(pb)], w=[R(qr[u].name, 0), R(qr[u].name, 1)])
                        for half in range(2):
                            for u, (ti, tile) in enumerate(batch):
                                if tile >= 32:
                                    continue
                                X = qf[u]
                                x1 = mk(X, 0, 128, 0, [[32, 16], [16, 2], [1, 8]])
                                x2 = mk(X, 0, 128, 8, [[32, 16], [16, 2], [1, 8]])
                                cs = mk(cosT, 0, 128, tile * 16, [[0, 16], [8, 2], [1, 8]])
                                sn = mk(sinT, 0, 128, tile * 16, [[0, 16], [8, 2], [1, 8]])
                                m_ = [mk(rm[u], 0, 128, q * 256, [[16, 16], [8, 2], [1, 8]]) for q in range(4)]
                                o1 = mk(qr[u], 0, 128, 0, [[32, 16], [16, 2], [1, 8]])
                                o2 = mk(qr[u], 0, 128, 8, [[32, 16], [16, 2], [1, 8]])
                                if half == 0:
                                    S.op('dve', lambda e: e.tensor_tensor(m_[0], x1, cs, ALU.mult), r=[R(X.name), R('cosT')], w=[R(rm[u].name, 0)])
                                    S.op('dve', lambda e: e.tensor_tensor(m_[1], x2, sn, ALU.mult), r=[R(X.name), R('sinT')], w=[R(rm[u].name, 1)])
                                    S.op('pool', lambda e: e.tensor_tensor(m_[2], x1, sn, ALU.mult), r=[R(X.name), R('sinT')], w=[R(rm[u].name, 2)])
                                    S.op('pool', lambda e: e.tensor_tensor(m_[3], x2, cs, ALU.mult), r=[R(X.name), R('cosT')], w=[R(rm[u].name, 3)])
                                else:
                                    S.op('dve', lambda e: e.tensor_tensor(o1, m_[0], m_[1], ALU.subtract),
                                         r=[R(rm[u].name, 0), R(rm[u].name, 1)], w=[R(qr[u].name, 0)])
                                    S.op('pool', lambda e: e.tensor_tensor(o2, m_[2], m_[3], ALU.add),
                                         r=[R(rm[u].name, 2), R(rm[u].name, 3)], w=[R(qr[u].name, 1)])
                        for u, (ti, tile) in enumerate(batch):
                            tb = tbs[u]
                            qrf = qr[u][:].rearrange("p a b -> p (a b)")
                            for hh in range(8):
                                S.op('pe', lambda e: e.transpose(psb[tb][0:64, hh * 128:(hh + 1) * 128], qrf[:, hh * 64:(hh + 1) * 64], identB[:]),
                                     r=[R(qr[u].name, 0), R(qr[u].name, 1), R('identB')], w=[PR(tb)])
                        for u, (ti, tile) in enumerate(batch):
                            tb = tbs[u]
                            S.op('act', lambda e: e.activation(qT_sb[0:64, :, tile * 128:(tile + 1) * 128],
                                                               psb[tb][0:64, 0:512].rearrange("p (h q) -> p h q", h=4), AF.Copy),
                                 r=[PR(tb)], w=[R('qT', tile // 4)])
                            S.op('dve', lambda e: e.tensor_copy(kT_sb[0:64, :, tile * 128:(tile + 1) * 128],
                                                                psb[tb][0:64, 512:1024].rearrange("p (h q) -> p h q", h=4)),
                                 r=[PR(tb)], w=[R('kT', tile)])

                def fin_diff(h, nqs, obanks, o_grp):
                    o0, o1 = obanks
                    rd = rden[h % 2]
                    t0, t1, tq = t0s[h % 2], t1s[h % 2], tsq[h % 2]
                    S.op('dve', lambda e: e.reciprocal(rd[:, 0:nqs], mk(ps[o0], 0, 128, 64, [[65, nqs]])), r=[PR(o0)], w=[R(rd.name, 0)])
                    S.op('dve', lambda e: e.reciprocal(rd[:, 4:4 + nqs], mk(ps[o1], 0, 128, 64, [[65, nqs]])), r=[PR(o1)], w=[R(rd.name, 1)])
                    S.op('dve', lambda e: e.tensor_scalar(rd[:, 4:4 + nqs], rd[:, 4:4 + nqs], nlam[:, l:l + 1], None, ALU.mult),
                         r=[R(rd.name, 1), R('nlam')], w=[R(rd.name, 1)])
                    S.op('dve', lambda e: e.tensor_tensor(t0[:, 0:nqs, :], mk(ps[o0], 0, 128, 0, [[65, nqs], [1, 64]]),
                                                          mk(rd, 0, 128, 0, [[1, nqs], [0, 64]]), ALU.mult),
                         r=[PR(o0), R(rd.name, 0)], w=[R(t0.name)])
                    S.op('dve', lambda e: e.tensor_tensor(t1[:, 0:nqs, :], mk(ps[o1], 0, 128, 0, [[65, nqs], [1, 64]]),
                                                          mk(rd, 0, 128, 4, [[1, nqs], [0, 64]]), ALU.mult),
                         r=[PR(o1), R(rd.name, 1)], w=[R(t1.name)])
                    S.op('dve', lambda e: e.tensor_tensor(t0[:, 0:nqs, :], t0[:, 0:nqs, :], t1[:, 0:nqs, :], ALU.add),
                         r=[R(t0.name), R(t1.name)], w=[R(t0.name)])
                    S.op('dve', lambda e: e.tensor_tensor(tq[:, 0:nqs, :], t0[:, 0:nqs, :], t0[:, 0:nqs, :], ALU.mult),
                         r=[R(t0.name)], w=[R(tq.name)])
                    S.op('dve', lambda e: e.reduce_sum(rd[:, 8:8 + nqs], tq[:, 0:nqs, :], axis=AX.X), r=[R(tq.name)], w=[R(rd.name, 2)])
                    S.op('dve', lambda e: e.tensor_scalar(rd[:, 8:8 + nqs], rd[:, 8:8 + nqs], 1.0 / 64, EPS, ALU.mult, ALU.add),
                         r=[R(rd.name, 2)], w=[R(rd.name, 2)])
                    S.op('pool', lambda e: e.tensor_tensor(rd[:, 12:12 + nqs], rd[:, 8:8 + nqs], mhalf[:, 0:nqs], ALU.pow),
                         r=[R(rd.name, 2), R('mhalf')], w=[R(rd.name, 3)])
                    S.op('dve', lambda e: e.tensor_tensor(t1[:, 0:nqs, :], t0[:, 0:nqs, :], mk(rd, 0, 128, 12, [[1, nqs], [0, 64]]), ALU.mult),
                         r=[R(t0.name), R(rd.name, 3)], w=[R(t1.name)])
                    S.op('dve', lambda e: e.tensor_tensor(o_grp[:, 0:nqs, h * 64:(h + 1) * 64], t1[:, 0:nqs, :],
                                                           mk(gB, 0, 128, l * 64, [[0, nqs], [1, 64]]), ALU.mult),
                         r=[R(t1.name), R('gB')], w=[R(o_grp.name)])

                def diff_pair(h):
                    def lane(m):
                        return (lambda c: kT_sb[m * 32:(m + 1) * 32, h, c * 128:(c + 1) * 128],
                                lambda q0, nq: qT_sb[m * 32:(m + 1) * 32, h, q0:q0 + nq],
                                lambda c: Vaug[:, c, h, :])
                    return ([lane(0), lane(1)], lambda nqs, obanks, o_grp: fin_diff(h, nqs, obanks, o_grp))

                dense_attention(1, [diff_pair(h) for h in range(4)], 32 ** -0.5, ph)
                S.phase_end()
            if stop == ('diff', l):
                break

            with ExitStack() as ph:
                wq = T(ph, "wqn", [128, 8, 768], BF16)
                qT_sb = T(ph, "qT_sbn", [64, 4, NTOK], BF16)
                kT_sb = T(ph, "kT_sbn", [64, 4, NTOK], BF16)
                Vaug = T(ph, "Vaugn", [128, NT, 4, 65], BF16)
                hTs = [T(ph, "hTn%d" % i, [128, 8, 512], BF16) for i in range(2)]
                pTa = [T(ph, "pTa%d" % i, [128, 512], BF16) for i in range(2)]
                pTb = [T(ph, "pTb%d" % i, [128, 512], BF16) for i in range(2)]
                o_grps = [T(ph, "ogn%d" % k, [128, 4, 256], BF16) for k in range(2)]
                brs2 = [T(ph, "brsn%d" % k, [64, 4, 512], BF16) for k in range(2)]
                rden = [T(ph, "rdenn%d" % i, [128, 4], F32) for i in range(2)]
                wload(wq, 'wq', 1024, 1792)
                S.op('pool', lambda e: e.memset(Vaug[:], 1.0), w=[R('V', c) for c in range(NT)])
                load_hT(hTs[0], GROUPS[0])
                pc = 0
                for gi, tiles in enumerate(GROUPS):
                    hb = hTs[gi % 2]
                    ntok = len(tiles) * 128
                    t0 = tiles[0] * 128
                    if gi + 1 < len(GROUPS):
                        load_hT(hTs[(gi + 1) % 2], GROUPS[gi + 1])
                    for qk in range(2):
                        for h in range(4):
                            pb = pc % 4
                            pc += 1
                            c0 = qk * 256 + h * 64
                            for k in range(8):
                                S.op('pe', lambda e: e.matmul(ps[pb][0:64, 0:ntok], lhsT=wq[:, k, c0:c0 + 64], rhs=hb[:, k, 0:ntok],
                                                              start=(k == 0), stop=(k == 7)),
                                     r=[R(hb.name), R('wq')], w=[PR(pb)])
                            dst = (qT_sb if qk == 0 else kT_sb)[0:64, h, t0:t0 + ntok]
                            wres = [R('qT', gi)] if qk == 0 else [R('kT', t) for t in tiles]
                            if pc % 2:
                                S.op('dve', lambda e: e.tensor_copy(dst, ps[pb][0:64, 0:ntok]), r=[PR(pb)], w=wres)
                            else:
                                S.op('act', lambda e: e.activation(dst, ps[pb][0:64, 0:ntok], AF.Copy), r=[PR(pb)], w=wres)
                    for ti, tile in enumerate(tiles):
                        pb = pc % 4
                        pc += 1
                        for k in range(8):
                            S.op('pe', lambda e: e.matmul(ps[pb][:, 0:256], lhsT=hb[:, k, ti * 128:(ti + 1) * 128], rhs=wq[:, k, 512:768],
                                                          start=(k == 0), stop=(k == 7)),
                                 r=[R(hb.name), R('wq')], w=[PR(pb)])
                        S.op('act', lambda e: e.activation(Vaug[:, tile, :, 0:64], ps[pb][:, 0:256].rearrange("p (h d) -> p h d", h=4), AF.Copy),
                             r=[PR(pb)], w=[R('V', tile)])
                qgroups = GROUPS if need_ctx else GROUPS[:8]
                sc = 0
                for gi, tiles in enumerate(qgroups):
                    o_grp = o_grps[gi % 2]
                    for ti, j in enumerate(tiles):
                        if j < 32:
                            kts = [(t, _nat_variant(j, t)) for t in _nat_keytiles(j)] + [(32, None), (33, None)]
                        else:
                            kts = [(32, None), (33, None)]
                        ob = 4 + (ti % 2)
                        hs = {}

                        def nat_a(h):
                            nonlocal sc
                            sa = (sc % 2) * 2
                            pa, pbb = pTa[sc % 2], pTb[sc % 2]
                            sc += 1
                            hs[h] = (pa, pbb)
                            for ii, (t, v) in enumerate(kts):
                                bank = sa + ii // 4
                                col = (ii % 4) * 128
                                S.op('pe', lambda e: e.matmul(ps[bank][:, col:col + 128], lhsT=kT_sb[0:64, h, t * 128:(t + 1) * 128],
                                                              rhs=qT_sb[0:64, h, j * 128:(j + 1) * 128], start=True, stop=(v is None)),
                                     r=[R('kT', t), R('qT', gi)], w=[PR(bank)])
                                if v is not None:
                                    S.op('pe', lambda e: e.matmul(ps[bank][:, col:col + 128], lhsT=biasM[:, v, h, :], rhs=antiB[:],
                                                                  start=False, stop=True),
                                         r=[R('biasM'), R('antiB')], w=[PR(bank)])
                            na = min(4, len(kts))
                            nb = len(kts) - na
                            S.op('act', lambda e: e.activation(pa[:, 0:na * 128], ps[sa][:, 0:na * 128], AF.Exp, scale=0.125),
                                 r=[PR(sa)], w=[R(pa.name)])
                            if nb:
                                S.op('act', lambda e: e.activation(pbb[:, 0:nb * 128], ps[sa + 1][:, 0:nb * 128], AF.Exp, scale=0.125),
                                     r=[PR(sa + 1)], w=[R(pbb.name)])

                        def nat_b(h):
                            pa, pbb = hs.pop(h)
                            for ii, (t, v) in enumerate(kts):
                                src = pa if ii < 4 else pbb
                                col = (ii % 4) * 128
                                S.op('pe', lambda e: e.matmul(ps[ob][:, h * 65:(h + 1) * 65], lhsT=src[:, col:col + 128], rhs=Vaug[:, t, h, :],
                                                              start=(ii == 0), stop=(ii == len(kts) - 1)),
                                     r=[R(src.name), R('V', t)], w=[PR(ob)])

                        nat_a(0)
                        for h in range(4):
                            if h + 1 < 4:
                                nat_a(h + 1)
                            nat_b(h)
                        rd = rden[ti % 2]
                        S.op('dve', lambda e: e.reciprocal(rd[:, 0:4], mk(ps[ob], 0, 128, 64, [[65, 4]])), r=[PR(ob)], w=[R(rd.name)])
                        S.op('dve', lambda e: e.tensor_tensor(o_grp[:, ti, :].rearrange("p (h d) -> p h d", h=4),
                                                              mk(ps[ob], 0, 128, 0, [[65, 4], [1, 64]]),
                                                              mk(rd, 0, 128, 0, [[1, 4], [0, 64]]), ALU.mult),
                             r=[PR(ob), R(rd.name)], w=[R(o_grp.name)])
                    transpose_out(2, o_grp, tiles, brs2[gi % 2], 7)
                S.phase_end()
            lay.close()
            if stop == ('nat', l):
                break

            up_groups = GROUPS if need_ctx else GROUPS[:8]
            with ExitStack() as ph:
                wgate = T(ph, "wgate", [128, 8, 4096], BF16)
                wbr = T(ph, "wbr", [128, 4, 2, D], BF16)
                hTs = [T(ph, "hTm%d" % i, [128, 8, 512], BF16) for i in range(2)]
                brTs = [T(ph, "brTm%d" % i, [128, 4, 2, 512], BF16) for i in range(2)]
                mTs = [T(ph, "mTs%d" % i, [128, 8, 512], BF16) for i in range(2)]
                sgs = [T(ph, "sg%d" % i, [128, 512], F32) for i in range(3)]
                accs = [T(ph, "acc%d" % i, [128, 512], F32) for i in range(2)]
                tmps = [T(ph, "tmpm%d" % i, [128, 512], F32) for i in range(2)]
                for q4 in range(4):
                    S.dma('pool', wgate[:, :, q4 * 1024:(q4 + 1) * 1024],
                          w_in[l][:, 2304 + q4 * 1024:2304 + (q4 + 1) * 1024].rearrange("(k p) n -> p k n", p=128),
                          own=R('wgate', q4), w=[R('wgate', q4)])
                S.dma('pool', wbr[:], w_branch[l].rearrange("b (m p) n -> p b m n", p=128), own=R('wbr'), w=[R('wbr')])

                def load_br(bt, tiles):
                    ntok = len(tiles) * 128
                    t0 = tiles[0] * 128
                    for hh in range(2):
                        S.dma('sp', bt[hh * 64:(hh + 1) * 64, :, :, 0:ntok],
                              brT_d[:, :, :, t0:t0 + ntok].rearrange("b (m hh) d t -> hh d b m t", hh=2)[hh],
                              own=R(bt.name, hh), r=[R('br', i, t) for i in range(4) for t in tiles], w=[R(bt.name, hh)])

                load_hT(hTs[0], up_groups[0])
                load_br(brTs[0], up_groups[0])
                cnt = 0
                for gi, tiles in enumerate(up_groups):
                    ntok = len(tiles) * 128
                    hb, bt, mt = hTs[gi % 2], brTs[gi % 2], mTs[gi % 2]
                    if gi + 1 < len(up_groups):
                        load_hT(hTs[(gi + 1) % 2], up_groups[gi + 1])
                        load_br(brTs[(gi + 1) % 2], up_groups[gi + 1])
                    for dc in range(8):
                        acc = accs[dc % 2]
                        for i in range(4):
                            pg = (cnt % 3)
                            pp = 3 + (cnt % 3)
                            sg = sgs[cnt % 3]
                            cnt += 1
                            c0 = i * 1024 + dc * 128
                            for k in range(8):
                                S.op('pe', lambda e: e.matmul(ps[pg][:, 0:ntok], lhsT=wgate[:, k, c0:c0 + 128], rhs=hb[:, k, 0:ntok],
                                                              start=(k == 0), stop=(k == 7)),
                                     r=[R(hb.name), R('wgate', i)], w=[PR(pg)])
                            for m in range(2):
                                S.op('pe', lambda e: e.matmul(ps[pp][:, 0:ntok], lhsT=wbr[:, i, m, dc * 128:(dc + 1) * 128], rhs=bt[:, i, m, 0:ntok],
                                                              start=(m == 0), stop=(m == 1)),
                                     r=[R(bt.name, 0), R(bt.name, 1), R('wbr')], w=[PR(pp)])
                            S.op('act', lambda e: e.activation(sg[:, 0:ntok], ps[pg][:, 0:ntok], AF.Sigmoid), r=[PR(pg)], w=[R(sg.name)])
                            if i == 0:
                                S.op('dve', lambda e: e.tensor_tensor(acc[:, 0:ntok], sg[:, 0:ntok], ps[pp][:, 0:ntok], ALU.mult),
                                     r=[R(sg.name), PR(pp)], w=[R(acc.name)])
                            else:
                                tm = tmps[i % 2]
                                S.op('dve', lambda e: e.tensor_tensor(tm[:, 0:ntok], sg[:, 0:ntok], ps[pp][:, 0:ntok], ALU.mult),
                                     r=[R(sg.name), PR(pp)], w=[R(tm.name)])
                                if i < 3:
                                    S.op('pool', lambda e: e.tensor_tensor(acc[:, 0:ntok], acc[:, 0:ntok], tm[:, 0:ntok], ALU.add),
                                         r=[R(acc.name), R(tm.name)], w=[R(acc.name)])
                                else:
                                    S.op('pool', lambda e: e.tensor_tensor(mt[:, dc, 0:ntok], acc[:, 0:ntok], tm[:, 0:ntok], ALU.add),
                                         r=[R(acc.name), R(tm.name)], w=[R(mt.name, dc)])
                    t0 = tiles[0] * 128
                    S.dma('sp', mT_d[:, :, t0:t0 + ntok].rearrange("c p t -> p c t"), mt[:, :, 0:ntok], own=R(mt.name, 0),
                          r=[R(mt.name, c) for c in range(8)], w=[R('mT', t) for t in tiles])
                S.phase_end()
            if stop == ('merge', l):
                break

            with ExitStack() as ph:
                wout = T(ph, "wout", [128, 8, D], BF16)
                mTs = [T(ph, "mTo%d" % i, [128, 8, 512], BF16) for i in range(2)]
                xts = [T(ph, "xto%d" % i, [128, 4, D], F32) for i in range(2)]
                gtb = T(ph, "gtb", [128, 2, D], F32)
                tmps = [T(ph, "tmpo%d" % i, [128, 512], F32) for i in range(3)]
                S.dma('pool', wout[:], w_out[l].rearrange("(k p) n -> p k n", p=128), own=R('wout'), w=[R('wout')])
                for kd in range(2):
                    S.dma('sp', gtb[:, kd, :], bass.AP(modd.tensor, modd[l, kd, 2 * D:3 * D].offset, [[0, 128], [1, D]]),
                          own=R('gtb'), r=[R('modd', l)], w=[R('gtb')])

                def load_m(mt, xt, tiles):
                    ntok = len(tiles) * 128
                    t0 = tiles[0] * 128
                    S.dma('sp', mt[:, :, 0:ntok], mT_d[:, :, t0:t0 + ntok].rearrange("c p t -> p c t"), own=R(mt.name),
                          r=[R('mT', t) for t in tiles], w=[R(mt.name)])
                    S.dma('sp', xt[:, 0:len(tiles), :], x_src(l, tiles), own=R(xt.name), r=x_res(l, tiles), w=[R(xt.name)])

                load_m(mTs[0], xts[0], up_groups[0])
                cnt = 0
                for gi, tiles in enumerate(up_groups):
                    n = len(tiles)
                    mt, xt = mTs[gi % 2], xts[gi % 2]
                    kd = kind_of(tiles[0])
                    if gi + 1 < len(up_groups):
                        load_m(mTs[(gi + 1) % 2], xts[(gi + 1) % 2], up_groups[gi + 1])
                    for ts in range(n):
                        for hf in range(2):
                            pb = cnt % 4
                            tm = tmps[cnt % 3]
                            cnt += 1
                            for k in range(8):
                                S.op('pe', lambda e: e.matmul(ps[pb][:, :], lhsT=mt[:, k, ts * 128:(ts + 1) * 128], rhs=wout[:, k, hf * 512:(hf + 1) * 512],
                                                              start=(k == 0), stop=(k == 7)),
                                     r=[R(mt.name), R('wout')], w=[PR(pb)])
                            S.op('dve', lambda e: e.tensor_tensor(tm[:], ps[pb][:, :], gtb[:, kd, hf * 512:(hf + 1) * 512], ALU.mult),
                                 r=[PR(pb), R('gtb')], w=[R(tm.name)])
                            S.op('pool', lambda e: e.tensor_tensor(xt[:, ts, hf * 512:(hf + 1) * 512], xt[:, ts, hf * 512:(hf + 1) * 512], tm[:], ALU.add),
                                 r=[R(tm.name), R(xt.name)], w=[R(xt.name)])
                    S.dma('sp', xs_d[tiles[0] * 128:(tiles[0] + n) * 128, :].rearrange("(t p) d -> p t d", p=128), xt[:, 0:n, :],
                          own=R(xt.name), r=[R(xt.name)], w=[R('xs', t) for t in tiles])
                S.phase_end()
            if stop == ('oproj', l):
                break

            if SPARSE:
                moe_sparse(l, need_ctx)
                if stop == ('moe', l):
                    break
                continue
            last = (l == DEPTH - 1)
            moe_tiles = list(range(34)) if need_ctx else list(range(32))
            nsg = 3
            per = (len(moe_tiles) + nsg - 1) // nsg
            SGS = [moe_tiles[i * per:(i + 1) * per] for i in range(nsg)]
            with ExitStack() as ph:
                tT_sb = T(ph, "tT_sb", [128, 8, per * 128], BF16)
                yacc = T(ph, "yacc", [128, per, D], F32)
                wgu = [T(ph, "wgu%d" % i, [128, 8, 1024], BF16) for i in range(2)]
                wdn = [T(ph, "wdn%d" % i, [128, 4, 1024], BF16) for i in range(2)]
                aT = [T(ph, "aT%d" % i, [128, 4, 512], BF16) for i in range(2)]
                sgt = [T(ph, "sgm%d" % i, [128, 512], BF16) for i in range(3)]
                comb = T(ph, "comb", [128, per, 32], F32)
                xt = T(ph, "xtm", [128, 4, D], F32)
                junk = T(ph, "junkm", [128, D], BF16)
                diag = T(ph, "diagm", [128, 4, 128], F32)
                ssb = T(ph, "ssbm", [128, 12], F32)
                tT32 = T(ph, "tT32", [128, 8, 512], F32)
                wr32 = T(ph, "wr32", [128, 8, 36], F32)
                brt = T(ph, "brt", [128, 36], F32)
                gt2 = T(ph, "gt2", [128, 2, D], F32)
                gfin = T(ph, "gfin", [128, D], F32)
                lg = [T(ph, "lg%d" % i, [128, 36], F32) for i in range(2)]
                rt = [T(ph, "rt%d" % i, [128, 128], F32) for i in range(2)]
                S.dma('sp', wr32[:, :, 0:4], w_rg[l].rearrange("(k p) n -> p k n", p=128), own=R('wr32'), w=[R('wr32')])
                S.dma('sp', wr32[:, :, 4:36], w_re[l].rearrange("(k p) n -> p k n", p=128), own=R('wr32'), w=[R('wr32')])
                S.dma('sp', brt[:, 0:4], bass.AP(b_rg.tensor, b_rg[l].offset, [[0, 128], [1, 4]]), own=R('brt'), w=[R('brt')])
                S.dma('sp', brt[:, 4:36], bass.AP(b_re.tensor, b_re[l].offset, [[0, 128], [1, 32]]), own=R('brt'), w=[R('brt')])
                for kd in range(2):
                    S.dma('sp', gt2[:, kd, :], bass.AP(modd.tensor, modd[l, kd, 5 * D:6 * D].offset, [[0, 128], [1, D]]),
                          own=R('gt2'), r=[R('modd', l)], w=[R('gt2')])
                S.dma('sp', gfin[:], bass.AP(g_final.tensor, g_final.offset, [[0, 128], [1, D]]), own=R('gfin'), w=[R('gfin')])

                def load_exp(e_, slot):
                    S.dma('pool', wgu[slot][:, :, 0:512], w_eg[l, e_].rearrange("(k p) n -> p k n", p=128),
                          own=R('wgu', slot, 0), w=[R('wgu', slot)])
                    S.dma('pool', wgu[slot][:, :, 512:1024], w_eu[l, e_].rearrange("(k p) n -> p k n", p=128),
                          own=R('wgu', slot, 1), w=[R('wgu', slot)])
                    S.dma('pool', wdn[slot][:], w_ed[l, e_].rearrange("(k p) n -> p k n", p=128),
                          own=R('wdn', slot), w=[R('wdn', slot)])

                ecnt = 0
                for sgi, sgt_tiles in enumerate(SGS):
                    chunks = []
                    cur = []
                    for t in sgt_tiles:
                        if cur and (len(cur) == 4 or kind_of(cur[0]) != kind_of(t)):
                            chunks.append(cur)
                            cur = []
                        cur.append(t)
                    if cur:
                        chunks.append(cur)
                    base = sgt_tiles[0]
                    load_exp(0, ecnt % 2)
                    for ch in chunks:
                        n = len(ch)
                        ntok = n * 128
                        off = (ch[0] - base) * 128
                        norm_mod_T(l, 2, ch, xt, junk, diag, ssb,
                                   lambda c: (tT_sb[:, c, off:off + ntok], [R('tT', t) for t in ch]),
                                   lambda c: (tT32[:, c, 0:ntok], [R('tT32', c)]),
                                   [0, 1, 2, 3], x_src(l, ch, True), x_res(l, ch, True))
                        for ti, tile in enumerate(ch):
                            lt = tile - base
                            b = lt % 2
                            L, Rt = lg[b], rt[b]
                            for k in range(8):
                                S.op('pe', lambda e: e.matmul(ps[4 + b][:, 0:36], lhsT=tT32[:, k, ti * 128:(ti + 1) * 128], rhs=wr32[:, k, :],
                                                              start=(k == 0), stop=(k == 7)),
                                     r=[R('tT32', k), R('wr32')], w=[PR(4 + b)])
                            S.op('dve', lambda e: e.tensor_tensor(L[:], ps[4 + b][:, 0:36], brt[:], ALU.add), r=[PR(4 + b), R('brt')], w=[R(L.name)])
                            S.op('dve', lambda e: e.reduce_max(Rt[:, 0:1], L[:, 0:4], axis=AX.X), r=[R(L.name)], w=[R(Rt.name, 0)])
                            S.op('dve', lambda e: e.tensor_scalar(Rt[:, 1:2], Rt[:, 0:1], -1.0, None, ALU.mult), r=[R(Rt.name, 0)], w=[R(Rt.name, 1)])
                            S.op('act', lambda e: e.activation(Rt[:, 12:16], L[:, 0:4], AF.Exp, bias=Rt[:, 1:2], scale=1.0, accum_out=Rt[:, 2:3]),
                                 r=[R(L.name), R(Rt.name, 1)], w=[R(Rt.name, 2)])
                            S.op('dve', lambda e: e.reciprocal(Rt[:, 3:4], Rt[:, 2:3]), r=[R(Rt.name, 2)], w=[R(Rt.name, 3)])
                            S.op('dve', lambda e: e.tensor_scalar(Rt[:, 4:8], L[:, 0:4], Rt[:, 0:1], None, ALU.is_equal),
                                 r=[R(L.name), R(Rt.name, 0)], w=[R(Rt.name, 4)])
                            S.op('dve', lambda e: e.tensor_scalar(Rt[:, 8:12], Rt[:, 4:8], -1.0, 1e30, ALU.add, ALU.mult),
                                 r=[R(Rt.name, 4)], w=[R(Rt.name, 5)])
                            S.op('dve', lambda e: e.tensor_tensor(Rt[:, 16:48].rearrange("p (g k) -> p g k", g=4),
                                                                  L[:, 4:36].rearrange("p (g k) -> p g k", g=4),
                                                                  mk(Rt, 0, 128, 8, [[1, 4], [0, 8]]), ALU.add),
                                 r=[R(L.name), R(Rt.name, 5)], w=[R(Rt.name, 6)])
                            S.op('dve', lambda e: e.reduce_max(Rt[:, 48:49], Rt[:, 16:48], axis=AX.X), r=[R(Rt.name, 6)], w=[R(Rt.name, 7)])
                            S.op('dve', lambda e: e.tensor_scalar(Rt[:, 56:88], Rt[:, 16:48], Rt[:, 48:49], None, ALU.is_equal),
                                 r=[R(Rt.name, 6), R(Rt.name, 7)], w=[R(Rt.name, 8)])
                            S.op('dve', lambda e: e.scalar_tensor_tensor(Rt[:, 88:120], Rt[:, 56:88], -1e30, Rt[:, 16:48], ALU.mult, ALU.add),
                                 r=[R(Rt.name, 8), R(Rt.name, 6)], w=[R(Rt.name, 9)])
                            S.op('dve', lambda e: e.reduce_max(Rt[:, 49:50], Rt[:, 88:120], axis=AX.X), r=[R(Rt.name, 9)], w=[R(Rt.name, 10)])
                            S.op('dve', lambda e: e.tensor_scalar(Rt[:, 88:120], Rt[:, 88:120], Rt[:, 49:50], None, ALU.is_equal),
                                 r=[R(Rt.name, 9), R(Rt.name, 10)], w=[R(Rt.name, 11)])
                            S.op('dve', lambda e: e.tensor_scalar(Rt[:, 50:51], Rt[:, 48:49], -1.0, None, ALU.mult), r=[R(Rt.name, 7)], w=[R(Rt.name, 12)])
                            S.op('act', lambda e: e.activation(Rt[:, 51:52], Rt[:, 49:50], AF.Exp, bias=Rt[:, 50:51], scale=1.0),
                                 r=[R(Rt.name, 10), R(Rt.name, 12)], w=[R(Rt.name, 13)])
                            S.op('dve', lambda e: e.tensor_scalar(Rt[:, 52:53], Rt[:, 51:52], 1.0, None, ALU.add), r=[R(Rt.name, 13)], w=[R(Rt.name, 14)])
                            S.op('dve', lambda e: e.reciprocal(Rt[:, 53:54], Rt[:, 52:53]), r=[R(Rt.name, 14)], w=[R(Rt.name, 15)])
                            S.op('dve', lambda e: e.tensor_tensor(Rt[:, 53:54], Rt[:, 53:54], Rt[:, 3:4], ALU.mult),
                                 r=[R(Rt.name, 15), R(Rt.name, 3)], w=[R(Rt.name, 15)])
                            S.op('dve', lambda e: e.tensor_tensor(Rt[:, 54:55], Rt[:, 53:54], Rt[:, 51:52], ALU.mult),
                                 r=[R(Rt.name, 15), R(Rt.name, 13)], w=[R(Rt.name, 16)])
                            S.op('dve', lambda e: e.tensor_scalar(Rt[:, 56:88], Rt[:, 56:88], Rt[:, 53:54], None, ALU.mult),
                                 r=[R(Rt.name, 8), R(Rt.name, 15)], w=[R(Rt.name, 8)])
                            S.op('dve', lambda e: e.scalar_tensor_tensor(comb[:, lt, :], Rt[:, 88:120], Rt[:, 54:55], Rt[:, 56:88], ALU.mult, ALU.add),
                                 r=[R(Rt.name, 11), R(Rt.name, 16), R(Rt.name, 8)], w=[R('comb', lt)])
                    nsgt = len(sgt_tiles)
                    S.op('pool', lambda e: e.memset(yacc[:, 0:nsgt, :], 0.0), w=[R('yacc', t) for t in range(nsgt)])
                    pcnt = 0
                    for e_ in range(32):
                        slot = ecnt % 2
                        ecnt += 1
                        if e_ + 1 < 32:
                            load_exp(e_ + 1, ecnt % 2)
                        for ch in chunks:
                            n = len(ch)
                            ntok = n * 128
                            off = (ch[0] - base) * 128
                            a_ = aT[pcnt % 2]
                            for fc in range(4):
                                pg = (pcnt * 4 + fc) % 2
                                pu = 2 + (pcnt * 4 + fc) % 2
                                sgb = sgt[(pcnt * 4 + fc) % 3]
                                for k in range(8):
                                    S.op('pe', lambda e: e.matmul(ps[pg][:, 0:ntok], lhsT=wgu[slot][:, k, fc * 128:(fc + 1) * 128],
                                                                  rhs=tT_sb[:, k, off:off + ntok], start=(k == 0), stop=(k == 7)),
                                         r=[R('wgu', slot)] + [R('tT', t) for t in ch], w=[PR(pg)])
                                for k in range(8):
                                    S.op('pe', lambda e: e.matmul(ps[pu][:, 0:ntok], lhsT=wgu[slot][:, k, 512 + fc * 128:512 + (fc + 1) * 128],
                                                                  rhs=tT_sb[:, k, off:off + ntok], start=(k == 0), stop=(k == 7)),
                                         r=[R('wgu', slot)] + [R('tT', t) for t in ch], w=[PR(pu)])
                                S.op('act', lambda e: e.activation(sgb[:, 0:ntok], ps[pg][:, 0:ntok], AF.Silu), r=[PR(pg)], w=[R(sgb.name)])
                                S.op('dve', lambda e: e.tensor_tensor(a_[:, fc, 0:ntok], sgb[:, 0:ntok], ps[pu][:, 0:ntok], ALU.mult),
                                     r=[R(sgb.name), PR(pu)], w=[R(a_.name, fc)])
                            for ti, tile in enumerate(ch):
                                lt = tile - base
                                for hf in range(2):
                                    py = 4 + (pcnt * 8 + ti * 2 + hf) % 4
                                    for fc in range(4):
                                        S.op('pe', lambda e: e.matmul(ps[py][:, :], lhsT=a_[:, fc, ti * 128:(ti + 1) * 128],
                                                                      rhs=wdn[slot][:, fc, hf * 512:(hf + 1) * 512], start=(fc == 0), stop=(fc == 3)),
                                             r=[R(a_.name, fc), R('wdn', slot)], w=[PR(py)])
                                    S.op('dve', lambda e: e.scalar_tensor_tensor(yacc[:, lt, hf * 512:(hf + 1) * 512], ps[py][:, :],
                                                                                 comb[:, lt, e_:e_ + 1], yacc[:, lt, hf * 512:(hf + 1) * 512],
                                                                                 ALU.mult, ALU.add),
                                         r=[PR(py), R('comb', lt), R('yacc', lt)], w=[R('yacc', lt)])
                            pcnt += 1
                    for ch in chunks:
                        n = len(ch)
                        kd = kind_of(ch[0])
                        S.dma('sp', xt[:, 0:n, :], x_src(l, ch, True), own=R(xt.name), r=x_res(l, ch, True), w=[R(xt.name)])
                        for ti, tile in enumerate(ch):
                            lt = tile - base
                            S.op('pool', lambda e: e.tensor_tensor(yacc[:, lt, :], yacc[:, lt, :], gt2[:, kd, :], ALU.mult),
                                 r=[R('yacc', lt), R('gt2')], w=[R('yacc', lt)])
                            S.op('pool', lambda e: e.tensor_tensor(xt[:, ti, :], xt[:, ti, :], yacc[:, lt, :], ALU.add),
                                 r=[R('yacc', lt), R(xt.name)], w=[R(xt.name)])
                        if not last:
                            S.dma('sp', xs_d[ch[0] * 128:(ch[0] + n) * 128, :].rearrange("(t p) d -> p t d", p=128), xt[:, 0:n, :],
                                  own=R(xt.name), r=[R(xt.name)], w=[R('xs', t) for t in ch])
                        else:
                            for ti in range(n):
                                S.op('act', lambda e: e.activation(junk[:], xt[:, ti, :], AF.Square, accum_out=ssb[:, ti:ti + 1]),
                                     r=[R(xt.name)], w=[R(junk.name), R(ssb.name, ti)])
                            S.op('dve', lambda e: e.tensor_scalar(ssb[:, 4:4 + n], ssb[:, 0:n], 1.0 / D, EPS, ALU.mult, ALU.add),
                                 r=[R(ssb.name, t) for t in range(n)], w=[R(ssb.name, 'm')])
                            S.op('pool', lambda e: e.tensor_tensor(ssb[:, 8:8 + n], ssb[:, 4:4 + n], mhalf[:, 0:n], ALU.pow),
                                 r=[R(ssb.name, 'm'), R('mhalf')], w=[R(ssb.name, 'r')])
                            for ti in range(n):
                                S.op('dve', lambda e: e.scalar_tensor_tensor(xt[:, ti, :], xt[:, ti, :], ssb[:, 8 + ti:9 + ti], gfin[:], ALU.mult, ALU.mult),
                                     r=[R(xt.name), R(ssb.name, 'r'), R('gfin')], w=[R(xt.name)])
                            S.dma('sp', out_d[ch[0] * 128:(ch[0] + n) * 128, :].rearrange("(t p) d -> p t d", p=128), xt[:, 0:n, :],
                                  own=R(xt.name), r=[R(xt.name)], w=[R('out', t) for t in ch])
                S.phase_end()
            if stop == ('moe', l):
                break
        S.barrier()
        build.stats = (S.nops, S.nwaits, S.nds)
    return nc


_CACHE = {}


def kernel(**inputs):
    consts = _host_consts()
    if 'nc' not in _CACHE:
        _CACHE['nc'] = build()
    nc = _CACHE['nc']
    f32 = lambda a: np.ascontiguousarray(np.asarray(a, dtype=np.float32))
    shared = {}
    for k in ('c_ctx', 'w_mod', 'b_mod', 'g_mix', 'g_ffn', 'w_in', 'pool_w', 'pool_scale', 'diff_norm_g',
              'gqa_q_norm', 'gqa_k_norm', 'w_branch', 'w_out', 'w_router_group', 'b_router_group',
              'w_router_expert', 'b_router_expert', 'w_exp_gate', 'w_exp_up', 'w_exp_down', 'g_final'):
        shared[k] = f32(inputs[k])
    shared['diff_lambda'] = f32(inputs['diff_lambda']).reshape(2, 128)
    shared['nat_rpb'] = f32(inputs['nat_rpb']).reshape(2, 60, 31)
    for k, v in consts.items():
        shared[k] = v
    x = f32(inputs['x'])
    ctx = f32(inputs['ctx'])
    c = f32(inputs['c'])
    in_maps = []
    for b in range(8):
        m = dict(shared)
        m['x'] = x[b]
        m['ctx'] = ctx[b]
        m['c'] = c[b]
        in_maps.append(m)
    res = run_bass_kernel_spmd(nc, in_maps, core_ids=list(range(8)))
    return np.stack([np.asarray(r['out'], dtype=np.float32) for r in res.results], axis=0)
```
